# Optimizing a Trainium2 kernel written in Bass

```python
import math
import jax, jax.numpy as jnp
from jax import lax
import numpy as np

D_MODEL = 1024
BATCH = 8
SEQ = 4096
DEPTH = 2

CHUNK = 64
Q_BLOCK = 128
EPS = 1e-6

RET_HEADS = 4
RET_DK = 128
RET_DV = 128
RET_W = RET_HEADS * RET_DV
SB_HEADS = 8
SB_DH = 64
SB_W = SB_HEADS * SB_DH
ROPE_BASE = 10000.0
EVEN_IN = 4 * RET_W + 3 * SB_W
EVEN_MIX = RET_W + SB_W

CONV_W = 512
CONV_K = 3
LRU_W = 512
LRU_HEADS = 8
LRU_BLK = LRU_W // LRU_HEADS
LRU_CONV_K = 4
LRU_C = 8.0
ODD_IN = 3 * CONV_W + 2 * LRU_W
ODD_MIX = CONV_W + LRU_W

D_FF = 2816
N_EXPERTS = 8
TOP_K = 2
D_EXPERT = 3584
EXPERT_BLOCK = 256

N_EVEN = (DEPTH + 1) // 2
N_ODD = DEPTH // 2

kernel_name = "hybrid_retention_stickbreak_shortconv_rglru_moe"


def rmsnorm(x, g):
    xf = x.astype(jnp.float32)
    y = xf * lax.rsqrt(jnp.mean(xf * xf, axis=-1, keepdims=True) + EPS)
    return y.astype(x.dtype) * g


def rotary(x, pos):
    d = x.shape[-1]
    inv_freq = 1.0 / (ROPE_BASE ** (jnp.arange(0, d, 2, dtype=jnp.float32) / d))
    ang = pos[:, None] * inv_freq[None, :]
    cos = jnp.cos(ang)[None, :, None, :].astype(x.dtype)
    sin = jnp.sin(ang)[None, :, None, :].astype(x.dtype)
    x1, x2 = jnp.split(x, 2, axis=-1)
    return jnp.concatenate([x1 * cos - x2 * sin, x1 * sin + x2 * cos], axis=-1)


def retention(q, k, v):
    B, T, H, dk = q.shape
    dv = v.shape[-1]
    n = T // CHUNK
    log_g = jnp.log(1.0 - 2.0 ** (-5.0 - jnp.arange(H, dtype=jnp.float32)))
    idx = jnp.arange(CHUNK, dtype=jnp.float32)
    intra = jnp.exp(log_g[:, None, None] * jnp.abs(idx[:, None] - idx[None, :]))
    q_decay = jnp.exp(log_g[None, :] * (idx + 1.0)[:, None])
    k_decay = jnp.exp(log_g[None, :] * (CHUNK - 1.0 - idx)[:, None])
    chunk_decay = jnp.exp(log_g * CHUNK)

    qc = q.reshape(B, n, CHUNK, H, dk)
    kc = k.reshape(B, n, CHUNK, H, dk)
    vc = v.reshape(B, n, CHUNK, H, dv)

    s = jnp.einsum('bnihd,bnjhd->bnhij', qc, kc) * intra.astype(q.dtype)
    o_intra = jnp.einsum('bnhij,bnjhe->bnihe', s, vc).astype(jnp.float32)

    kc_dec = kc * k_decay.astype(k.dtype)[None, None, :, :, None]
    kv = jnp.einsum('bnjhd,bnjhe->bnhde', kc_dec, vc).astype(jnp.float32)

    def step(state, kv_n):
        return chunk_decay[None, :, None, None] * state + kv_n, state

    _, prev = lax.scan(step, jnp.zeros((B, H, dk, dv), jnp.float32), jnp.moveaxis(kv, 1, 0))
    prev = jnp.moveaxis(prev, 0, 1)
    o_cross = jnp.einsum('bnihd,bnhde->bnihe', qc.astype(jnp.float32), prev) * q_decay[None, None, :, :, None]
    return (o_intra + o_cross).reshape(B, T, H, dv)


def stick_breaking(q, k, v):
    B, T, H, d = q.shape
    scale = d ** -0.5
    outs = []
    for qb in range(T // Q_BLOCK):
        q0 = qb * Q_BLOCK
        kl = q0 + Q_BLOCK
        z = jnp.einsum('bihd,bjhd->bhij', q[:, q0:kl], k[:, :kl]).astype(jnp.float32) * scale
        t_pos = q0 + jnp.arange(Q_BLOCK)
        mask = jnp.arange(kl)[None, :] < t_pos[:, None]
        log_stay = jnp.where(mask, jax.nn.log_sigmoid(-z), 0.0)
        after = lax.cumsum(log_stay, axis=3, reverse=True) - log_stay
        a = jnp.where(mask, jnp.exp(jax.nn.log_sigmoid(z) + after), 0.0)
        outs.append(jnp.einsum('bhij,bjhd->bihd', a.astype(v.dtype), v[:, :kl]))
    return jnp.concatenate(outs, axis=1)


def causal_depthwise_conv(x, w):
    K, C = w.shape
    return lax.conv_general_dilated(
        x, w[:, None, :].astype(x.dtype), window_strides=(1,), padding=[(K - 1, 0)],
        dimension_numbers=('NWC', 'WIO', 'NWC'), feature_group_count=C)


def _lin_rec_combine(left, right):
    a_l, b_l = left
    a_r, b_r = right
    return a_l * a_r, a_r * b_l + b_r


def rg_lru(xr, conv_w, conv_b, wa, ba, wx, bx, lam):
    B, T, W = xr.shape
    xr = causal_depthwise_conv(xr, conv_w) + conv_b
    xh = xr.reshape(B, T, LRU_HEADS, LRU_BLK)
    r = jax.nn.sigmoid(jnp.einsum('bthi,hij->bthj', xh, wa).reshape(B, T, W) + ba)
    ig = jax.nn.sigmoid(jnp.einsum('bthi,hij->bthj', xh, wx).reshape(B, T, W) + bx)
    log_a = (-LRU_C * r.astype(jnp.float32)) * jax.nn.softplus(-lam.astype(jnp.float32))
    a = jnp.exp(log_a)
    mult = jnp.sqrt(-jnp.expm1(2.0 * log_a))
    b = mult * (ig * xr).astype(jnp.float32)
    _, h = lax.associative_scan(_lin_rec_combine, (a, b), axis=1)
    return h


def even_mixer(h, w_in, gn_w, w_out):
    B, T, _ = h.shape
    proj = h @ w_in
    cuts = [RET_W, 2 * RET_W, 3 * RET_W, 4 * RET_W, 4 * RET_W + SB_W, 4 * RET_W + 2 * SB_W]
    rq, rk, rv, rg, sq, sk, sv = jnp.split(proj, cuts, axis=-1)
    pos = jnp.arange(T, dtype=jnp.float32)
    rq = rotary(rq.reshape(B, T, RET_HEADS, RET_DK), pos)
    rk = rotary(rk.reshape(B, T, RET_HEADS, RET_DK), pos) * (RET_DK ** -0.5)
    o = retention(rq, rk, rv.reshape(B, T, RET_HEADS, RET_DV))
    mu = jnp.mean(o, axis=-1, keepdims=True)
    var = jnp.mean(jnp.square(o - mu), axis=-1, keepdims=True)
    o = ((o - mu) * lax.rsqrt(var + EPS)).reshape(B, T, RET_W)
    y_ret = (o.astype(h.dtype) * gn_w) * jax.nn.silu(rg)
    y_sb = stick_breaking(sq.reshape(B, T, SB_HEADS, SB_DH), sk.reshape(B, T, SB_HEADS, SB_DH),
                          sv.reshape(B, T, SB_HEADS, SB_DH)).reshape(B, T, SB_W)
    return jnp.concatenate([y_ret, y_sb.astype(h.dtype)], axis=-1) @ w_out


def odd_mixer(h, w_in, conv_w, lru_conv_w, lru_conv_b, wa, ba, wx, bx, lam, w_out):
    proj = h @ w_in
    cuts = [CONV_W, 2 * CONV_W, 3 * CONV_W, 3 * CONV_W + LRU_W]
    gb, gc, u, xr, xg = jnp.split(proj, cuts, axis=-1)
    y_conv = gb * causal_depthwise_conv(gc * u, conv_w)
    y_lru = rg_lru(xr, lru_conv_w, lru_conv_b, wa, ba, wx, bx, lam).astype(h.dtype) * jax.nn.gelu(xg)
    return jnp.concatenate([y_conv, y_lru], axis=-1) @ w_out


def swiglu(h, w_gate, w_up, w_down):
    return (jax.nn.silu(h @ w_gate) * (h @ w_up)) @ w_down


def moe_swiglu(h, w_router, b_router, w_gate, w_up, w_down):
    B, T, D = h.shape
    xt = h.reshape(-1, D)
    N = xt.shape[0]
    logits = (xt @ w_router).astype(jnp.float32) + b_router.astype(jnp.float32)
    top_logit, top_idx = lax.top_k(logits, TOP_K)
    top_w = jax.nn.softmax(top_logit, axis=-1)
    flat_e = top_idx.reshape(-1)
    flat_tok = jnp.repeat(jnp.arange(N, dtype=jnp.int32), TOP_K)
    flat_w = top_w.reshape(-1)
    order = jnp.argsort(flat_e)
    sorted_e = flat_e[order]
    counts = jnp.bincount(flat_e, length=N_EXPERTS)
    start = jnp.cumsum(counts) - counts
    padded = (counts + EXPERT_BLOCK - 1) // EXPERT_BLOCK * EXPERT_BLOCK
    pad_end = jnp.cumsum(padded)
    pad_start = pad_end - padded
    rank = jnp.arange(N * TOP_K) - start[sorted_e]
    dest = pad_start[sorted_e] + rank
    n_slots = N * TOP_K + N_EXPERTS * EXPERT_BLOCK
    n_blocks = n_slots // EXPERT_BLOCK
    slot_tok = jnp.zeros((n_slots,), jnp.int32).at[dest].set(flat_tok[order])
    slot_w = jnp.zeros((n_slots,), jnp.float32).at[dest].set(flat_w[order])
    block_e = jnp.minimum(jnp.searchsorted(pad_end, jnp.arange(n_blocks) * EXPERT_BLOCK, side='right'),
                          N_EXPERTS - 1)
    xb = xt[slot_tok].reshape(n_blocks, EXPERT_BLOCK, D)

    def expert_block(args):
        xg, e = args
        return (jax.nn.silu(xg @ w_gate[e]) * (xg @ w_up[e])) @ w_down[e]

    yb = lax.map(expert_block, (xb, block_e)).reshape(n_slots, D)
    y = yb * slot_w[:, None].astype(yb.dtype)
    out = jnp.zeros_like(xt).at[slot_tok].add(y)
    return out.reshape(B, T, D)


def _normal(k, shape, scale):
    return jax.random.normal(k, shape, jnp.float32) * scale


def setup_inputs(seed: int = 0) -> dict:
    key = jax.random.key(seed)
    ks = jax.random.split(key, 32)
    D = D_MODEL
    u = jax.random.uniform(ks[18], (N_ODD, LRU_W), jnp.float32, 0.9, 0.999)
    a0 = u ** (1.0 / LRU_C)
    lam = jnp.log(a0) - jnp.log1p(-a0)
    return {
        "x": _normal(ks[0], (BATCH, SEQ, D), 1.0),
        "norm_mix": 1.0 + _normal(ks[1], (DEPTH, D), 0.02),
        "norm_ffn": 1.0 + _normal(ks[2], (DEPTH, D), 0.02),
        "norm_final": 1.0 + _normal(ks[3], (D,), 0.02),
        "ev_w_in": _normal(ks[4], (N_EVEN, D, EVEN_IN), D ** -0.5),
        "ev_ret_gn": 1.0 + _normal(ks[5], (N_EVEN, RET_W), 0.02),
        "ev_w_out": _normal(ks[6], (N_EVEN, EVEN_MIX, D), EVEN_MIX ** -0.5),
        "ev_ffn_gate": _normal(ks[7], (N_EVEN, D, D_FF), D ** -0.5),
        "ev_ffn_up": _normal(ks[8], (N_EVEN, D, D_FF), D ** -0.5),
        "ev_ffn_down": _normal(ks[9], (N_EVEN, D_FF, D), D_FF ** -0.5),
        "od_w_in": _normal(ks[10], (N_ODD, D, ODD_IN), D ** -0.5),
        "od_conv_w": _normal(ks[11], (N_ODD, CONV_K, CONV_W), CONV_K ** -0.5),
        "od_lru_conv_w": _normal(ks[12], (N_ODD, LRU_CONV_K, LRU_W), LRU_CONV_K ** -0.5),
        "od_lru_conv_b": _normal(ks[13], (N_ODD, LRU_W), 0.01),
        "od_lru_wa": _normal(ks[14], (N_ODD, LRU_HEADS, LRU_BLK, LRU_BLK), LRU_BLK ** -0.5),
        "od_lru_ba": _normal(ks[15], (N_ODD, LRU_W), 0.01),
        "od_lru_wx": _normal(ks[16], (N_ODD, LRU_HEADS, LRU_BLK, LRU_BLK), LRU_BLK ** -0.5),
        "od_lru_bx": _normal(ks[17], (N_ODD, LRU_W), 0.01),
        "od_lru_lambda": lam,
        "od_w_out": _normal(ks[19], (N_ODD, ODD_MIX, D), ODD_MIX ** -0.5),
        "od_router_w": _normal(ks[20], (N_ODD, D, N_EXPERTS), D ** -0.5),
        "od_router_b": _normal(ks[21], (N_ODD, N_EXPERTS), 0.01),
        "od_exp_gate": _normal(ks[22], (N_ODD, N_EXPERTS, D, D_EXPERT), D ** -0.5),
        "od_exp_up": _normal(ks[23], (N_ODD, N_EXPERTS, D, D_EXPERT), D ** -0.5),
        "od_exp_down": _normal(ks[24], (N_ODD, N_EXPERTS, D_EXPERT, D), D_EXPERT ** -0.5),
    }


def reference(x, norm_mix, norm_ffn, norm_final, ev_w_in, ev_ret_gn, ev_w_out,
              ev_ffn_gate, ev_ffn_up, ev_ffn_down, od_w_in, od_conv_w, od_lru_conv_w,
              od_lru_conv_b, od_lru_wa, od_lru_ba, od_lru_wx, od_lru_bx, od_lru_lambda,
              od_w_out, od_router_w, od_router_b, od_exp_gate, od_exp_up, od_exp_down):
    for layer in range(DEPTH):
        j = layer // 2
        h = rmsnorm(x, norm_mix[layer])
        if layer % 2 == 0:
            x = x + even_mixer(h, ev_w_in[j], ev_ret_gn[j], ev_w_out[j])
            h = rmsnorm(x, norm_ffn[layer])
            x = x + swiglu(h, ev_ffn_gate[j], ev_ffn_up[j], ev_ffn_down[j])
        else:
            x = x + odd_mixer(h, od_w_in[j], od_conv_w[j], od_lru_conv_w[j], od_lru_conv_b[j],
                              od_lru_wa[j], od_lru_ba[j], od_lru_wx[j], od_lru_bx[j],
                              od_lru_lambda[j], od_w_out[j])
            h = rmsnorm(x, norm_ffn[layer])
            x = x + moe_swiglu(h, od_router_w[j], od_router_b[j], od_exp_gate[j],
                               od_exp_up[j], od_exp_down[j])
    return rmsnorm(x, norm_final)
```

```python
import contextlib
import numpy as np
import ml_dtypes
import concourse.bass as bass
import concourse.mybir as mybir
from concourse.bass_utils import run_bass_kernel_spmd
from concourse.alu_op_type import AluOpType as ALU

AF = mybir.ActivationFunctionType
F32 = mybir.dt.float32
BF16 = mybir.dt.bfloat16
I32 = mybir.dt.int32
U32 = mybir.dt.uint32
AX = mybir.AxisListType

SAME_ENGINE_SYNC = True
NDMA_SLOTS = {"sp": 20, "pool": 8, "act": 4}


class Buf:
    __slots__ = ("w", "r")

    def __init__(self):
        self.w = None
        self.r = {}


class Prog:
    def __init__(self, nc, es):
        self.nc = nc
        self.es = es
        self.eng = {"pe": nc.tensor, "act": nc.scalar, "dve": nc.vector, "pool": nc.gpsimd, "sp": nc.sync}
        self.q = {e: [] for e in self.eng}
        self.sems = []
        self.own = {}
        for e in ("pe", "act", "dve", "pool"):
            self.own[e] = self._newsem("c_" + e)
        self.cnt = {e: 0 for e in self.own}
        self.waited = {e: {} for e in self.eng}
        self.slots = {}
        self.dn = {}
        for qn, k in NDMA_SLOTS.items():
            self.slots[qn] = [[self._newsem("d_%s%d" % (qn, i)), 0] for i in range(k)]
            self.dn[qn] = 0
        self.n_inst = 0

    def _newsem(self, name):
        s = self.es.enter_context(self.nc.semaphore(name))
        self.sems.append(s)
        return len(self.sems) - 1

    def _deps(self, reads, writes):
        deps = {}
        for b in reads:
            if b.w is not None and deps.get(b.w[0], 0) < b.w[1]:
                deps[b.w[0]] = b.w[1]
        for b in writes:
            if b.w is not None and deps.get(b.w[0], 0) < b.w[1]:
                deps[b.w[0]] = b.w[1]
            for s, v in b.r.items():
                if deps.get(s, 0) < v:
                    deps[s] = v
        return deps

    def _waits(self, eng, deps, skip=None):
        wd = self.waited[eng]
        waits = []
        for s, v in deps.items():
            if s == skip:
                continue
            if wd.get(s, 0) >= v:
                continue
            wd[s] = v
            waits.append((s, v))
        return waits

    def _mark(self, ev, reads, writes):
        s, v = ev
        for b in reads:
            if b.r.get(s, 0) < v:
                b.r[s] = v
        for b in writes:
            b.w = ev
            b.r = {}

    def op(self, eng, fn, reads=(), writes=()):
        own = self.own[eng]
        deps = self._deps(reads, writes)
        skip = own if (eng == "pe" or not SAME_ENGINE_SYNC) else None
        waits = self._waits(eng, deps, skip)
        self.cnt[eng] += 1
        ev = (own, self.cnt[eng])
        self.q[eng].append((waits, fn, own, 1))
        self._mark(ev, reads, writes)
        self.n_inst += 1

    def dma(self, qn, fn, reads=(), writes=()):
        deps = self._deps(reads, writes)
        sl = self.slots[qn][self.dn[qn] % len(self.slots[qn])]
        self.dn[qn] += 1
        if sl[1] > 0 and deps.get(sl[0], 0) < sl[1]:
            deps[sl[0]] = sl[1]
        waits = self._waits(qn, deps)
        sl[1] += 16
        ev = (sl[0], sl[1])
        self.q[qn].append((waits, fn, sl[0], 16))
        self._mark(ev, reads, writes)
        self.n_inst += 1

    def finish(self):
        waits = []
        for qn in self.slots:
            for s, v in self.slots[qn]:
                if v > 0:
                    waits.append((s, v))
        for e in self.own:
            if self.cnt[e] > 0:
                waits.append((self.own[e], self.cnt[e]))
        self.q["sp"].append((waits, None, None, 0))

    def barrier(self):
        allw = {}
        for qn in self.slots:
            for s, v in self.slots[qn]:
                if v > 0:
                    allw[s] = v
        for e in self.own:
            if self.cnt[e] > 0:
                allw[self.own[e]] = self.cnt[e]
        for eng in self.eng:
            waits = self._waits(eng, allw)
            if waits:
                self.q[eng].append((waits, None, None, 0))

    @contextlib.contextmanager
    def phase(self):
        old = self.es
        with contextlib.ExitStack() as pes:
            self.es = pes
            yield
            self.barrier()
            self.emit()
        self.es = old

    def simulate(self, q):
        if not hasattr(self, "_simval"):
            self._simval = {}
        val = self._simval
        pos = {e: 0 for e in q}
        progress = True
        while progress:
            progress = False
            for e in q:
                while pos[e] < len(q[e]):
                    waits, fn, s_, inc = q[e][pos[e]]
                    if any(val.get(ws, 0) < wv for ws, wv in waits):
                        break
                    if fn is not None:
                        val[s_] = val.get(s_, 0) + inc
                    pos[e] += 1
                    progress = True
        stuck = {e: pos[e] for e in q if pos[e] < len(q[e])}
        if stuck:
            msg = []
            for e, i in stuck.items():
                waits = q[e][i][0]
                msg.append("%s@%d/%d waits %s have %s" % (e, i, len(q[e]), waits, [val.get(ws, 0) for ws, _ in waits]))
            raise RuntimeError("DEADLOCK in emitted program: " + "; ".join(msg))

    def emit(self):
        nc = self.nc
        sems = self.sems
        q = self.q
        self.q = {e: [] for e in self.eng}
        self.simulate(q)
        with nc.Block() as block:
            def mk(ename):
                def body(e):
                    for waits, fn, s, inc in q[ename]:
                        for ws, wv in waits:
                            e.wait_ge(sems[ws], wv)
                        if fn is not None:
                            fn(e).then_inc(sems[s], inc)
                return body
            block.tensor(mk("pe"))
            block.scalar(mk("act"))
            block.vector(mk("dve"))
            block.gpsimd(mk("pool"))
            block.sync(mk("sp"))


class T:
    _n = [0]

    def __init__(self, p, name, shape, dtype, psum=False):
        T._n[0] += 1
        name = "%s_%d" % (name, T._n[0])
        if psum:
            self.t = p.es.enter_context(p.nc.psum_tensor(name, shape, dtype))
        else:
            self.t = p.es.enter_context(p.nc.sbuf_tensor(name, shape, dtype))
        self.b = Buf()

    def __getitem__(self, k):
        return self.t[k]


class D:
    def __init__(self, nc, name, shape, dtype, kind="Internal"):
        self.t = nc.dram_tensor(name, list(shape), dtype, kind=kind)
        self.bufs = {}

    @classmethod
    def wrap(cls, handle):
        o = cls.__new__(cls)
        o.t = handle
        o.bufs = {}
        return o

    def b(self, key=0):
        if key not in self.bufs:
            self.bufs[key] = Buf()
        return self.bufs[key]

    def ap(self):
        return self.t.ap()


TT = 4096
DM = 1024
NSUB = TT // 128
EVW = 4608
DFF = 2816
DEXP = 3584
NEXP = 8
CAP = 1280
SB_FILL = 0
SB_WB = 32
EPS = 1e-6


def host_consts():
    c = {}
    c["ident"] = np.eye(128, dtype=ml_dtypes.bfloat16)
    c["identf"] = np.eye(128, dtype=np.float32)
    inv = (1.0 / (np.float32(10000.0) ** (np.arange(0, 128, 2, dtype=np.float32) / np.float32(128)))).astype(np.float32)
    ang = (np.arange(TT, dtype=np.float32)[None, :] * inv[:, None]).astype(np.float32)
    cos = np.cos(ang.astype(np.float64)).astype(np.float32)
    sin = np.sin(ang.astype(np.float64)).astype(np.float32)
    c["cosT"] = np.concatenate([cos, cos], 0)
    c["sinT"] = np.concatenate([-sin, sin], 0)
    g = 1.0 - 2.0 ** (-5.0 - np.arange(4, dtype=np.float64))
    i = np.arange(128)
    jj, ii = np.meshgrid(i, i, indexing="ij")
    same = (jj // 64) == (ii // 64)
    causal2 = (jj < 64) & (ii >= 64)
    m = np.zeros((128, 4, 128), np.float64)
    for h in range(4):
        m[:, h, :] = np.where(same | causal2, g[h] ** np.abs(ii - jj), 0.0) * 128 ** -0.5
    c["rmask"] = m.astype(np.float32)
    qd = np.zeros((128, 4, 512), np.float64)
    for h in range(4):
        qd[:, h, :] = (g[h] ** ((np.arange(512) % 128) + 1.0))[None, :]
    c["qdec"] = qd.astype(np.float32)
    kd = np.zeros((128, 4), np.float64)
    for h in range(4):
        kd[:, h] = g[h] ** (127.0 - i) * 128 ** -0.5
    c["kdec"] = kd.astype(np.float32)
    c["_cd"] = [float(g[h] ** 128) for h in range(4)]
    sm = np.zeros((128, 4, 512), np.float32)
    s = np.arange(128)[:, None]
    t = np.arange(512)[None, :]
    for r in range(4):
        sm[:, r, :] = ((r * 128 + s) < t)
    c["sbmask"] = sm.astype(ml_dtypes.bfloat16)
    c["negtri"] = (-(jj >= ii).astype(np.float32)).astype(ml_dtypes.bfloat16)
    c["negones"] = (-np.ones((128, 128), np.float32)).astype(ml_dtypes.bfloat16)
    c["negcomp"] = (-(jj < ii).astype(np.float32)).astype(ml_dtypes.bfloat16)
    c["triex"] = ((jj < ii).astype(np.float32)).astype(ml_dtypes.bfloat16)
    c["ones"] = np.ones((128, 128), ml_dtypes.bfloat16)
    c["eoff"] = np.tile((np.arange(8, dtype=np.float32) * CAP)[None, :], (128, 1))
    return c


CONST_DT = {"ident": BF16, "identf": F32, "cosT": F32, "sinT": F32, "rmask": F32, "qdec": F32, "kdec": F32,
            "sbmask": BF16, "negtri": BF16, "negones": BF16, "negcomp": BF16, "triex": BF16, "ones": BF16, "eoff": F32}

IN_SHAPES = {
    "x": ([TT, DM], F32), "norms": ([5, DM], F32),
    "ev_w_in": ([DM, EVW], F32), "ev_gn": ([1, 512], F32), "ev_w_out": ([DM, DM], F32),
    "ev_gate": ([DM, DFF], F32), "ev_up": ([DM, DFF], F32), "ev_down": ([DFF, DM], F32),
    "od_w_in": ([DM, 2560], F32), "od_small": ([128, 4, 12], F32), "od_wa": ([128, 4, 128], F32),
    "od_wx": ([128, 4, 128], F32), "od_w_out": ([DM, DM], F32), "od_rw": ([DM, 8], F32), "od_rb": ([1, 8], F32),
    "od_eg": ([NEXP, DM, DEXP], F32), "od_eu": ([NEXP, DM, DEXP], F32), "od_ed": ([NEXP, DEXP, DM], F32),
}


def rmsnorm_tile(p, W, xt, gt, ht, tag=""):
    sq, ss, rs = W["sq"], W["ss"], W["rs"]
    p.op("act", lambda e: e.activation(out=sq[:], in_=xt[:], func=AF.Square, accum_out=ss[:]), reads=[xt.b], writes=[sq.b, ss.b])
    p.op("dve", lambda e: e.tensor_scalar(out=rs[:], in0=ss[:], scalar1=1.0 / DM, scalar2=EPS, op0=ALU.mult, op1=ALU.add), reads=[ss.b], writes=[rs.b])
    p.op("act", lambda e: e.activation(out=rs[:], in_=rs[:], func=AF.Sqrt), reads=[rs.b], writes=[rs.b])
    p.op("dve", lambda e: e.reciprocal(out=rs[:], in_=rs[:]), reads=[rs.b], writes=[rs.b])
    p.op("dve", lambda e: e.scalar_tensor_tensor(out=ht[:], in0=xt[:], scalar=rs[:], in1=gt[:], op0=ALU.mult, op1=ALU.mult), reads=[xt.b, rs.b, gt.b], writes=[ht.b])


def build(upto=99, taps=()):
    nc = bass.Bass("TRN2", target_bir_lowering=False)
    cs = host_consts()
    cd = cs.pop("_cd")
    shp = dict(IN_SHAPES)
    if upto < 7:
        for k in ("od_eg", "od_eu", "od_ed"):
            shp[k] = ([1, 8, 8], F32)
    IN = {k: nc.dram_tensor(k, v[0], v[1], kind="ExternalInput") for k, v in shp.items()}
    CI = {k: nc.dram_tensor("c_" + k, list(cs[k].shape), CONST_DT[k], kind="ExternalInput") for k in cs}
    out = D(nc, "out", [TT, DM], F32, kind="ExternalOutput")

    def scratch(name, shape, dt):
        return D(nc, name, shape, dt, kind="ExternalOutput" if name in taps else "Internal")

    sb_qT = scratch("sb_qT", [512, TT], BF16)
    sb_kT = scratch("sb_kT", [512, TT], BF16)
    sb_v = scratch("sb_v", [TT, 512], BF16)
    ymixT = scratch("ymixT", [DM, TT], BF16)
    x1 = scratch("x1", [TT, DM], F32)
    x2 = scratch("x2", [TT, DM], F32)
    ymix1T = scratch("ymix1T", [DM, TT], BF16)
    x3 = scratch("x3", [TT, DM], F32)
    hn = scratch("hn", [TT, DM], BF16)
    rinfo = scratch("rinfo", [TT, 8], F32)
    ffn_bf = [scratch("ffn_gate_bf", [DM, DFF], BF16), scratch("ffn_up_bf", [DM, DFF], BF16), scratch("ffn_down_bf", [DFF, DM], BF16)]
    xg = scratch("xg", [NEXP * CAP, DM], BF16)
    yg = scratch("yg", [NEXP * CAP, DM], F32)

    with contextlib.ExitStack() as es:
        p = Prog(nc, es)
        class CL:
            def __init__(self):
                self.c = {}

            def __getitem__(self, k):
                if k not in self.c:
                    t = T(p, "k_" + k, list(cs[k].shape), CONST_DT[k])
                    p.dma("sp", lambda e: e.dma_start(out=t[:], in_=CI[k].ap()), writes=[t.b])
                    self.c[k] = t
                return self.c[k]

        class GL:
            def __init__(self):
                self.c = {}

            def __getitem__(self, i):
                if i not in self.c:
                    g = T(p, "g%d" % i, [128, DM], F32)
                    p.dma("sp", lambda e: e.dma_start(out=g[:], in_=IN["norms"].ap()[i:i + 1, :].partition_broadcast(128)), writes=[g.b])
                    self.c[i] = g
                return self.c[i]

        def newW():
            return {"sq": T(p, "n_sq", [128, DM], F32), "ss": T(p, "n_ss", [128, 1], F32), "rs": T(p, "n_rs", [128, 1], F32)}
        env = {"CL": CL, "GL": GL, "newW": newW, "CI": CI}
        xin = D.wrap(IN["x"])

        if upto >= 1:
            phase1(p, IN, env, cd, sb_qT, sb_kT, sb_v, ymixT, xin)
        if upto >= 2:
            phase2(p, IN, env, sb_qT, sb_kT, sb_v, ymixT, wcast=list(zip(("ev_gate", "ev_up", "ev_down"), ffn_bf)), xg=xg)
        if upto >= 3:
            phase3(p, IN, env, ymixT, xin, x1, "ev_w_out", lambda it: [("r", it)] + [("s", h, it) for h in range(8)])
        with contextlib.ExitStack() as s45:
            old_es, p.es = p.es, s45
            PW = p5_weights_alloc(p)
            p.es = old_es
            if upto >= 4:
                phase4(p, IN, env, x1, x2, pre_hook=(lambda: p5_weights_load(p, IN, PW)) if upto >= 5 else None, wbf=ffn_bf)
            if upto >= 5:
                phase5(p, IN, env, x2, x3, PW)
        R = {"rt": T(p, "rt", [128, NSUB, 20], F32), "idx12": T(p, "idx12", [128, NSUB, 2], I32)}
        if upto >= 6:
            phase6(p, IN, env, x3, xg, R)
        if upto >= 7:
            phase7(p, IN, env, xg, yg)
        if upto >= 8:
            phase8(p, IN, env, x3, yg, out, R)
        p.finish()
    return nc, cs


def phase1(p, IN, env, cd, sb_qT, sb_kT, sb_v, ymixT, x_src):
    with p.phase():
        C, G, W = env["CL"](), env["GL"](), env["newW"]()
        win = T(p, "win", [128, 8, EVW], BF16)
        wb = [Buf() for _ in range(9)]
        for cg in (0, 7, 1, 8, 4, 5, 2, 3, 6):
            p.dma("pool", lambda e, cg=cg: e.dma_start(out=win[:, :, cg * 512:(cg + 1) * 512],
                  in_=IN["ev_w_in"].ap()[:, cg * 512:(cg + 1) * 512].rearrange("(k p) n -> p k n", p=128)), writes=[wb[cg]])
        gnb = T(p, "gnb", [128, 512], F32)
        p.dma("sp", lambda e: e.dma_start(out=gnb[:], in_=IN["ev_gn"].ap().partition_broadcast(128)), writes=[gnb.b])
        xt = [T(p, "xt%d" % i, [128, DM], F32) for i in range(2)]
        ht = [T(p, "ht%d" % i, [128, DM], BF16) for i in range(2)]
        hT = [T(p, "hT%d" % i, [128, 8, 512], BF16) for i in range(2)]
        hTb = [[Buf() for _ in range(4)] for _ in range(2)]
        pT = T(p, "pT", [128, 8, 128], BF16, psum=True)
        pF = [T(p, "pF%d" % i, [128, 512], F32, psum=True) for i in range(2)]
        pM = T(p, "pM", [128, 512], F32, psum=True)
        pS = T(p, "pS", [128, 4, 128], F32, psum=True)
        pK = T(p, "pK", [128, 4, 128], BF16, psum=True)
        pOs = [T(p, "pO%d" % i, [128, 4, 128], F32, psum=True) for i in range(2)]
        pKV = pS
        t1 = [T(p, "t1_%d" % i, [128, 512], F32) for i in range(2)]
        t2 = [T(p, "t2_%d" % i, [128, 512], F32) for i in range(2)]
        qT = [T(p, "qT%d" % h, [128, 512], BF16) for h in range(4)]
        qdT = [T(p, "qdT%d" % h, [128, 512], BF16) for h in range(4)]
        kT = [T(p, "kT%d" % h, [128, 512], BF16) for h in range(4)]
        sbq = [T(p, "sbq0", [128, 4, 512], BF16)] * 2
        sbk = [T(p, "sbk0", [128, 4, 512], BF16)] * 2
        svt = [T(p, "svt0", [128, 4, 512], BF16)] * 2
        cstt = [T(p, "cst%d" % i, [128, 512], F32) for i in range(2)]
        sntt = [T(p, "snt%d" % i, [128, 512], F32) for i in range(2)]
        vt = [T(p, "vt%d" % i, [128, 512], BF16) for i in range(4)]
        Gt = [T(p, "Gt%d" % i, [128, 512], F32) for i in range(4)]
        Pt = T(p, "Pt", [128, 4, 128], BF16)
        kd = T(p, "kd", [128, 4, 128], BF16)
        st = T(p, "st", [128, 4, 128], F32)
        stbf = T(p, "stbf", [128, 4, 128], BF16)
        bst = T(p, "bst", [128, 4, 6], F32)
        mv = T(p, "mv", [128, 4, 2], F32)
        rstd = T(p, "rstd", [128, 4], F32)
        nb = T(p, "nb", [128, 4], F32)
        on = T(p, "on", [128, 512], F32)
        yr = T(p, "yr", [128, 512], BF16)
        yT = [T(p, "yT%d" % i, [128, 4, 512], BF16) for i in range(2)]
        ident = C["ident"]

        p.op("pool", lambda e: e.memset(st[:], 0.0), writes=[st.b])
        p.op("pool", lambda e: e.memset(stbf[:], 0.0), writes=[stbf.b])

        def wcols(c0, n):
            return [wb[i] for i in range(c0 // 512, (c0 + n - 1) // 512 + 1)]

        def fm_proj(ps, c0, hTt, hbs):
            for k in range(8):
                p.op("pe", lambda e, k=k: e.matmul(ps[:], lhsT=win[:, k, c0:c0 + 128], rhs=hTt[:, k, :], start=(k == 0), stop=(k == 7)),
                     reads=wcols(c0, 128) + hbs, writes=[ps.b])

        def run_rr(gens):
            gens = list(gens)
            while gens:
                for g_ in list(gens):
                    try:
                        next(g_)
                    except StopIteration:
                        gens.remove(g_)

        def A_gen(it):
            par = it % 2
            t0 = it * 512

            def nrm(sub):
                xs = xt[sub % 2]
                hs = ht[sub % 2]
                r0 = t0 + sub * 128
                p.dma("sp", lambda e, xs=xs, r0=r0: e.dma_start(out=xs[:], in_=x_src.ap()[r0:r0 + 128, :]), reads=[x_src.b(r0 // 128)], writes=[xs.b])
                rmsnorm_tile(p, W, xs, G[0], hs)

            def trs(sub):
                hs = ht[sub % 2]
                for k in range(8):
                    p.op("pe", lambda e, k=k, hs=hs: e.transpose(out=pT[:, k, :], in_=hs[:, k * 128:(k + 1) * 128], identity=ident[:]),
                         reads=[hs.b, ident.b], writes=[pT.b])
                p.op("act", lambda e, sub=sub, par=par: e.copy(out=hT[par][:, :, sub * 128:(sub + 1) * 128], in_=pT[:]), reads=[pT.b], writes=[hTb[par][sub]])

            for fn_, sub in ((nrm, 0), (nrm, 1), (trs, 0), (nrm, 2), (trs, 1), (nrm, 3), (trs, 2), (trs, 3)):
                fn_(sub)
                yield

        def do_tile(it):
            par = it % 2
            t0 = it * 512
            hTt = hT[par]
            hbs = hTb[par]
            cst, snt = cstt[par], sntt[par]
            p.dma("sp", lambda e, cst=cst: e.dma_start(out=cst[:], in_=env["CI"]["cosT"].ap()[:, t0:t0 + 512]), writes=[cst.b])
            p.dma("sp", lambda e, snt=snt: e.dma_start(out=snt[:], in_=env["CI"]["sinT"].ap()[:, t0:t0 + 512]), writes=[snt.b])
            cosv, sinv = cst[:], snt[:]
            for h in range(4):
                for which in range(2):
                    cbase = which * 512 + h * 128
                    sbase = 3584 + which * 512 + h * 128
                    a, b = t1[which], t2[which]
                    fm_proj(pF[0], cbase, hTt, hbs)
                    p.op("dve", lambda e, a=a: e.tensor_tensor(out=a[:], in0=pF[0][:], in1=cosv, op=ALU.mult), reads=[pF[0].b, cst.b], writes=[a.b])
                    fm_proj(pF[1], sbase, hTt, hbs)
                    p.op("dve", lambda e, b=b: e.tensor_tensor(out=b[:], in0=pF[1][:], in1=sinv, op=ALU.mult), reads=[pF[1].b, snt.b], writes=[b.b])
                    if which == 0:
                        p.op("pool", lambda e, a=a, b=b, h=h: e.tensor_tensor(out=qT[h][:], in0=a[:], in1=b[:], op=ALU.add), reads=[a.b, b.b], writes=[qT[h].b])
                        p.op("pool", lambda e, a=a, b=b: e.tensor_tensor(out=a[:], in0=a[:], in1=b[:], op=ALU.add), reads=[a.b, b.b], writes=[a.b])
                        p.op("pool", lambda e, a=a, h=h: e.tensor_tensor(out=qdT[h][:], in0=a[:], in1=C["qdec"][:, h, :], op=ALU.mult), reads=[a.b, C["qdec"].b], writes=[qdT[h].b])
                    else:
                        p.op("dve", lambda e, a=a, b=b, h=h: e.tensor_tensor(out=kT[h][:], in0=a[:], in1=b[:], op=ALU.add), reads=[a.b, b.b], writes=[kT[h].b])
            def C_gen():
                for which, (stg, dst) in enumerate(((sbq[par], sb_qT), (sbk[par], sb_kT))):
                    for g in range(4):
                        ps = pF[g % 2]
                        fm_proj(ps, 2048 + which * 512 + g * 128, hTt, hbs)
                        p.op("act", lambda e, ps=ps, stg=stg, g=g: e.copy(out=stg[:, g, :], in_=ps[:]), reads=[ps.b], writes=[stg.b])
                        yield
                    p.dma("sp", lambda e, stg=stg, dst=dst: e.dma_start(out=dst.ap()[:, t0:t0 + 512].rearrange("(g p) t -> p g t", p=128), in_=stg[:]),
                          reads=[stg.b], writes=[dst.b(it)])
            def D_gen(sub):
                for (c0, kind) in ((1024, "v"), (1536, "g"), (3072, "sv")):
                    for k in range(8):
                        p.op("pe", lambda e, k=k, sub=sub, c0=c0: e.matmul(pM[:], lhsT=hTt[:, k, sub * 128:(sub + 1) * 128], rhs=win[:, k, c0:c0 + 512], start=(k == 0), stop=(k == 7)),
                             reads=wcols(c0, 512) + [hbs[sub]], writes=[pM.b])
                    if kind == "v":
                        p.op("act", lambda e, sub=sub: e.copy(out=vt[sub][:], in_=pM[:]), reads=[pM.b], writes=[vt[sub].b])
                    elif kind == "g":
                        p.op("act", lambda e, sub=sub: e.activation(out=Gt[sub][:], in_=pM[:], func=AF.Silu), reads=[pM.b], writes=[Gt[sub].b])
                        p.op("pool", lambda e, sub=sub: e.tensor_tensor(out=Gt[sub][:], in0=Gt[sub][:], in1=gnb[:], op=ALU.mult), reads=[Gt[sub].b, gnb.b], writes=[Gt[sub].b])
                    else:
                        p.op("act", lambda e, sub=sub, par=par: e.copy(out=svt[par][:, sub, :], in_=pM[:]), reads=[pM.b], writes=[svt[par].b])
                    yield
                if sub == 3:
                    p.dma("sp", lambda e, par=par: e.dma_start(out=sb_v.ap()[t0:t0 + 512, :].rearrange("(s p) c -> p s c", p=128), in_=svt[par][:]),
                          reads=[svt[par].b], writes=[sb_v.b(it)])
            def E1_gen(sub):
                sl = slice(sub * 128, (sub + 1) * 128)
                pO = pOs[sub % 2]
                for h in range(4):
                    p.op("pe", lambda e, h=h, sl=sl: e.matmul(pS[:, h, :], lhsT=kT[h][:, sl], rhs=qT[h][:, sl], start=True, stop=True, skip_group_check=True),
                         reads=[kT[h].b, qT[h].b], writes=[pS.b])
                yield
                p.op("dve", lambda e: e.tensor_tensor(out=Pt[:], in0=pS[:], in1=C["rmask"][:], op=ALU.mult), reads=[pS.b, C["rmask"].b], writes=[Pt.b])
                yield
                for h in range(4):
                    p.op("pe", lambda e, h=h, sl=sl: e.transpose(out=pK[:, h, :], in_=kT[h][:, sl], identity=ident[:]), reads=[kT[h].b, ident.b], writes=[pK.b])
                yield
                for h in range(4):
                    p.op("dve", lambda e, h=h: e.tensor_scalar(out=kd[:, h, :], in0=pK[:, h, :], scalar1=C["kdec"][:, h:h + 1], scalar2=None, op0=ALU.mult),
                         reads=[pK.b, C["kdec"].b], writes=[kd.b])
                yield
                for h in range(4):
                    hs_ = slice(h * 128, (h + 1) * 128)
                    p.op("pe", lambda e, h=h, hs_=hs_, sub=sub: e.matmul(pO[:, h, :], lhsT=Pt[:, h, :], rhs=vt[sub][:, hs_], start=True, stop=False, skip_group_check=True),
                         reads=[Pt.b, vt[sub].b], writes=[pO.b])
                    p.op("pe", lambda e, h=h, sl=sl: e.matmul(pO[:, h, :], lhsT=qdT[h][:, sl], rhs=stbf[:, h, :], start=False, stop=True, skip_group_check=True),
                         reads=[qdT[h].b, stbf.b], writes=[pO.b])
                yield
                for h in range(4):
                    hs_ = slice(h * 128, (h + 1) * 128)
                    p.op("pe", lambda e, h=h, hs_=hs_, sub=sub: e.matmul(pKV[:, h, :], lhsT=kd[:, h, :], rhs=vt[sub][:, hs_], start=True, stop=True, skip_group_check=True),
                         reads=[kd.b, vt[sub].b], writes=[pKV.b])
                yield
                for h in range(4):
                    p.op("dve", lambda e, h=h: e.scalar_tensor_tensor(out=st[:, h, :], in0=st[:, h, :], scalar=cd[h], in1=pKV[:, h, :], op0=ALU.mult, op1=ALU.add),
                         reads=[st.b, pKV.b], writes=[st.b])
                yield
                p.op("act", lambda e: e.copy(out=stbf[:], in_=st[:]), reads=[st.b], writes=[stbf.b])
                yield

            def E2_gen(sub):
                sl = slice(sub * 128, (sub + 1) * 128)
                pO = pOs[sub % 2]
                for h in range(4):
                    p.op("dve", lambda e, h=h: e.bn_stats(out=bst[:, h, :], in_=pO[:, h, :]), reads=[pO.b], writes=[bst.b])
                yield
                for h in range(4):
                    p.op("dve", lambda e, h=h: e.bn_aggr(out=mv[:, h, :], in_=bst[:, h, :]), reads=[bst.b], writes=[mv.b])
                yield
                p.op("dve", lambda e: e.tensor_scalar(out=rstd[:], in0=mv[:, :, 1], scalar1=EPS, scalar2=None, op0=ALU.add), reads=[mv.b], writes=[rstd.b])
                yield
                p.op("act", lambda e: e.activation(out=rstd[:], in_=rstd[:], func=AF.Sqrt), reads=[rstd.b], writes=[rstd.b])
                yield
                p.op("dve", lambda e: e.reciprocal(out=rstd[:], in_=rstd[:]), reads=[rstd.b], writes=[rstd.b])
                yield
                p.op("dve", lambda e: e.scalar_tensor_tensor(out=nb[:], in0=mv[:, :, 0], scalar=-1.0, in1=rstd[:], op0=ALU.mult, op1=ALU.mult), reads=[mv.b, rstd.b], writes=[nb.b])
                yield
                for h in range(4):
                    p.op("act", lambda e, h=h: e.activation(out=on[:, h * 128:(h + 1) * 128], in_=pO[:, h, :], func=AF.Identity, scale=rstd[:, h:h + 1], bias=nb[:, h:h + 1]),
                         reads=[pO.b, rstd.b, nb.b], writes=[on.b])
                yield
                p.op("dve", lambda e, sub=sub: e.tensor_tensor(out=yr[:], in0=on[:], in1=Gt[sub][:], op=ALU.mult), reads=[on.b, Gt[sub].b], writes=[yr.b])
                yield
                for c in range(4):
                    p.op("pe", lambda e, c=c: e.transpose(out=pT[:, c, :], in_=yr[:, c * 128:(c + 1) * 128], identity=ident[:]), reads=[yr.b, ident.b], writes=[pT.b])
                p.op("act", lambda e, sl=sl, par=par: e.copy(out=yT[par][:, :, sl], in_=pT[:, 0:4, :]), reads=[pT.b], writes=[yT[par].b])
                yield
                if sub == 3:
                    p.dma("sp", lambda e, par=par: e.dma_start(out=ymixT.ap()[0:512, t0:t0 + 512].rearrange("(c p) t -> p c t", p=128), in_=yT[par][:]),
                          reads=[yT[par].b], writes=[ymixT.b(("r", it))])

            run_rr([D_gen(0)])
            run_rr([E1_gen(0), D_gen(1), C_gen()])
            run_rr([E2_gen(0), E1_gen(1), D_gen(2)] + ([A_gen(it + 1)] if it + 1 < TT // 512 else []))
            run_rr([E2_gen(1), E1_gen(2), D_gen(3)])
            run_rr([E2_gen(2), E1_gen(3)])
            run_rr([E2_gen(3)])

        run_rr([A_gen(0)])
        for it in range(TT // 512):
            do_tile(it)


def prep_shared(inp):
    f = lambda a: np.ascontiguousarray(np.asarray(a, dtype=np.float32))
    d = {}
    d["norms"] = f(np.concatenate([inp["norm_mix"][0:1], inp["norm_ffn"][0:1], inp["norm_mix"][1:2], inp["norm_ffn"][1:2], inp["norm_final"][None, :]], 0))
    w = np.asarray(inp["ev_w_in"][0])
    swap = np.concatenate([np.arange(h * 128 + 64, h * 128 + 128).tolist() + np.arange(h * 128, h * 128 + 64).tolist() for h in range(4)]).astype(np.int64)
    d["ev_w_in"] = f(np.concatenate([w, w[:, 0:512][:, swap], w[:, 512:1024][:, swap]], 1))
    d["ev_gn"] = f(inp["ev_ret_gn"][0:1])
    d["ev_w_out"] = f(inp["ev_w_out"][0])
    d["ev_gate"] = f(inp["ev_ffn_gate"][0])
    d["ev_up"] = f(inp["ev_ffn_up"][0])
    d["ev_down"] = f(inp["ev_ffn_down"][0])
    d["od_w_in"] = f(inp["od_w_in"][0])
    sm = np.zeros((512, 12), np.float32)
    sm[:, 0:3] = np.asarray(inp["od_conv_w"][0]).T
    sm[:, 3:7] = np.asarray(inp["od_lru_conv_w"][0]).T
    sm[:, 7] = np.asarray(inp["od_lru_conv_b"][0])
    sm[:, 8] = np.asarray(inp["od_lru_ba"][0])
    sm[:, 9] = np.asarray(inp["od_lru_bx"][0])
    sm[:, 10] = np.asarray(inp["od_lru_lambda"][0])
    d["od_small"] = f(sm.reshape(4, 128, 12).transpose(1, 0, 2))
    for nm, key in (("od_wa", "od_lru_wa"), ("od_wx", "od_lru_wx")):
        wsrc = np.asarray(inp[key][0])
        bd = np.zeros((128, 4, 128), np.float32)
        for hh in range(8):
            c, o = hh // 2, (hh % 2) * 64
            bd[o:o + 64, c, o:o + 64] = wsrc[hh]
        d[nm] = bd
    d["od_w_out"] = f(inp["od_w_out"][0])
    d["od_rw"] = f(inp["od_router_w"][0])
    d["od_rb"] = f(inp["od_router_b"][0:1])
    d["od_eg"] = f(inp["od_exp_gate"][0])
    d["od_eu"] = f(inp["od_exp_up"][0])
    d["od_ed"] = f(inp["od_exp_down"][0])
    return d


_CACHE = {}


def kernel(**inputs):
    if "nc" not in _CACHE:
        _CACHE["nc"] = build()
    nc, cs = _CACHE["nc"]
    shared = prep_shared(inputs)
    for k, v in cs.items():
        shared["c_" + k] = v
    x = np.asarray(inputs["x"], dtype=np.float32)
    in_maps = []
    for c in range(8):
        m = dict(shared)
        m["x"] = np.ascontiguousarray(x[c])
        in_maps.append(m)
    res = run_bass_kernel_spmd(nc, in_maps, core_ids=list(range(8)))
    return np.stack([np.asarray(r["out"]) for r in res.results], 0).astype(np.float32)


def phase2(p, IN, env, sb_qT, sb_kT, sb_v, ymixT, wcast=None, xg=None):
    with p.phase():
        C = env["CL"]()
        sbmask, negtri, negcomp = C["sbmask"], C["negtri"], C["negcomp"]
        vall = T(p, "vall", [128, NSUB, 512], BF16)
        p.dma("sp", lambda e: e.dma_start(out=vall[:], in_=sb_v.ap().rearrange("(n p) c -> p n c", p=128)), writes=[vall.b])
        qz = [[T(p, "qz%d_%d" % (i, hh), [128, TT], BF16) for hh in range(2)] for i in range(2)]
        for i in range(2):
            for hh in range(2):
                o = (1 - hh) * 64
                p.op("pool", lambda e, i=i, hh=hh, o=o: e.memset(qz[i][hh][o:o + 64, :], 0.0), writes=[qz[i][hh].b])
        kp = [T(p, "kp%d" % i, [128, TT], BF16) for i in range(2)]
        pZ = [T(p, "pZ%d" % i, [128, 512], F32, psum=True) for i in range(2)]
        pB = [T(p, "pB%d" % i, [128, 512], F32, psum=True) for i in range(2)]
        pOo = [T(p, "pOo%d" % i, [128, 512], F32, psum=True) for i in range(4)]
        NE, NS, NX, NA = 6, 4, 3, 3
        Et = [T(p, "Et%d" % i, [128, 512], F32) for i in range(NE)]
        SPt = [T(p, "SPt%d" % i, [128, 512], BF16) for i in range(NS)]
        Xt = [T(p, "Xt%d" % i, [128, 512], F32) for i in range(NX)]
        At = [T(p, "At%d" % i, [128, 512], BF16) for i in range(NA)]
        ost = [T(p, "ost%d" % i, [128, 512], BF16) for i in range(4)]
        tiles = []
        for pair in range(4):
            for Q in range(8):
                kmax = 4 * Q + 3
                kmin = max(0, 4 * Q - SB_WB)
                for kb in range(kmax, kmin - 1, -1):
                    for hh in range(2):
                        tiles.append(dict(pair=pair, head=pair * 2 + hh, pb=hh * 64, Q=Q, kb=kb, first=(kb == kmax), last=(kb == kmin), r=kb - 4 * Q,
                                          ob=(2 * (pair * 8 + Q) + hh) % 4, st=hh, newpair=(hh == 0 and Q == 0 and kb == kmax)))

        def S0(i, t):
            kt = kp[t["pair"] % 2]
            qt = qz[t["pair"] % 2][t["st"]]
            pair, pb, kb, Q = t["pair"], t["pb"], t["kb"], t["Q"]
            w0 = max(t["r"], 0) * 128
            if t["newpair"]:
                for hh in range(2):
                    qh = qz[pair % 2][hh]
                    p.dma("sp", lambda e, hh=hh, qh=qh: e.dma_start(out=qh[hh * 64:(hh + 1) * 64, :], in_=sb_qT.ap()[pair * 128 + hh * 64:pair * 128 + (hh + 1) * 64, :]), writes=[qh.b])
                p.dma("sp", lambda e: e.dma_start(out=kt[:], in_=sb_kT.ap()[pair * 128:(pair + 1) * 128, :]), writes=[kt.b])
            z, e_ = pZ[i % 2], Et[i % NE]
            p.op("pe", lambda e: e.matmul(z[:, w0:512], lhsT=kt[:, kb * 128:(kb + 1) * 128], rhs=qt[:, Q * 512 + w0:(Q + 1) * 512], start=True, stop=True),
                 reads=[kt.b, qt.b], writes=[z.b])
            p.op("act", lambda e: e.activation(out=e_[:, w0:512], in_=z[:, w0:512], func=AF.Exp, scale=0.125), reads=[z.b], writes=[e_.b])

        def S1(i, t):
            e_, sp_ = Et[i % NE], SPt[i % NS]
            r = t["r"]
            w0 = max(r, 0) * 128
            p.op("act", lambda e: e.activation(out=sp_[:, w0:512], in_=e_[:, w0:512], func=AF.Ln, bias=1.0), reads=[e_.b], writes=[sp_.b])
            if r >= 0:
                p.op("pool", lambda e: e.tensor_tensor(out=sp_[:, w0:w0 + 128], in0=sp_[:, w0:w0 + 128], in1=sbmask[:, r, w0:w0 + 128], op=ALU.mult), reads=[sp_.b, sbmask.b], writes=[sp_.b])

        def S2a(i, t):
            B, sp_ = pB[t["st"]], SPt[i % NS]
            w0 = max(t["r"], 0) * 128
            p.op("pe", lambda e: e.matmul(B[:, w0:512], lhsT=negtri[:], rhs=sp_[:, w0:512], start=t["first"], stop=True, skip_group_check=True), reads=[negtri.b, sp_.b], writes=[B.b])

        def S2b(i, t):
            B, x = pB[t["st"]], Xt[i % NX]
            w0 = max(t["r"], 0) * 128
            p.op("act", lambda e: e.activation(out=x[:, w0:512], in_=B[:, w0:512], func=AF.Exp), reads=[B.b], writes=[x.b])

        def S2c(i, t):
            B, sp_ = pB[t["st"]], SPt[i % NS]
            w0 = max(t["r"], 0) * 128
            if not t["last"]:
                p.op("pe", lambda e: e.matmul(B[:, w0:512], lhsT=negcomp[:], rhs=sp_[:, w0:512], start=False, stop=True, skip_group_check=True), reads=[negcomp.b, sp_.b], writes=[B.b])

        def S3(i, t):
            e_, x, a = Et[i % NE], Xt[i % NX], At[i % NA]
            r = t["r"]
            w0 = max(r, 0) * 128
            p.op("dve", lambda e: e.tensor_tensor(out=a[:, w0:512], in0=e_[:, w0:512], in1=x[:, w0:512], op=ALU.mult), reads=[e_.b, x.b], writes=[a.b])
            if r >= 0:
                p.op("pool", lambda e: e.tensor_tensor(out=a[:, w0:w0 + 128], in0=a[:, w0:w0 + 128], in1=sbmask[:, r, w0:w0 + 128], op=ALU.mult), reads=[a.b, sbmask.b], writes=[a.b])

        def S4(i, t):
            a = At[i % NA]
            po, osb = pOo[t["ob"]], ost[t["ob"]]
            kb, head, Q, pair, pb = t["kb"], t["head"], t["Q"], t["pair"], t["pb"]
            w0 = max(t["r"], 0) * 128
            p.op("pe", lambda e: e.matmul(po[:, w0:512], lhsT=vall[:, kb, pair * 128:(pair + 1) * 128], rhs=a[:, w0:512], start=t["first"], stop=t["last"], skip_group_check=True),
                 reads=[vall.b, a.b], writes=[po.b])
            if t["last"]:
                p.op("dve", lambda e: e.tensor_copy(out=osb[pb:pb + 64, :], in_=po[pb:pb + 64, :]), reads=[po.b], writes=[osb.b])
                p.dma("sp", lambda e: e.dma_start(out=ymixT.ap()[512 + head * 64:512 + (head + 1) * 64, Q * 512:(Q + 1) * 512], in_=osb[pb:pb + 64, :]),
                      reads=[osb.b], writes=[ymixT.b(("s", head, Q))])

        fill_b = Buf()

        def SF(i, t):
            pass

        NTL = len(tiles)
        order = ((3, S2a), (3, S2b), (0, S0), (3, S2c), (1, S1), (4, S3), (5, S4), (0, SF))
        if xg is not None:
            zt = T(p, "zt", [128, CAP // 128, DM], BF16)
            p.op("pool", lambda e: e.memset(zt[:], 0.0), writes=[zt.b])
        for s_ in range(NTL + 5):
            if xg is not None and 500 <= s_ < 500 + NEXP * 20 and (s_ - 500) % 20 == 0:
                ex_ = (s_ - 500) // 20
                p.dma("sp", lambda e, ex_=ex_: e.dma_start(out=xg.ap()[ex_ * CAP:(ex_ + 1) * CAP, :].rearrange("(n p) d -> p n d", p=128), in_=zt[:]), reads=[zt.b], writes=[Buf()])
            if wcast is not None and s_ in (60, 200, 340):
                src_name, dst = wcast[(60, 200, 340).index(s_)]
                p.dma("pool", lambda e, src_name=src_name, dst=dst: e.dma_start(out=dst.ap(), in_=IN[src_name].ap()), writes=[Buf()])
            for d_, fn_ in order:
                if 0 <= s_ - d_ < NTL:
                    fn_(s_ - d_, tiles[s_ - d_])


def load_norm_T(p, W, src, r0, g, xs, hs, pT, ident, dstT, c0, dbuf, hdt_fp32=False, part=0):
    if part in (0, 1):
        p.dma("sp", lambda e: e.dma_start(out=xs[:], in_=src.ap()[r0:r0 + 128, :]), reads=[src.b(r0 // 128)], writes=[xs.b])
        rmsnorm_tile(p, W, xs, g, hs)
    if part == 1:
        return
    for k in range(8):
        p.op("pe", lambda e, k=k: e.transpose(out=pT[:, k, :], in_=hs[:, k * 128:(k + 1) * 128], identity=ident[:]), reads=[hs.b, ident.b], writes=[pT.b])
    p.op("act", lambda e: e.copy(out=dstT[:, :, c0:c0 + 128], in_=pT[:]), reads=[pT.b], writes=[dbuf])


def phase3(p, IN, env, ymixT, x_src, x_dst, wname, ykeys, xg=None):
    with p.phase():
        wo = T(p, "wo", [128, 8, DM], BF16)
        p.dma("pool", lambda e: e.dma_start(out=wo[:], in_=IN[wname].ap().rearrange("(k p) n -> p k n", p=128)), writes=[wo.b])
        ym = [T(p, "ym%d" % i, [128, 8, 512], BF16) for i in range(2)]
        xt = [T(p, "xt%d" % i, [128, DM], F32) for i in range(4)]
        xo = [T(p, "xo%d" % i, [128, DM], F32) for i in range(2)]
        pY = [T(p, "pY%d" % i, [128, 512], F32, psum=True) for i in range(4)]

        def ymload(it):
            t0 = it * 512
            ymt = ym[it % 2]
            p.dma("sp", lambda e: e.dma_start(out=ymt[:], in_=ymixT.ap()[:, t0:t0 + 512].rearrange("(k p) t -> p k t", p=128)),
                  reads=[ymixT.b(k) for k in ykeys(it)], writes=[ymt.b])

        def xload(n):
            xs = xt[n % 4]
            r0 = n * 128
            p.dma("sp", lambda e: e.dma_start(out=xs[:], in_=x_src.ap()[r0:r0 + 128, :]), reads=[x_src.b(n)], writes=[xs.b])

        ymload(0)
        for n in range(3):
            xload(n)

        def do_tile(it):
            t0 = it * 512
            ymt = ym[it % 2]
            if it + 1 < TT // 512:
                ymload(it + 1)
            for sub in range(4):
                n = it * 4 + sub
                xs, xos = xt[n % 4], xo[n % 2]
                r0 = t0 + sub * 128
                if n + 3 < NSUB:
                    xload(n + 3)
                for half in range(2):
                    py = pY[(n * 2 + half) % 4]
                    for k in range(8):
                        p.op("pe", lambda e, k=k, py=py, sub=sub, half=half: e.matmul(py[:], lhsT=ymt[:, k, sub * 128:(sub + 1) * 128], rhs=wo[:, k, half * 512:(half + 1) * 512], start=(k == 0), stop=(k == 7)),
                             reads=[ymt.b, wo.b], writes=[py.b])
                    p.op("dve", lambda e, py=py, xs=xs, xos=xos, half=half: e.tensor_tensor(out=xos[:, half * 512:(half + 1) * 512], in0=py[:], in1=xs[:, half * 512:(half + 1) * 512], op=ALU.add),
                         reads=[py.b, xs.b], writes=[xos.b])
                p.dma("sp", lambda e, xos=xos, r0=r0: e.dma_start(out=x_dst.ap()[r0:r0 + 128, :], in_=xos[:]), reads=[xos.b], writes=[x_dst.b(r0 // 128)])

        for it in range(TT // 512):
            do_tile(it)


def ffn_alloc(p, ntok_max):
    FB = {"n": 0, "ln": 0, "j": 0, "jj": 0}
    FB["wg"] = [T(p, "wg%d" % i, [128, 8, 512], BF16) for i in range(2)]
    FB["wu"] = [T(p, "wu%d" % i, [128, 8, 512], BF16) for i in range(2)]
    FB["wd"] = [T(p, "wd%d" % i, [128, 4, DM], BF16) for i in range(2)]
    FB["act"] = [T(p, "actT%d" % i, [128, 4, ntok_max], BF16) for i in range(2)]
    FB["sg"] = [T(p, "sg%d" % i, [128, 512], F32) for i in range(2)]
    FB["pG"] = [T(p, "pG%d" % i, [128, 512], F32, psum=True) for i in range(2)]
    FB["pU"] = [T(p, "pU%d" % i, [128, 512], F32, psum=True) for i in range(2)]
    FB["pY"] = [T(p, "pYf%d" % i, [128, 512], F32, psum=True) for i in range(2)]
    return FB


def ffn_load(p, FB, wg_ap, wu_ap, wd_ap, fs, fsz, wq="pool"):
    i = FB["ln"] % 2
    FB["ln"] += 1
    wg, wu, wd = FB["wg"][i], FB["wu"][i], FB["wd"][i]
    nfc = fsz // 128
    p.dma(wq, lambda e: e.dma_start(out=wg[:, :, 0:fsz], in_=wg_ap[:, fs:fs + fsz].rearrange("(k p) n -> p k n", p=128)), writes=[wg.b])
    p.dma(wq, lambda e: e.dma_start(out=wu[:, :, 0:fsz], in_=wu_ap[:, fs:fs + fsz].rearrange("(k p) n -> p k n", p=128)), writes=[wu.b])
    p.dma(wq, lambda e: e.dma_start(out=wd[:, 0:nfc, :], in_=wd_ap[fs:fs + fsz, :].rearrange("(c p) n -> p c n", p=128)), writes=[wd.b])


def ffn_block(p, FB, xT, xbufs, ntok, wg_ap, wu_ap, wd_ap, F, yacc, ybufs, hooks=(), hooks_early=(), wq="pool", preloaded=False):
    groups = [(fs, min(512, F - fs)) for fs in range(0, F, 512)]
    hooks = list(hooks)
    hooks_early = list(hooks_early)
    late_ready = []
    per = -(-len(hooks) // max(1, len(groups) - 1)) if hooks else 0

    def do_group(gi, fs, fsz):
        if gi == 0 and not preloaded:
            ffn_load(p, FB, wg_ap, wu_ap, wd_ap, fs, fsz, wq)
        if gi + 1 < len(groups):
            ffn_load(p, FB, wg_ap, wu_ap, wd_ap, groups[gi + 1][0], groups[gi + 1][1], wq)
        i = FB["n"] % 2
        FB["n"] += 1
        wg, wu, wd, act = FB["wg"][i], FB["wu"][i], FB["wd"][i], FB["act"][i]
        nfc = fsz // 128
        for tt0 in range(0, ntok, 512):
            tn = min(512, ntok - tt0)
            for fc in range(nfc):
                j = FB["j"] % 2
                FB["j"] += 1
                pg, pu, sg = FB["pG"][j], FB["pU"][j], FB["sg"][j]
                for k in range(8):
                    p.op("pe", lambda e, k=k, pg=pg, fc=fc, tt0=tt0, tn=tn: e.matmul(pg[:, 0:tn], lhsT=wg[:, k, fc * 128:(fc + 1) * 128], rhs=xT[:, k, tt0:tt0 + tn], start=(k == 0), stop=(k == 7)),
                         reads=[wg.b] + xbufs, writes=[pg.b])
                for k in range(8):
                    p.op("pe", lambda e, k=k, pu=pu, fc=fc, tt0=tt0, tn=tn: e.matmul(pu[:, 0:tn], lhsT=wu[:, k, fc * 128:(fc + 1) * 128], rhs=xT[:, k, tt0:tt0 + tn], start=(k == 0), stop=(k == 7)),
                         reads=[wu.b] + xbufs, writes=[pu.b])
                p.op("act", lambda e, pg=pg, sg=sg, tn=tn: e.activation(out=sg[:, 0:tn], in_=pg[:, 0:tn], func=AF.Silu), reads=[pg.b], writes=[sg.b])
                p.op("dve", lambda e, pu=pu, sg=sg, fc=fc, tt0=tt0, tn=tn: e.tensor_tensor(out=act[:, fc, tt0:tt0 + tn], in0=pu[:, 0:tn], in1=sg[:, 0:tn], op=ALU.mult),
                     reads=[pu.b, sg.b], writes=[act.b])
        if hooks_early or late_ready:
            for _ in range(len(late_ready)):
                late_ready.pop(0)()
            for _ in range(per):
                if hooks_early:
                    hooks_early.pop(0)()
                    late_ready.append(hooks.pop(0))
        for ts in range(ntok // 128):
            for half in range(2):
                jj = FB["jj"] % 2
                FB["jj"] += 1
                py = FB["pY"][jj]
                for fc in range(nfc):
                    p.op("pe", lambda e, fc=fc, py=py, ts=ts, half=half: e.matmul(py[:], lhsT=act[:, fc, ts * 128:(ts + 1) * 128], rhs=wd[:, fc, half * 512:(half + 1) * 512], start=(fc == 0), stop=(fc == nfc - 1)),
                         reads=[act.b, wd.b], writes=[py.b])
                if gi == 0:
                    p.op("act", lambda e, py=py, ts=ts, half=half: e.copy(out=yacc[:, ts, half * 512:(half + 1) * 512], in_=py[:]), reads=[py.b], writes=[ybufs[ts]])
                else:
                    p.op("dve", lambda e, py=py, ts=ts, half=half: e.tensor_tensor(out=yacc[:, ts, half * 512:(half + 1) * 512], in0=py[:], in1=yacc[:, ts, half * 512:(half + 1) * 512], op=ALU.add),
                         reads=[py.b, ybufs[ts]], writes=[ybufs[ts]])

    for gi, (fs, fsz) in enumerate(groups):
        do_group(gi, fs, fsz)
    for h_ in late_ready + hooks:
        h_()


def phase4(p, IN, env, x_src, x_dst, pre_hook=None, wbf=None):
    NTK = 1024
    with p.phase():
        C, G, W = env["CL"](), env["GL"](), env["newW"]()
        ident = C["ident"]
        FB = ffn_alloc(p, NTK)
        pT = T(p, "pT", [128, 8, 128], BF16, psum=True)
        xTs = [T(p, "xT%d" % i, [128, 8, NTK], BF16) for i in range(2)]
        xbs = [[Buf() for _ in range(NTK // 128)] for _ in range(2)]
        yacc = T(p, "yacc", [128, NTK // 128, DM], F32)
        yb = [Buf() for _ in range(NTK // 128)]
        xt = [T(p, "xt%d" % i, [128, DM], F32) for i in range(2)]
        xt2 = [T(p, "xtb%d" % i, [128, DM], F32) for i in range(2)]
        ht = [T(p, "ht%d" % i, [128, DM], BF16) for i in range(2)]

        def prep(s, sub, part=0):
            load_norm_T(p, W, x_src, s * NTK + sub * 128, G[1], xt2[sub % 2], ht[sub % 2], pT, ident, xTs[s % 2], sub * 128, xbs[s % 2][sub], part=part)

        for sub in range(NTK // 128):
            prep(0, sub)
        if pre_hook is not None:
            pre_hook()

        def do_super(s):
            nxt = s + 1 < TT // NTK
            hooks_e = [(lambda sub=sub: prep(s + 1, sub, 1)) for sub in range(NTK // 128)] if nxt else []
            hooks_l = [(lambda sub=sub: prep(s + 1, sub, 2)) for sub in range(NTK // 128)] if nxt else []
            waps = (IN["ev_gate"].ap(), IN["ev_up"].ap(), IN["ev_down"].ap()) if wbf is None else (wbf[0].ap(), wbf[1].ap(), wbf[2].ap())
            ffn_block(p, FB, xTs[s % 2], xbs[s % 2], NTK, waps[0], waps[1], waps[2], DFF, yacc, yb, hooks=hooks_l, hooks_early=hooks_e, preloaded=(s > 0))
            if nxt:
                ffn_load(p, FB, waps[0], waps[1], waps[2], 0, 512)
            xq = xt + xt2
            nsub = NTK // 128

            def rload(sub):
                r0 = s * NTK + sub * 128
                xs = xq[sub % 4]
                p.dma("sp", lambda e: e.dma_start(out=xs[:], in_=x_src.ap()[r0:r0 + 128, :]), reads=[x_src.b(r0 // 128)], writes=[xs.b])

            for sub in range(3):
                rload(sub)
            for sub in range(nsub):
                r0 = s * NTK + sub * 128
                xs = xq[sub % 4]
                p.op("pool", lambda e, xs=xs, sub=sub: e.tensor_tensor(out=xs[:], in0=xs[:], in1=yacc[:, sub, :], op=ALU.add), reads=[xs.b, yb[sub]], writes=[xs.b])
                if sub + 3 < nsub:
                    rload(sub + 3)
                p.dma("sp", lambda e, xs=xs, r0=r0: e.dma_start(out=x_dst.ap()[r0:r0 + 128, :], in_=xs[:]), reads=[xs.b], writes=[x_dst.b(r0 // 128)])

        for s in range(TT // NTK):
            do_super(s)


def p5_weights_alloc(p):
    PW = {"win": T(p, "win1", [128, 8, 2560], BF16), "wbs": [Buf() for _ in range(5)],
          "wa": T(p, "wa", [128, 4, 128], BF16), "wx": T(p, "wx", [128, 4, 128], BF16)}
    return PW


def p5_weights_load(p, IN, PW):
    win, wbs, wa, wx = PW["win"], PW["wbs"], PW["wa"], PW["wx"]
    for cg in (1, 2, 0, 3, 4):
        p.dma("pool", lambda e, cg=cg: e.dma_start(out=win[:, :, cg * 512:(cg + 1) * 512],
              in_=IN["od_w_in"].ap()[:, cg * 512:(cg + 1) * 512].rearrange("(k p) n -> p k n", p=128)), writes=[wbs[cg]])
    p.dma("pool", lambda e: e.dma_start(out=wa[:], in_=IN["od_wa"].ap()), writes=[wa.b])
    p.dma("pool", lambda e: e.dma_start(out=wx[:], in_=IN["od_wx"].ap()), writes=[wx.b])


def phase5(p, IN, env, x_src, x_dst, PW):
    with p.phase():
        C, G, W = env["CL"](), env["GL"](), env["newW"]()
        ident = C["ident"]
        win, wbs, wa, wx = PW["win"], PW["wbs"], PW["wa"], PW["wx"]
        wo = T(p, "wo1", [128, 8, DM], BF16)
        p.dma("pool", lambda e: e.dma_start(out=wo[:], in_=IN["od_w_out"].ap().rearrange("(k p) n -> p k n", p=128)), writes=[wo.b])
        sm = T(p, "sm", [128, 4, 12], F32)
        p.dma("sp", lambda e: e.dma_start(out=sm[:], in_=IN["od_small"].ap()), writes=[sm.b])
        asc = T(p, "asc", [128, 4], F32)
        p.op("act", lambda e: e.activation(out=asc[:], in_=sm[:, :, 10], func=AF.Exp, scale=-1.0), reads=[sm.b], writes=[asc.b])
        p.op("act", lambda e: e.activation(out=asc[:], in_=asc[:], func=AF.Ln, bias=1.0), reads=[asc.b], writes=[asc.b])
        p.op("dve", lambda e: e.tensor_scalar(out=asc[:], in0=asc[:], scalar1=-8.0, scalar2=None, op0=ALU.mult), reads=[asc.b], writes=[asc.b])
        xt = [T(p, "xt%d" % i, [128, DM], F32) for i in range(2)]
        xo = [T(p, "xo%d" % i, [128, DM], F32) for i in range(2)]
        xt2 = [T(p, "xtr%d" % i, [128, DM], F32) for i in range(2)]
        ht = [T(p, "ht%d" % i, [128, DM], BF16) for i in range(2)]
        hT = [T(p, "hT%d" % i, [128, 8, 512], BF16) for i in range(2)]
        hTb = [[Buf() for _ in range(4)] for _ in range(2)]
        ymT = [T(p, "ymT%d" % i, [128, 8, 512], BF16) for i in range(2)]
        ymb = [[Buf() for _ in range(8)] for _ in range(2)]
        pT = T(p, "pT", [128, 8, 128], BF16, psum=True)
        pF = [T(p, "pF%d" % i, [128, 512], F32, psum=True) for i in range(5)]
        pY = [T(p, "pY%d" % i, [128, 512], F32, psum=True) for i in range(2)]
        vbuf = [T(p, "vbuf%d" % c, [128, 514], F32) for c in range(4)]
        xbuf = [T(p, "xbuf%d" % c, [128, 515], F32) for c in range(4)]
        hprev = T(p, "hprev", [128, 4], F32)
        ctmp = [{k: T(p, "ct%d_%s" % (i, k), [128, 512], F32) for k in ("gc", "o")} for i in range(4)]
        ltmp = [{k: T(p, "lt%d_%s" % (i, k), [128, 512], F32) for k in ("xc", "r", "ig", "a", "a2", "b", "h", "xg", "x2", "th", "gl")} for i in range(2)]
        xcbs = [T(p, "xcb%d" % i, [128, 512], BF16) for i in range(2)]
        for c in range(4):
            p.op("pool", lambda e, c=c: e.memset(vbuf[c][:], 0.0), writes=[vbuf[c].b])
            p.op("pool", lambda e, c=c: e.memset(xbuf[c][:], 0.0), writes=[xbuf[c].b])
        p.op("pool", lambda e: e.memset(hprev[:], 0.0), writes=[hprev.b])
        cnt = [0]
        NT5 = TT // 512

        def run_rr(gens):
            gens = list(gens)
            while gens:
                for g_ in list(gens):
                    try:
                        next(g_)
                    except StopIteration:
                        gens.remove(g_)

        def A_gen(it):
            par = it % 2
            for part, sub in ((1, 0), (1, 1), (2, 0), (1, 2), (2, 1), (1, 3), (2, 2), (2, 3)):
                load_norm_T(p, W, x_src, it * 512 + sub * 128, G[2], xt[sub % 2], ht[sub % 2], pT, ident, hT[par], sub * 128, hTb[par][sub], part=part)
                yield

        def proj(it, c0):
            hTt, hbs = hT[it % 2], hTb[it % 2]
            ps = pF[cnt[0] % 5]
            cnt[0] += 1
            for k in range(8):
                p.op("pe", lambda e, k=k: e.matmul(ps[:], lhsT=win[:, k, c0:c0 + 128], rhs=hTt[:, k, :], start=(k == 0), stop=(k == 7)),
                     reads=[wbs[c0 // 512]] + hbs, writes=[ps.b])
            return ps

        def conv_gen(it, c):
            yt, ybs = ymT[it % 2], ymb[it % 2]
            vb = vbuf[c]
            gc, o = ctmp[c]["gc"], ctmp[c]["o"]
            pgc = proj(it, 512 + c * 128)
            p.op("act", lambda e: e.copy(out=gc[:], in_=pgc[:]), reads=[pgc.b], writes=[gc.b])
            yield
            pu = proj(it, 1024 + c * 128)
            p.op("dve", lambda e: e.tensor_tensor(out=vb[:, 2:514], in0=pu[:], in1=gc[:], op=ALU.mult), reads=[pu.b, gc.b], writes=[vb.b])
            yield
            p.op("dve", lambda e: e.tensor_scalar(out=o[:], in0=vb[:, 2:514], scalar1=sm[:, c, 2:3], scalar2=None, op0=ALU.mult), reads=[vb.b, sm.b], writes=[o.b])
            yield
            p.op("dve", lambda e: e.scalar_tensor_tensor(out=o[:], in0=vb[:, 1:513], scalar=sm[:, c, 1:2], in1=o[:], op0=ALU.mult, op1=ALU.add), reads=[vb.b, sm.b, o.b], writes=[o.b])
            yield
            p.op("dve", lambda e: e.scalar_tensor_tensor(out=o[:], in0=vb[:, 0:512], scalar=sm[:, c, 0:1], in1=o[:], op0=ALU.mult, op1=ALU.add), reads=[vb.b, sm.b, o.b], writes=[o.b])
            p.op("pool", lambda e: e.tensor_copy(out=vb[:, 0:2], in_=vb[:, 512:514]), reads=[vb.b], writes=[vb.b])
            yield
            pgb = proj(it, c * 128)
            p.op("dve", lambda e: e.tensor_tensor(out=yt[:, c, :], in0=pgb[:], in1=o[:], op=ALU.mult), reads=[pgb.b, o.b], writes=[ybs[c]])
            yield

        def lru_gen(it, c):
            yt, ybs = ymT[it % 2], ymb[it % 2]
            xb = xbuf[c]
            tm = ltmp[c % 2]
            xcb = xcbs[c % 2]
            xc, r, ig, a, a2, b, h, xg, x2, th, gl = (tm[k] for k in ("xc", "r", "ig", "a", "a2", "b", "h", "xg", "x2", "th", "gl"))
            pxr = proj(it, 1536 + c * 128)
            p.op("act", lambda e: e.copy(out=xb[:, 3:515], in_=pxr[:]), reads=[pxr.b], writes=[xb.b])
            yield
            p.op("dve", lambda e: e.tensor_scalar(out=xc[:], in0=xb[:, 3:515], scalar1=sm[:, c, 6:7], scalar2=sm[:, c, 7:8], op0=ALU.mult, op1=ALU.add), reads=[xb.b, sm.b], writes=[xc.b])
            yield
            for j, off in ((5, 2), (4, 1), (3, 0)):
                p.op("dve", lambda e, j=j, off=off: e.scalar_tensor_tensor(out=xc[:], in0=xb[:, off:off + 512], scalar=sm[:, c, j:j + 1], in1=xc[:], op0=ALU.mult, op1=ALU.add),
                     reads=[xb.b, sm.b, xc.b], writes=[xc.b])
                yield
            p.op("pool", lambda e: e.tensor_copy(out=xb[:, 0:3], in_=xb[:, 512:515]), reads=[xb.b], writes=[xb.b])
            p.op("act", lambda e: e.copy(out=xcb[:], in_=xc[:]), reads=[xc.b], writes=[xcb.b])
            yield
            pr = pF[cnt[0] % 5]
            cnt[0] += 1
            p.op("pe", lambda e: e.matmul(pr[:], lhsT=wa[:, c, :], rhs=xcb[:], start=True, stop=True), reads=[wa.b, xcb.b], writes=[pr.b])
            pi = pF[cnt[0] % 5]
            cnt[0] += 1
            p.op("pe", lambda e: e.matmul(pi[:], lhsT=wx[:, c, :], rhs=xcb[:], start=True, stop=True), reads=[wx.b, xcb.b], writes=[pi.b])
            yield
            p.op("act", lambda e: e.activation(out=r[:], in_=pr[:], func=AF.Sigmoid, bias=sm[:, c, 8:9]), reads=[pr.b, sm.b], writes=[r.b])
            p.op("act", lambda e: e.activation(out=ig[:], in_=pi[:], func=AF.Sigmoid, bias=sm[:, c, 9:10]), reads=[pi.b, sm.b], writes=[ig.b])
            yield
            p.op("act", lambda e: e.activation(out=a[:], in_=r[:], func=AF.Exp, scale=asc[:, c:c + 1]), reads=[r.b, asc.b], writes=[a.b])
            yield
            p.op("pool", lambda e: e.tensor_tensor(out=a2[:], in0=a[:], in1=a[:], op=ALU.mult), reads=[a.b], writes=[a2.b])
            yield
            p.op("act", lambda e: e.activation(out=a2[:], in_=a2[:], func=AF.Sqrt, scale=-1.0, bias=1.0), reads=[a2.b], writes=[a2.b])
            yield
            p.op("pool", lambda e: e.tensor_tensor(out=b[:], in0=a2[:], in1=ig[:], op=ALU.mult), reads=[a2.b, ig.b], writes=[b.b])
            yield
            p.op("pool", lambda e: e.tensor_tensor(out=b[:], in0=b[:], in1=xc[:], op=ALU.mult), reads=[b.b, xc.b], writes=[b.b])
            yield
            p.op("dve", lambda e: e.tensor_tensor_scan(out=h[:], data0=a[:], data1=b[:], initial=hprev[:, c:c + 1], op0=ALU.mult, op1=ALU.add),
                 reads=[a.b, b.b, hprev.b], writes=[h.b])
            p.op("pool", lambda e: e.tensor_copy(out=hprev[:, c:c + 1], in_=h[:, 511:512]), reads=[h.b], writes=[hprev.b])
            yield
            pxg = proj(it, 2048 + c * 128)
            p.op("act", lambda e: e.copy(out=xg[:], in_=pxg[:]), reads=[pxg.b], writes=[xg.b])
            yield
            p.op("pool", lambda e: e.tensor_tensor(out=x2[:], in0=xg[:], in1=xg[:], op=ALU.mult), reads=[xg.b], writes=[x2.b])
            yield
            p.op("dve", lambda e: e.tensor_scalar(out=x2[:], in0=x2[:], scalar1=0.044715, scalar2=1.0, op0=ALU.mult, op1=ALU.add), reads=[x2.b], writes=[x2.b])
            yield
            p.op("pool", lambda e: e.tensor_tensor(out=x2[:], in0=x2[:], in1=xg[:], op=ALU.mult), reads=[x2.b, xg.b], writes=[x2.b])
            yield
            p.op("act", lambda e: e.activation(out=th[:], in_=x2[:], func=AF.Tanh, scale=0.7978845608028654), reads=[x2.b], writes=[th.b])
            yield
            p.op("dve", lambda e: e.scalar_tensor_tensor(out=gl[:], in0=th[:], scalar=1.0, in1=xg[:], op0=ALU.add, op1=ALU.mult), reads=[th.b, xg.b], writes=[gl.b])
            yield
            p.op("dve", lambda e: e.scalar_tensor_tensor(out=yt[:, 4 + c, :], in0=h[:], scalar=0.5, in1=gl[:], op0=ALU.mult, op1=ALU.mult), reads=[h.b, gl.b], writes=[ybs[4 + c]])
            yield

        def out_gen(it):
            yt, ybs = ymT[it % 2], ymb[it % 2]
            t0 = it * 512
            for sub in range(4):
                n = it * 4 + sub
                xs, xos = xt2[n % 2], xo[n % 2]
                r0 = t0 + sub * 128
                p.dma("sp", lambda e, xs=xs, r0=r0: e.dma_start(out=xs[:], in_=x_src.ap()[r0:r0 + 128, :]), reads=[x_src.b(r0 // 128)], writes=[xs.b])
                for half in range(2):
                    py = pY[(n * 2 + half) % 2]
                    for k in range(8):
                        p.op("pe", lambda e, k=k, py=py, sub=sub, half=half: e.matmul(py[:], lhsT=yt[:, k, sub * 128:(sub + 1) * 128], rhs=wo[:, k, half * 512:(half + 1) * 512], start=(k == 0), stop=(k == 7)),
                             reads=[ybs[k], wo.b], writes=[py.b])
                    p.op("dve", lambda e, py=py, xs=xs, xos=xos, half=half: e.tensor_tensor(out=xos[:, half * 512:(half + 1) * 512], in0=py[:], in1=xs[:, half * 512:(half + 1) * 512], op=ALU.add),
                         reads=[py.b, xs.b], writes=[xos.b])
                    yield
                p.dma("sp", lambda e, xos=xos, r0=r0: e.dma_start(out=x_dst.ap()[r0:r0 + 128, :], in_=xos[:]), reads=[xos.b], writes=[x_dst.b(r0 // 128)])

        run_rr([A_gen(0)])
        for it in range(NT5):
            extra = []
            if it + 1 < NT5:
                extra.append(A_gen(it + 1))
            if it >= 1:
                extra.append(out_gen(it - 1))
            run_rr([conv_gen(it, 0), conv_gen(it, 1), lru_gen(it, 0), lru_gen(it, 1)] + extra)
            run_rr([conv_gen(it, 2), conv_gen(it, 3), lru_gen(it, 2), lru_gen(it, 3)])
        run_rr([out_gen(NT5 - 1)])


BIGIDX = 1.0e6


def phase6(p, IN, env, x_src, xg, R):
    with p.phase():
        C, G, W = env["CL"](), env["GL"](), env["newW"]()
        identf, triex, ones, eoff = C["identf"], C["triex"], C["ones"], C["eoff"]
        rt, idx12 = R["rt"], R["idx12"]
        wr = T(p, "wr", [128, 8, 8], F32)
        p.dma("sp", lambda e: e.dma_start(out=wr[:], in_=IN["od_rw"].ap().rearrange("(k p) n -> p k n", p=128)), writes=[wr.b])
        rbt = T(p, "rbt", [128, 8], F32)
        p.dma("sp", lambda e: e.dma_start(out=rbt[:], in_=IN["od_rb"].ap().partition_broadcast(128)), writes=[rbt.b])
        zb = []
        xt = [T(p, "xt%d" % i, [128, DM], F32) for i in range(2)]
        hf = [T(p, "hf%d" % i, [128, DM], F32) for i in range(2)]
        hb = [T(p, "hb%d" % i, [128, DM], BF16) for i in range(4)]
        pTf = [T(p, "pTf%d" % i, [128, 4, 128], F32, psum=True) for i in range(2)]
        hTfs = [T(p, "hTf%d" % i, [128, 8, 128], F32) for i in range(2)]
        pLs = [T(p, "pL%d" % i, [128, 8], F32, psum=True) for i in range(2)]
        pP = T(p, "pP", [128, 8], F32, psum=True)
        pC = T(p, "pC", [128, 8], F32, psum=True)
        cntb = T(p, "cntb", [128, 8], F32)
        p.op("pool", lambda e: e.memset(cntb[:], 0.0), writes=[cntb.b])
        s8 = {k: T(p, "s8_" + k, [128, 8], F32) for k in ("lg", "mx", "sel2", "pos", "v", "dst", "junk")}
        selb = T(p, "selb", [128, 8], BF16)
        s1 = {k: T(p, "s1_" + k, [128, 1], F32) for k in ("d", "ex", "i1", "i2")}
        dsti = [T(p, "dsti%d" % i, [128, 8], I32) for i in range(2)]

        regbox = {}

        def bcreg(e):
            if "r" not in regbox:
                regbox["r"] = e.to_reg(NEXP * CAP - 1)
            return regbox["r"]

        def front(n):
            xs, hfs, hbs = xt[n % 2], hf[n % 2], hb[n % 4]
            hTf, pL = hTfs[n % 2], pLs[n % 2]
            r0 = n * 128
            p.dma("sp", lambda e: e.dma_start(out=xs[:], in_=x_src.ap()[r0:r0 + 128, :]), reads=[x_src.b(n)], writes=[xs.b])
            rmsnorm_tile(p, W, xs, G[3], hfs)
            yield
            p.op("act", lambda e: e.copy(out=hbs[:], in_=hfs[:]), reads=[hfs.b], writes=[hbs.b])
            for k in range(8):
                pt = pTf[k // 4]
                p.op("pe", lambda e, k=k, pt=pt: e.transpose(out=pt[:, k % 4, :], in_=hfs[:, k * 128:(k + 1) * 128], identity=identf[:]), reads=[hfs.b, identf.b], writes=[pt.b])
            yield
            for j in range(2):
                p.op("act", lambda e, j=j: e.copy(out=hTf[:, j * 4:(j + 1) * 4, :], in_=pTf[j][:]), reads=[pTf[j].b], writes=[hTf.b])
            yield
            for k in range(8):
                p.op("pe", lambda e, k=k: e.matmul(pL[:], lhsT=hTf[:, k, :], rhs=wr[:, k, :], start=(k == 0), stop=(k == 7)), reads=[hTf.b, wr.b], writes=[pL.b])
            yield

        def back(n):
            hbs = hb[n % 4]
            pL = pLs[n % 2]
            lg, mx, sel2, pos, v, dst, junk = (s8[k] for k in ("lg", "mx", "sel2", "pos", "v", "dst", "junk"))
            d, ex, i1, i2 = (s1[k] for k in ("d", "ex", "i1", "i2"))
            ops = [
                lambda: p.op("dve", lambda e: e.tensor_tensor(out=lg[:], in0=pL[:], in1=rbt[:], op=ALU.add), reads=[pL.b, rbt.b], writes=[lg.b]),
                lambda: p.op("dve", lambda e: e.max(out=mx[:], in_=lg[:]), reads=[lg.b], writes=[mx.b]),
                lambda: p.op("dve", lambda e: e.tensor_scalar(out=rt[:, n, 0:8], in0=lg[:], scalar1=mx[:, 1:2], scalar2=None, op0=ALU.is_ge), reads=[lg.b, mx.b], writes=[rt.b]),
                lambda: p.op("dve", lambda e: e.tensor_scalar(out=rt[:, n, 8:16], in0=lg[:], scalar1=mx[:, 0:1], scalar2=None, op0=ALU.is_ge), reads=[lg.b, mx.b], writes=[rt.b]),
                lambda: p.op("dve", lambda e: e.tensor_tensor(out=sel2[:], in0=rt[:, n, 0:8], in1=rt[:, n, 8:16], op=ALU.subtract), reads=[rt.b], writes=[sel2.b]),
                lambda: p.op("dve", lambda e: e.tensor_tensor(out=rt[:, n, 16:17], in0=mx[:, 1:2], in1=mx[:, 0:1], op=ALU.subtract), reads=[mx.b], writes=[rt.b]),
                lambda: p.op("dve", lambda e: e.tensor_copy(out=selb[:], in_=rt[:, n, 0:8]), reads=[rt.b], writes=[selb.b]),
                lambda: (p.op("pe", lambda e: e.matmul(pP[:], lhsT=triex[:], rhs=selb[:], start=True, stop=True), reads=[triex.b, selb.b], writes=[pP.b]),
                         p.op("pe", lambda e: e.matmul(pC[:], lhsT=ones[:], rhs=selb[:], start=True, stop=True), reads=[ones.b, selb.b], writes=[pC.b])),
                lambda: (p.op("dve", lambda e: e.tensor_tensor(out=pos[:], in0=pP[:], in1=cntb[:], op=ALU.add), reads=[pP.b, cntb.b], writes=[pos.b]),
                         p.op("dve", lambda e: e.tensor_tensor(out=cntb[:], in0=pC[:], in1=cntb[:], op=ALU.add), reads=[pC.b, cntb.b], writes=[cntb.b])),
                lambda: p.op("dve", lambda e: e.tensor_scalar(out=v[:], in0=pos[:], scalar1=CAP - 0.5, scalar2=None, op0=ALU.is_lt), reads=[pos.b], writes=[v.b]),
                lambda: p.op("dve", lambda e: e.tensor_tensor(out=v[:], in0=v[:], in1=rt[:, n, 0:8], op=ALU.mult), reads=[v.b, rt.b], writes=[v.b]),
                lambda: p.op("dve", lambda e: e.tensor_tensor(out=dst[:], in0=pos[:], in1=eoff[:], op=ALU.add), reads=[pos.b, eoff.b], writes=[dst.b]),
                lambda: p.op("dve", lambda e: e.scalar_tensor_tensor(out=dst[:], in0=dst[:], scalar=-BIGIDX, in1=v[:], op0=ALU.add, op1=ALU.mult), reads=[dst.b, v.b], writes=[dst.b]),
                lambda: p.op("dve", lambda e: e.tensor_scalar(out=dst[:], in0=dst[:], scalar1=BIGIDX, scalar2=None, op0=ALU.add), reads=[dst.b], writes=[dst.b]),
                lambda: p.op("dve", lambda e: e.tensor_tensor(out=junk[:], in0=rt[:, n, 8:16], in1=dst[:], op=ALU.mult), reads=[rt.b, dst.b], writes=[junk.b]),
                lambda: p.op("dve", lambda e: e.tensor_reduce(out=i1[:], in_=junk[:], axis=AX.X, op=ALU.add), reads=[junk.b], writes=[i1.b]),
                lambda: p.op("dve", lambda e: e.tensor_copy(out=idx12[:, n, 0:1], in_=i1[:]), reads=[i1.b], writes=[idx12.b]),
                lambda: p.op("dve", lambda e: e.tensor_tensor(out=junk[:], in0=sel2[:], in1=dst[:], op=ALU.mult), reads=[sel2.b, dst.b], writes=[junk.b]),
                lambda: p.op("dve", lambda e: e.tensor_reduce(out=i2[:], in_=junk[:], axis=AX.X, op=ALU.add), reads=[junk.b], writes=[i2.b]),
                lambda: p.op("dve", lambda e: e.tensor_copy(out=idx12[:, n, 1:2], in_=i2[:]), reads=[i2.b], writes=[idx12.b]),
            ]
            for k, f_ in enumerate(ops):
                f_()
                if k % 4 == 3:
                    yield
            for j in range(2):
                p.dma("pool", lambda e, j=j: e.indirect_dma_start(out=xg.ap(), out_offset=bass.IndirectOffsetOnAxis(ap=idx12[:, n, j:j + 1], axis=0), in_=hbs[:], in_offset=None,
                                                                   bounds_check=bcreg(e), oob_is_err=False),
                      reads=[hbs.b, idx12.b] + zb, writes=[Buf()])
            yield

        def run_rr(gens):
            gens = list(gens)
            while gens:
                for g_ in list(gens):
                    try:
                        next(g_)
                    except StopIteration:
                        gens.remove(g_)

        run_rr([front(0)])
        for n in range(NSUB):
            run_rr([back(n)] + ([front(n + 1)] if n + 1 < NSUB else []))
        exa = T(p, "exa", [128, NSUB], F32)
        dna = T(p, "dna", [128, NSUB], F32)
        p.op("act", lambda e: e.activation(out=exa[:], in_=rt[:, :, 16], func=AF.Exp), reads=[rt.b], writes=[exa.b])
        p.op("dve", lambda e: e.tensor_scalar(out=dna[:], in0=exa[:], scalar1=1.0, scalar2=None, op0=ALU.add), reads=[exa.b], writes=[dna.b])
        p.op("dve", lambda e: e.reciprocal(out=rt[:, :, 16], in_=dna[:]), reads=[dna.b], writes=[rt.b])
        p.op("dve", lambda e: e.tensor_tensor(out=rt[:, :, 17], in0=exa[:], in1=rt[:, :, 16], op=ALU.mult), reads=[exa.b, rt.b], writes=[rt.b])

def phase7(p, IN, env, xg, yg):
    with p.phase():
        C = env["CL"]()
        ident = C["ident"]
        FB = ffn_alloc(p, CAP)
        NT_ = CAP // 128
        pT = T(p, "pT", [128, 8, 128], BF16, psum=True)
        xTs = [T(p, "xT%d" % i, [128, 8, CAP], BF16) for i in range(2)]
        xbs = [[Buf() for _ in range(NT_)] for _ in range(2)]
        yacc = T(p, "yacc", [128, NT_, DM], F32)
        yb = [Buf() for _ in range(NT_)]
        xr = [T(p, "xr%d" % i, [128, DM], BF16) for i in range(2)]

        def prep(ex_, n, part=0):
            r0 = ex_ * CAP + n * 128
            xs = xr[n % 2]
            xT = xTs[ex_ % 2]
            if part in (0, 1):
                p.dma("sp", lambda e: e.dma_start(out=xs[:], in_=xg.ap()[r0:r0 + 128, :]), writes=[xs.b])
            if part == 1:
                return
            for k in range(8):
                p.op("pe", lambda e, k=k: e.transpose(out=pT[:, k, :], in_=xs[:, k * 128:(k + 1) * 128], identity=ident[:]), reads=[xs.b, ident.b], writes=[pT.b])
            p.op("act", lambda e: e.copy(out=xT[:, :, n * 128:(n + 1) * 128], in_=pT[:]), reads=[pT.b], writes=[xbs[ex_ % 2][n]])

        for n in range(NT_):
            prep(0, n)

        def do_expert(ex_):
            nxt = ex_ + 1 < NEXP
            hooks_e = [(lambda n=n: prep(ex_ + 1, n, 1)) for n in range(NT_)] if nxt else []
            hooks_l = [(lambda n=n: prep(ex_ + 1, n, 2)) for n in range(NT_)] if nxt else []
            ffn_block(p, FB, xTs[ex_ % 2], xbs[ex_ % 2], CAP, IN["od_eg"].ap()[ex_], IN["od_eu"].ap()[ex_], IN["od_ed"].ap()[ex_], DEXP, yacc, yb, hooks=hooks_l, hooks_early=hooks_e,
                      preloaded=(ex_ > 0))
            if nxt:
                ffn_load(p, FB, IN["od_eg"].ap()[ex_ + 1], IN["od_eu"].ap()[ex_ + 1], IN["od_ed"].ap()[ex_ + 1], 0, 512)
            p.dma("sp", lambda e: e.dma_start(out=yg.ap()[ex_ * CAP:(ex_ + 1) * CAP, :].rearrange("(n p) d -> p n d", p=128), in_=yacc[:]), reads=yb, writes=[Buf()])

        for ex_ in range(NEXP):
            do_expert(ex_)


def phase8(p, IN, env, x_src, yg, out, R):
    with p.phase():
        G, W = env["GL"](), env["newW"]()
        rt, idx12 = R["rt"], R["idx12"]
        NB8 = 6
        xt = [T(p, "xt%d" % i, [128, DM], F32) for i in range(NB8)]
        g1 = [T(p, "g1_%d" % i, [128, DM], F32) for i in range(NB8)]
        g2 = [T(p, "g2_%d" % i, [128, DM], F32) for i in range(NB8)]
        ot = [T(p, "ot%d" % i, [128, DM], F32) for i in range(NB8)]

        regbox = {}

        def bcreg(e):
            if "r" not in regbox:
                regbox["r"] = e.to_reg(NEXP * CAP - 1)
            return regbox["r"]

        def do_tile(n):
            xs, a, b, o = xt[n % NB8], g1[n % NB8], g2[n % NB8], ot[n % NB8]
            r0 = n * 128
            Wn = Ws[n % 2]
            sq, ss, rs = Wn["sq"], Wn["ss"], Wn["rs"]
            gfin = G[4]
            for j, gt in enumerate((a, b)):
                p.op("dve", lambda e, gt=gt, j=j: e.scalar_tensor_tensor(out=xs[:], in0=gt[:], scalar=rt[:, n, 16 + j:17 + j], in1=xs[:], op0=ALU.mult, op1=ALU.add), reads=[gt.b, rt.b, xs.b], writes=[xs.b])
                yield
            p.op("act", lambda e: e.activation(out=sq[:], in_=xs[:], func=AF.Square, accum_out=ss[:]), reads=[xs.b], writes=[sq.b, ss.b])
            yield
            p.op("dve", lambda e: e.tensor_scalar(out=rs[:], in0=ss[:], scalar1=1.0 / DM, scalar2=EPS, op0=ALU.mult, op1=ALU.add), reads=[ss.b], writes=[rs.b])
            yield
            p.op("act", lambda e: e.activation(out=rs[:], in_=rs[:], func=AF.Sqrt), reads=[rs.b], writes=[rs.b])
            yield
            p.op("dve", lambda e: e.reciprocal(out=rs[:], in_=rs[:]), reads=[rs.b], writes=[rs.b])
            yield
            p.op("dve", lambda e: e.scalar_tensor_tensor(out=o[:], in0=xs[:], scalar=rs[:], in1=gfin[:], op0=ALU.mult, op1=ALU.mult), reads=[xs.b, rs.b, gfin.b], writes=[o.b])
            p.dma("sp", lambda e: e.dma_start(out=out.ap()[r0:r0 + 128, :], in_=o[:]), reads=[o.b], writes=[Buf()])
            yield

        def fetch(n):
            xs, a, b = xt[n % NB8], g1[n % NB8], g2[n % NB8]
            r0 = n * 128
            p.dma("sp", lambda e: e.dma_start(out=xs[:], in_=x_src.ap()[r0:r0 + 128, :]), writes=[xs.b])
            for j, gt in enumerate((a, b)):
                p.op("act", lambda e, gt=gt: e.memzero(gt[:]), writes=[gt.b])
                p.dma("pool", lambda e, gt=gt, j=j: e.indirect_dma_start(out=gt[:], out_offset=None, in_=yg.ap(), in_offset=bass.IndirectOffsetOnAxis(ap=idx12[:, n, j:j + 1], axis=0),
                                                                        bounds_check=bcreg(e), oob_is_err=False), reads=[idx12.b], writes=[gt.b])

        Ws = [W, env["newW"]()]

        def run_rr(gens):
            gens = list(gens)
            while gens:
                for g_ in list(gens):
                    try:
                        next(g_)
                    except StopIteration:
                        gens.remove(g_)

        for n in range(NB8 - 2):
            fetch(n)
        for n in range(0, NSUB, 2):
            for m_ in (n, n + 1):
                if m_ + NB8 - 2 < NSUB:
                    fetch(m_ + NB8 - 2)
            run_rr([do_tile(n), do_tile(n + 1)])
```

```python
import contextlib
import numpy as np
import ml_dtypes
import concourse.bass as bass
import concourse.mybir as mybir
from concourse.bass_utils import run_bass_kernel_spmd
from concourse.alu_op_type import AluOpType as ALU

AF = mybir.ActivationFunctionType
F32 = mybir.dt.float32
BF16 = mybir.dt.bfloat16
I32 = mybir.dt.int32
U32 = mybir.dt.uint32
AX = mybir.AxisListType

SAME_ENGINE_SYNC = True
NDMA_SLOTS = {"sp": 12, "pool": 6, "act": 4}


class Buf:
    __slots__ = ("w", "r")

    def __init__(self):
        self.w = None
        self.r = {}


class Prog:
    def __init__(self, nc, es):
        self.nc = nc
        self.es = es
        self.eng = {"pe": nc.tensor, "act": nc.scalar, "dve": nc.vector, "pool": nc.gpsimd, "sp": nc.sync}
        self.q = {e: [] for e in self.eng}
        self.sems = []
        self.own = {}
        for e in ("pe", "act", "dve", "pool"):
            self.own[e] = self._newsem("c_" + e)
        self.cnt = {e: 0 for e in self.own}
        self.waited = {e: {} for e in self.eng}
        self.slots = {}
        self.dn = {}
        for qn, k in NDMA_SLOTS.items():
            self.slots[qn] = [[self._newsem("d_%s%d" % (qn, i)), 0] for i in range(k)]
            self.dn[qn] = 0
        self.n_inst = 0

    def _newsem(self, name):
        s = self.es.enter_context(self.nc.semaphore(name))
        self.sems.append(s)
        return len(self.sems) - 1

    def _deps(self, reads, writes):
        deps = {}
        for b in reads:
            if b.w is not None and deps.get(b.w[0], 0) < b.w[1]:
                deps[b.w[0]] = b.w[1]
        for b in writes:
            if b.w is not None and deps.get(b.w[0], 0) < b.w[1]:
                deps[b.w[0]] = b.w[1]
            for s, v in b.r.items():
                if deps.get(s, 0) < v:
                    deps[s] = v
        return deps

    def _waits(self, eng, deps, skip=None):
        wd = self.waited[eng]
        waits = []
        for s, v in deps.items():
            if s == skip:
                continue
            if wd.get(s, 0) >= v:
                continue
            wd[s] = v
            waits.append((s, v))
        return waits

    def _mark(self, ev, reads, writes):
        s, v = ev
        for b in reads:
            if b.r.get(s, 0) < v:
                b.r[s] = v
        for b in writes:
            b.w = ev
            b.r = {}

    def op(self, eng, fn, reads=(), writes=()):
        own = self.own[eng]
        deps = self._deps(reads, writes)
        skip = own if (eng == "pe" or not SAME_ENGINE_SYNC) else None
        waits = self._waits(eng, deps, skip)
        self.cnt[eng] += 1
        ev = (own, self.cnt[eng])
        self.q[eng].append((waits, fn, own, 1))
        self._mark(ev, reads, writes)
        self.n_inst += 1

    def dma(self, qn, fn, reads=(), writes=()):
        deps = self._deps(reads, writes)
        sl = self.slots[qn][self.dn[qn] % len(self.slots[qn])]
        self.dn[qn] += 1
        if sl[1] > 0 and deps.get(sl[0], 0) < sl[1]:
            deps[sl[0]] = sl[1]
        waits = self._waits(qn, deps)
        sl[1] += 16
        ev = (sl[0], sl[1])
        self.q[qn].append((waits, fn, sl[0], 16))
        self._mark(ev, reads, writes)
        self.n_inst += 1

    def finish(self):
        waits = []
        for qn in self.slots:
            for s, v in self.slots[qn]:
                if v > 0:
                    waits.append((s, v))
        for e in self.own:
            if self.cnt[e] > 0:
                waits.append((self.own[e], self.cnt[e]))
        self.q["sp"].append((waits, None, None, 0))

    def barrier(self):
        allw = {}
        for qn in self.slots:
            for s, v in self.slots[qn]:
                if v > 0:
                    allw[s] = v
        for e in self.own:
            if self.cnt[e] > 0:
                allw[self.own[e]] = self.cnt[e]
        for eng in self.eng:
            waits = self._waits(eng, allw)
            if waits:
                self.q[eng].append((waits, None, None, 0))

    @contextlib.contextmanager
    def phase(self):
        old = self.es
        with contextlib.ExitStack() as pes:
            self.es = pes
            yield
            self.barrier()
            self.emit()
        self.es = old

    def simulate(self, q):
        if not hasattr(self, "_simval"):
            self._simval = {}
        val = self._simval
        pos = {e: 0 for e in q}
        progress = True
        while progress:
            progress = False
            for e in q:
                while pos[e] < len(q[e]):
                    waits, fn, s_, inc = q[e][pos[e]]
                    if any(val.get(ws, 0) < wv for ws, wv in waits):
                        break
                    if fn is not None:
                        val[s_] = val.get(s_, 0) + inc
                    pos[e] += 1
                    progress = True
        stuck = {e: pos[e] for e in q if pos[e] < len(q[e])}
        if stuck:
            msg = []
            for e, i in stuck.items():
                waits = q[e][i][0]
                msg.append("%s@%d/%d waits %s have %s" % (e, i, len(q[e]), waits, [val.get(ws, 0) for ws, _ in waits]))
            raise RuntimeError("DEADLOCK in emitted program: " + "; ".join(msg))

    def emit(self):
        nc = self.nc
        sems = self.sems
        q = self.q
        self.q = {e: [] for e in self.eng}
        self.simulate(q)
        with nc.Block() as block:
            def mk(ename):
                def body(e):
                    for waits, fn, s, inc in q[ename]:
                        for ws, wv in waits:
                            e.wait_ge(sems[ws], wv)
                        if fn is not None:
                            fn(e).then_inc(sems[s], inc)
                return body
            block.tensor(mk("pe"))
            block.scalar(mk("act"))
            block.vector(mk("dve"))
            block.gpsimd(mk("pool"))
            block.sync(mk("sp"))


class T:
    _n = [0]

    def __init__(self, p, name, shape, dtype, psum=False):
        T._n[0] += 1
        name = "%s_%d" % (name, T._n[0])
        if psum:
            self.t = p.es.enter_context(p.nc.psum_tensor(name, shape, dtype))
        else:
            self.t = p.es.enter_context(p.nc.sbuf_tensor(name, shape, dtype))
        self.b = Buf()

    def __getitem__(self, k):
        return self.t[k]


class D:
    def __init__(self, nc, name, shape, dtype, kind="Internal"):
        self.t = nc.dram_tensor(name, list(shape), dtype, kind=kind)
        self.bufs = {}

    @classmethod
    def wrap(cls, handle):
        o = cls.__new__(cls)
        o.t = handle
        o.bufs = {}
        return o

    def b(self, key=0):
        if key not in self.bufs:
            self.bufs[key] = Buf()
        return self.bufs[key]

    def ap(self):
        return self.t.ap()


TT = 4096
DM = 1024
NSUB = TT // 128
EVW = 4608
DFF = 2816
DEXP = 3584
NEXP = 8
CAP = 1280
SB_FILL = 0
SB_WB = 32
EPS = 1e-6


def host_consts():
    c = {}
    c["ident"] = np.eye(128, dtype=ml_dtypes.bfloat16)
    c["identf"] = np.eye(128, dtype=np.float32)
    inv = (1.0 / (np.float32(10000.0) ** (np.arange(0, 128, 2, dtype=np.float32) / np.float32(128)))).astype(np.float32)
    ang = (np.arange(TT, dtype=np.float32)[None, :] * inv[:, None]).astype(np.float32)
    cos = np.cos(ang.astype(np.float64)).astype(np.float32)
    sin = np.sin(ang.astype(np.float64)).astype(np.float32)
    c["cosT"] = np.concatenate([cos, cos], 0)
    c["sinT"] = np.concatenate([-sin, sin], 0)
    g = 1.0 - 2.0 ** (-5.0 - np.arange(4, dtype=np.float64))
    i = np.arange(128)
    jj, ii = np.meshgrid(i, i, indexing="ij")
    same = (jj // 64) == (ii // 64)
    causal2 = (jj < 64) & (ii >= 64)
    m = np.zeros((128, 4, 128), np.float64)
    for h in range(4):
        m[:, h, :] = np.where(same | causal2, g[h] ** np.abs(ii - jj), 0.0) * 128 ** -0.5
    c["rmask"] = m.astype(np.float32)
    qd = np.zeros((128, 4, 512), np.float64)
    for h in range(4):
        qd[:, h, :] = (g[h] ** ((np.arange(512) % 128) + 1.0))[None, :]
    c["qdec"] = qd.astype(np.float32)
    kd = np.zeros((128, 4), np.float64)
    for h in range(4):
        kd[:, h] = g[h] ** (127.0 - i) * 128 ** -0.5
    c["kdec"] = kd.astype(np.float32)
    c["_cd"] = [float(g[h] ** 128) for h in range(4)]
    sm = np.zeros((128, 4, 512), np.float32)
    s = np.arange(128)[:, None]
    t = np.arange(512)[None, :]
    for r in range(4):
        sm[:, r, :] = ((r * 128 + s) < t)
    c["sbmask"] = sm.astype(ml_dtypes.bfloat16)
    c["negtri"] = (-(jj >= ii).astype(np.float32)).astype(ml_dtypes.bfloat16)
    c["negones"] = (-np.ones((128, 128), np.float32)).astype(ml_dtypes.bfloat16)
    c["negcomp"] = (-(jj < ii).astype(np.float32)).astype(ml_dtypes.bfloat16)
    c["triex"] = ((jj < ii).astype(np.float32)).astype(ml_dtypes.bfloat16)
    c["ones"] = np.ones((128, 128), ml_dtypes.bfloat16)
    c["eoff"] = np.tile((np.arange(8, dtype=np.float32) * CAP)[None, :], (128, 1))
    return c


CONST_DT = {"ident": BF16, "identf": F32, "cosT": F32, "sinT": F32, "rmask": F32, "qdec": F32, "kdec": F32,
            "sbmask": BF16, "negtri": BF16, "negones": BF16, "negcomp": BF16, "triex": BF16, "ones": BF16, "eoff": F32}

IN_SHAPES = {
    "x": ([TT, DM], F32), "norms": ([5, DM], F32),
    "ev_w_in": ([DM, EVW], F32), "ev_gn": ([1, 512], F32), "ev_w_out": ([DM, DM], F32),
    "ev_gate": ([DM, DFF], F32), "ev_up": ([DM, DFF], F32), "ev_down": ([DFF, DM], F32),
    "od_w_in": ([DM, 2560], F32), "od_small": ([128, 4, 12], F32), "od_wa": ([128, 4, 128], F32),
    "od_wx": ([128, 4, 128], F32), "od_w_out": ([DM, DM], F32), "od_rw": ([DM, 8], F32), "od_rb": ([1, 8], F32),
    "od_eg": ([NEXP, DM, DEXP], F32), "od_eu": ([NEXP, DM, DEXP], F32), "od_ed": ([NEXP, DEXP, DM], F32),
}


def rmsnorm_tile(p, W, xt, gt, ht, tag=""):
    sq, ss, rs = W["sq"], W["ss"], W["rs"]
    p.op("act", lambda e: e.activation(out=sq[:], in_=xt[:], func=AF.Square, accum_out=ss[:]), reads=[xt.b], writes=[sq.b, ss.b])
    p.op("dve", lambda e: e.tensor_scalar(out=rs[:], in0=ss[:], scalar1=1.0 / DM, scalar2=EPS, op0=ALU.mult, op1=ALU.add), reads=[ss.b], writes=[rs.b])
    p.op("act", lambda e: e.activation(out=rs[:], in_=rs[:], func=AF.Sqrt), reads=[rs.b], writes=[rs.b])
    p.op("dve", lambda e: e.reciprocal(out=rs[:], in_=rs[:]), reads=[rs.b], writes=[rs.b])
    p.op("dve", lambda e: e.scalar_tensor_tensor(out=ht[:], in0=xt[:], scalar=rs[:], in1=gt[:], op0=ALU.mult, op1=ALU.mult), reads=[xt.b, rs.b, gt.b], writes=[ht.b])


def build(upto=99, taps=()):
    nc = bass.Bass("TRN2", target_bir_lowering=False)
    cs = host_consts()
    cd = cs.pop("_cd")
    shp = dict(IN_SHAPES)
    if upto < 7:
        for k in ("od_eg", "od_eu", "od_ed"):
            shp[k] = ([1, 8, 8], F32)
    IN = {k: nc.dram_tensor(k, v[0], v[1], kind="ExternalInput") for k, v in shp.items()}
    CI = {k: nc.dram_tensor("c_" + k, list(cs[k].shape), CONST_DT[k], kind="ExternalInput") for k in cs}
    out = D(nc, "out", [TT, DM], F32, kind="ExternalOutput")

    def scratch(name, shape, dt):
        return D(nc, name, shape, dt, kind="ExternalOutput" if name in taps else "Internal")

    sb_qT = scratch("sb_qT", [512, TT], BF16)
    sb_kT = scratch("sb_kT", [512, TT], BF16)
    sb_v = scratch("sb_v", [TT, 512], BF16)
    ymixT = scratch("ymixT", [DM, TT], BF16)
    x1 = scratch("x1", [TT, DM], F32)
    x2 = scratch("x2", [TT, DM], F32)
    ymix1T = scratch("ymix1T", [DM, TT], BF16)
    x3 = scratch("x3", [TT, DM], F32)
    hn = scratch("hn", [TT, DM], BF16)
    rinfo = scratch("rinfo", [TT, 8], F32)
    ffn_bf = [scratch("ffn_gate_bf", [DM, DFF], BF16), scratch("ffn_up_bf", [DM, DFF], BF16), scratch("ffn_down_bf", [DFF, DM], BF16)]
    xg = scratch("xg", [NEXP * CAP, DM], BF16)
    yg = scratch("yg", [NEXP * CAP, DM], F32)

    with contextlib.ExitStack() as es:
        p = Prog(nc, es)
        class CL:
            def __init__(self):
                self.c = {}

            def __getitem__(self, k):
                if k not in self.c:
                    t = T(p, "k_" + k, list(cs[k].shape), CONST_DT[k])
                    p.dma("sp", lambda e: e.dma_start(out=t[:], in_=CI[k].ap()), writes=[t.b])
                    self.c[k] = t
                return self.c[k]

        class GL:
            def __init__(self):
                self.c = {}

            def __getitem__(self, i):
                if i not in self.c:
                    g = T(p, "g%d" % i, [128, DM], F32)
                    p.dma("sp", lambda e: e.dma_start(out=g[:], in_=IN["norms"].ap()[i:i + 1, :].partition_broadcast(128)), writes=[g.b])
                    self.c[i] = g
                return self.c[i]

        def newW():
            return {"sq": T(p, "n_sq", [128, DM], F32), "ss": T(p, "n_ss", [128, 1], F32), "rs": T(p, "n_rs", [128, 1], F32)}
        env = {"CL": CL, "GL": GL, "newW": newW, "CI": CI}
        xin = D.wrap(IN["x"])

        if upto >= 1:
            phase1(p, IN, env, cd, sb_qT, sb_kT, sb_v, ymixT, xin)
        if upto >= 2:
            phase2(p, IN, env, sb_qT, sb_kT, sb_v, ymixT, wcast=list(zip(("ev_gate", "ev_up", "ev_down"), ffn_bf)), xg=xg)
        if upto >= 3:
            phase3(p, IN, env, ymixT, xin, x1, "ev_w_out", lambda it: [("r", it)] + [("s", pr_, it) for pr_ in range(4)])
        with contextlib.ExitStack() as s45:
            old_es, p.es = p.es, s45
            PW = p5_weights_alloc(p)
            p.es = old_es
            if upto >= 4:
                phase4(p, IN, env, x1, x2, pre_hook=(lambda: p5_weights_load(p, IN, PW)) if upto >= 5 else None, wbf=ffn_bf)
            if upto >= 5:
                phase5(p, IN, env, x2, x3, PW)
        R = {"rt": T(p, "rt", [128, NSUB, 20], F32), "idx12": T(p, "idx12", [128, NSUB, 2], I32)}
        if upto >= 6:
            phase6(p, IN, env, x3, xg, R)
        if upto >= 7:
            phase7(p, IN, env, xg, yg)
        if upto >= 8:
            phase8(p, IN, env, x3, yg, out, R)
        p.finish()
    return nc, cs


def phase1(p, IN, env, cd, sb_qT, sb_kT, sb_v, ymixT, x_src):
    with p.phase():
        C, G, W = env["CL"](), env["GL"](), env["newW"]()
        win = T(p, "win", [128, 8, EVW], BF16)
        wb = [Buf() for _ in range(9)]
        for cg in (0, 7, 1, 8, 4, 5, 2, 3, 6):
            p.dma("pool", lambda e, cg=cg: e.dma_start(out=win[:, :, cg * 512:(cg + 1) * 512],
                  in_=IN["ev_w_in"].ap()[:, cg * 512:(cg + 1) * 512].rearrange("(k p) n -> p k n", p=128)), writes=[wb[cg]])
        gnb = T(p, "gnb", [128, 512], F32)
        p.dma("sp", lambda e: e.dma_start(out=gnb[:], in_=IN["ev_gn"].ap().partition_broadcast(128)), writes=[gnb.b])
        xt = [T(p, "xt%d" % i, [128, DM], F32) for i in range(2)]
        ht = [T(p, "ht%d" % i, [128, DM], BF16) for i in range(2)]
        hT = [T(p, "hT%d" % i, [128, 8, 512], BF16) for i in range(2)]
        hTb = [[Buf() for _ in range(4)] for _ in range(2)]
        pT = T(p, "pT", [128, 8, 128], BF16, psum=True)
        pF = [T(p, "pF%d" % i, [128, 512], F32, psum=True) for i in range(2)]
        pM = T(p, "pM", [128, 512], F32, psum=True)
        pS = T(p, "pS", [128, 4, 128], F32, psum=True)
        pK = T(p, "pK", [128, 4, 128], BF16, psum=True)
        pOs = [T(p, "pO%d" % i, [128, 4, 128], F32, psum=True) for i in range(2)]
        pKV = pS
        t1 = [T(p, "t1_%d" % i, [128, 512], F32) for i in range(2)]
        t2 = [T(p, "t2_%d" % i, [128, 512], F32) for i in range(2)]
        qT = [T(p, "qT%d" % h, [128, 512], BF16) for h in range(4)]
        qdT = [T(p, "qdT%d" % h, [128, 512], BF16) for h in range(4)]
        kT = [T(p, "kT%d" % h, [128, 512], BF16) for h in range(4)]
        sbq = [T(p, "sbq0", [128, 4, 512], BF16)] * 2
        sbk = [T(p, "sbk0", [128, 4, 512], BF16)] * 2
        svt = [T(p, "svt0", [128, 4, 512], BF16)] * 2
        cstt = [T(p, "cst%d" % i, [128, 512], F32) for i in range(2)]
        sntt = [T(p, "snt%d" % i, [128, 512], F32) for i in range(2)]
        vt = [T(p, "vt%d" % i, [128, 512], BF16) for i in range(4)]
        Gt = [T(p, "Gt%d" % i, [128, 512], F32) for i in range(4)]
        Pt = T(p, "Pt", [128, 4, 128], BF16)
        kd = T(p, "kd", [128, 4, 128], BF16)
        st = T(p, "st", [128, 4, 128], F32)
        stbf = T(p, "stbf", [128, 4, 128], BF16)
        bst = T(p, "bst", [128, 4, 6], F32)
        mv = T(p, "mv", [128, 4, 2], F32)
        rstd = T(p, "rstd", [128, 4], F32)
        nb = T(p, "nb", [128, 4], F32)
        on = T(p, "on", [128, 512], F32)
        yr = T(p, "yr", [128, 512], BF16)
        yT = [T(p, "yT%d" % i, [128, 4, 512], BF16) for i in range(2)]
        ident = C["ident"]

        p.op("pool", lambda e: e.memset(st[:], 0.0), writes=[st.b])
        p.op("pool", lambda e: e.memset(stbf[:], 0.0), writes=[stbf.b])

        def wcols(c0, n):
            return [wb[i] for i in range(c0 // 512, (c0 + n - 1) // 512 + 1)]

        def fm_proj(ps, c0, hTt, hbs):
            for k in range(8):
                p.op("pe", lambda e, k=k: e.matmul(ps[:], lhsT=win[:, k, c0:c0 + 128], rhs=hTt[:, k, :], start=(k == 0), stop=(k == 7)),
                     reads=wcols(c0, 128) + hbs, writes=[ps.b])

        def run_rr(gens):
            gens = list(gens)
            while gens:
                for g_ in list(gens):
                    try:
                        next(g_)
                    except StopIteration:
                        gens.remove(g_)

        def A_gen(it):
            par = it % 2
            t0 = it * 512

            def nrm(sub):
                xs = xt[sub % 2]
                hs = ht[sub % 2]
                r0 = t0 + sub * 128
                p.dma("sp", lambda e, xs=xs, r0=r0: e.dma_start(out=xs[:], in_=x_src.ap()[r0:r0 + 128, :]), reads=[x_src.b(r0 // 128)], writes=[xs.b])
                rmsnorm_tile(p, W, xs, G[0], hs)

            def trs(sub):
                hs = ht[sub % 2]
                for k in range(8):
                    p.op("pe", lambda e, k=k, hs=hs: e.transpose(out=pT[:, k, :], in_=hs[:, k * 128:(k + 1) * 128], identity=ident[:]),
                         reads=[hs.b, ident.b], writes=[pT.b])
                p.op("act", lambda e, sub=sub, par=par: e.copy(out=hT[par][:, :, sub * 128:(sub + 1) * 128], in_=pT[:]), reads=[pT.b], writes=[hTb[par][sub]])

            for fn_, sub in ((nrm, 0), (nrm, 1), (trs, 0), (nrm, 2), (trs, 1), (nrm, 3), (trs, 2), (trs, 3)):
                fn_(sub)
                yield

        def do_tile(it):
            par = it % 2
            t0 = it * 512
            hTt = hT[par]
            hbs = hTb[par]
            cst, snt = cstt[par], sntt[par]
            p.dma("sp", lambda e, cst=cst: e.dma_start(out=cst[:], in_=env["CI"]["cosT"].ap()[:, t0:t0 + 512]), writes=[cst.b])
            p.dma("sp", lambda e, snt=snt: e.dma_start(out=snt[:], in_=env["CI"]["sinT"].ap()[:, t0:t0 + 512]), writes=[snt.b])
            cosv, sinv = cst[:], snt[:]
            for h in range(4):
                for which in range(2):
                    cbase = which * 512 + h * 128
                    sbase = 3584 + which * 512 + h * 128
                    a, b = t1[which], t2[which]
                    fm_proj(pF[0], cbase, hTt, hbs)
                    p.op("dve", lambda e, a=a: e.tensor_tensor(out=a[:], in0=pF[0][:], in1=cosv, op=ALU.mult), reads=[pF[0].b, cst.b], writes=[a.b])
                    fm_proj(pF[1], sbase, hTt, hbs)
                    p.op("dve", lambda e, b=b: e.tensor_tensor(out=b[:], in0=pF[1][:], in1=sinv, op=ALU.mult), reads=[pF[1].b, snt.b], writes=[b.b])
                    if which == 0:
                        p.op("pool", lambda e, a=a, b=b, h=h: e.tensor_tensor(out=qT[h][:], in0=a[:], in1=b[:], op=ALU.add), reads=[a.b, b.b], writes=[qT[h].b])
                        p.op("pool", lambda e, a=a, b=b: e.tensor_tensor(out=a[:], in0=a[:], in1=b[:], op=ALU.add), reads=[a.b, b.b], writes=[a.b])
                        p.op("pool", lambda e, a=a, h=h: e.tensor_tensor(out=qdT[h][:], in0=a[:], in1=C["qdec"][:, h, :], op=ALU.mult), reads=[a.b, C["qdec"].b], writes=[qdT[h].b])
                    else:
                        p.op("pool", lambda e, a=a, b=b, h=h: e.tensor_tensor(out=kT[h][:], in0=a[:], in1=b[:], op=ALU.add), reads=[a.b, b.b], writes=[kT[h].b])
            def C_gen():
                for which, (stg, dst) in enumerate(((sbq[par], sb_qT), (sbk[par], sb_kT))):
                    for g in range(4):
                        ps = pF[g % 2]
                        fm_proj(ps, 2048 + which * 512 + g * 128, hTt, hbs)
                        p.op("act", lambda e, ps=ps, stg=stg, g=g: e.copy(out=stg[:, g, :], in_=ps[:]), reads=[ps.b], writes=[stg.b])
                        yield
                    p.dma("sp", lambda e, stg=stg, dst=dst: e.dma_start(out=dst.ap()[:, t0:t0 + 512].rearrange("(g p) t -> p g t", p=128), in_=stg[:]),
                          reads=[stg.b], writes=[dst.b(it)])
            def D_gen(sub):
                for (c0, kind) in ((1024, "v"), (1536, "g"), (3072, "sv")):
                    for k in range(8):
                        p.op("pe", lambda e, k=k, sub=sub, c0=c0: e.matmul(pM[:], lhsT=hTt[:, k, sub * 128:(sub + 1) * 128], rhs=win[:, k, c0:c0 + 512], start=(k == 0), stop=(k == 7)),
                             reads=wcols(c0, 512) + [hbs[sub]], writes=[pM.b])
                    if kind == "v":
                        p.op("act", lambda e, sub=sub: e.copy(out=vt[sub][:], in_=pM[:]), reads=[pM.b], writes=[vt[sub].b])
                    elif kind == "g":
                        p.op("act", lambda e, sub=sub: e.activation(out=Gt[sub][:], in_=pM[:], func=AF.Silu), reads=[pM.b], writes=[Gt[sub].b])
                        p.op("pool", lambda e, sub=sub: e.tensor_tensor(out=Gt[sub][:], in0=Gt[sub][:], in1=gnb[:], op=ALU.mult), reads=[Gt[sub].b, gnb.b], writes=[Gt[sub].b])
                    else:
                        p.op("act", lambda e, sub=sub, par=par: e.copy(out=svt[par][:, sub, :], in_=pM[:]), reads=[pM.b], writes=[svt[par].b])
                    yield
                if sub == 3:
                    p.dma("sp", lambda e, par=par: e.dma_start(out=sb_v.ap()[t0:t0 + 512, :].rearrange("(s p) c -> p s c", p=128), in_=svt[par][:]),
                          reads=[svt[par].b], writes=[sb_v.b(it)])
            def E1_gen(sub):
                sl = slice(sub * 128, (sub + 1) * 128)
                pO = pOs[sub % 2]
                for h in range(4):
                    p.op("pe", lambda e, h=h, sl=sl: e.matmul(pS[:, h, :], lhsT=kT[h][:, sl], rhs=qT[h][:, sl], start=True, stop=True, skip_group_check=True),
                         reads=[kT[h].b, qT[h].b], writes=[pS.b])
                yield
                p.op("dve", lambda e: e.tensor_tensor(out=Pt[:], in0=pS[:], in1=C["rmask"][:], op=ALU.mult), reads=[pS.b, C["rmask"].b], writes=[Pt.b])
                yield
                for h in range(4):
                    p.op("pe", lambda e, h=h, sl=sl: e.transpose(out=pK[:, h, :], in_=kT[h][:, sl], identity=ident[:]), reads=[kT[h].b, ident.b], writes=[pK.b])
                yield
                for h in range(4):
                    p.op("dve", lambda e, h=h: e.tensor_scalar(out=kd[:, h, :], in0=pK[:, h, :], scalar1=C["kdec"][:, h:h + 1], scalar2=None, op0=ALU.mult),
                         reads=[pK.b, C["kdec"].b], writes=[kd.b])
                yield
                for h in range(4):
                    hs_ = slice(h * 128, (h + 1) * 128)
                    p.op("pe", lambda e, h=h, hs_=hs_, sub=sub: e.matmul(pO[:, h, :], lhsT=Pt[:, h, :], rhs=vt[sub][:, hs_], start=True, stop=False, skip_group_check=True),
                         reads=[Pt.b, vt[sub].b], writes=[pO.b])
                    p.op("pe", lambda e, h=h, sl=sl: e.matmul(pO[:, h, :], lhsT=qdT[h][:, sl], rhs=stbf[:, h, :], start=False, stop=True, skip_group_check=True),
                         reads=[qdT[h].b, stbf.b], writes=[pO.b])
                yield
                for h in range(4):
                    hs_ = slice(h * 128, (h + 1) * 128)
                    p.op("pe", lambda e, h=h, hs_=hs_, sub=sub: e.matmul(pKV[:, h, :], lhsT=kd[:, h, :], rhs=vt[sub][:, hs_], start=True, stop=True, skip_group_check=True),
                         reads=[kd.b, vt[sub].b], writes=[pKV.b])
                yield
                for h in range(4):
                    p.op("dve", lambda e, h=h: e.scalar_tensor_tensor(out=st[:, h, :], in0=st[:, h, :], scalar=cd[h], in1=pKV[:, h, :], op0=ALU.mult, op1=ALU.add),
                         reads=[st.b, pKV.b], writes=[st.b])
                yield
                p.op("act", lambda e: e.copy(out=stbf[:], in_=st[:]), reads=[st.b], writes=[stbf.b])
                yield

            def E2_gen(sub):
                sl = slice(sub * 128, (sub + 1) * 128)
                pO = pOs[sub % 2]
                for h in range(4):
                    p.op("dve", lambda e, h=h: e.bn_stats(out=bst[:, h, :], in_=pO[:, h, :]), reads=[pO.b], writes=[bst.b])
                yield
                for h in range(4):
                    p.op("dve", lambda e, h=h: e.bn_aggr(out=mv[:, h, :], in_=bst[:, h, :]), reads=[bst.b], writes=[mv.b])
                yield
                p.op("dve", lambda e: e.tensor_scalar(out=rstd[:], in0=mv[:, :, 1], scalar1=EPS, scalar2=None, op0=ALU.add), reads=[mv.b], writes=[rstd.b])
                yield
                p.op("act", lambda e: e.activation(out=rstd[:], in_=rstd[:], func=AF.Sqrt), reads=[rstd.b], writes=[rstd.b])
                yield
                p.op("dve", lambda e: e.reciprocal(out=rstd[:], in_=rstd[:]), reads=[rstd.b], writes=[rstd.b])
                yield
                p.op("dve", lambda e: e.scalar_tensor_tensor(out=nb[:], in0=mv[:, :, 0], scalar=-1.0, in1=rstd[:], op0=ALU.mult, op1=ALU.mult), reads=[mv.b, rstd.b], writes=[nb.b])
                yield
                for h in range(4):
                    p.op("act", lambda e, h=h: e.activation(out=on[:, h * 128:(h + 1) * 128], in_=pO[:, h, :], func=AF.Identity, scale=rstd[:, h:h + 1], bias=nb[:, h:h + 1]),
                         reads=[pO.b, rstd.b, nb.b], writes=[on.b])
                yield
                p.op("dve", lambda e, sub=sub: e.tensor_tensor(out=yr[:], in0=on[:], in1=Gt[sub][:], op=ALU.mult), reads=[on.b, Gt[sub].b], writes=[yr.b])
                yield
                for c in range(4):
                    p.op("pe", lambda e, c=c: e.transpose(out=pT[:, c, :], in_=yr[:, c * 128:(c + 1) * 128], identity=ident[:]), reads=[yr.b, ident.b], writes=[pT.b])
                p.op("act", lambda e, sl=sl, par=par: e.copy(out=yT[par][:, :, sl], in_=pT[:, 0:4, :]), reads=[pT.b], writes=[yT[par].b])
                yield
                if sub == 3:
                    p.dma("sp", lambda e, par=par: e.dma_start(out=ymixT.ap()[0:512, t0:t0 + 512].rearrange("(c p) t -> p c t", p=128), in_=yT[par][:]),
                          reads=[yT[par].b], writes=[ymixT.b(("r", it))])

            run_rr([D_gen(0)])
            run_rr([E1_gen(0), D_gen(1), C_gen()])
            run_rr([E2_gen(0), E1_gen(1), D_gen(2)] + ([A_gen(it + 1)] if it + 1 < TT // 512 else []))
            run_rr([E2_gen(1), E1_gen(2), D_gen(3)])
            run_rr([E2_gen(2), E1_gen(3)])
            run_rr([E2_gen(3)])

        run_rr([A_gen(0)])
        for it in range(TT // 512):
            do_tile(it)


def prep_shared(inp):
    f = lambda a: np.ascontiguousarray(np.asarray(a, dtype=np.float32))
    d = {}
    d["norms"] = f(np.concatenate([inp["norm_mix"][0:1], inp["norm_ffn"][0:1], inp["norm_mix"][1:2], inp["norm_ffn"][1:2], inp["norm_final"][None, :]], 0))
    w = np.asarray(inp["ev_w_in"][0])
    swap = np.concatenate([np.arange(h * 128 + 64, h * 128 + 128).tolist() + np.arange(h * 128, h * 128 + 64).tolist() for h in range(4)]).astype(np.int64)
    d["ev_w_in"] = f(np.concatenate([w, w[:, 0:512][:, swap], w[:, 512:1024][:, swap]], 1))
    d["ev_gn"] = f(inp["ev_ret_gn"][0:1])
    d["ev_w_out"] = f(inp["ev_w_out"][0])
    d["ev_gate"] = f(inp["ev_ffn_gate"][0])
    d["ev_up"] = f(inp["ev_ffn_up"][0])
    d["ev_down"] = f(inp["ev_ffn_down"][0])
    d["od_w_in"] = f(inp["od_w_in"][0])
    sm = np.zeros((512, 12), np.float32)
    sm[:, 0:3] = np.asarray(inp["od_conv_w"][0]).T
    sm[:, 3:7] = np.asarray(inp["od_lru_conv_w"][0]).T
    sm[:, 7] = np.asarray(inp["od_lru_conv_b"][0])
    sm[:, 8] = np.asarray(inp["od_lru_ba"][0])
    sm[:, 9] = np.asarray(inp["od_lru_bx"][0])
    sm[:, 10] = np.asarray(inp["od_lru_lambda"][0])
    d["od_small"] = f(sm.reshape(4, 128, 12).transpose(1, 0, 2))
    for nm, key in (("od_wa", "od_lru_wa"), ("od_wx", "od_lru_wx")):
        wsrc = np.asarray(inp[key][0])
        bd = np.zeros((128, 4, 128), np.float32)
        for hh in range(8):
            c, o = hh // 2, (hh % 2) * 64
            bd[o:o + 64, c, o:o + 64] = wsrc[hh]
        d[nm] = bd
    d["od_w_out"] = f(inp["od_w_out"][0])
    d["od_rw"] = f(inp["od_router_w"][0])
    d["od_rb"] = f(inp["od_router_b"][0:1])
    d["od_eg"] = f(inp["od_exp_gate"][0])
    d["od_eu"] = f(inp["od_exp_up"][0])
    d["od_ed"] = f(inp["od_exp_down"][0])
    return d


_CACHE = {}


def kernel(**inputs):
    if "nc" not in _CACHE:
        _CACHE["nc"] = build()
    nc, cs = _CACHE["nc"]
    shared = prep_shared(inputs)
    for k, v in cs.items():
        shared["c_" + k] = v
    x = np.asarray(inputs["x"], dtype=np.float32)
    in_maps = []
    for c in range(8):
        m = dict(shared)
        m["x"] = np.ascontiguousarray(x[c])
        in_maps.append(m)
    res = run_bass_kernel_spmd(nc, in_maps, core_ids=list(range(8)))
    return np.stack([np.asarray(r["out"]) for r in res.results], 0).astype(np.float32)


def phase2(p, IN, env, sb_qT, sb_kT, sb_v, ymixT, wcast=None, xg=None):
    with p.phase():
        C = env["CL"]()
        sbmask, negtri, negcomp = C["sbmask"], C["negtri"], C["negcomp"]
        vall = T(p, "vall", [128, NSUB, 512], BF16)
        p.dma("sp", lambda e: e.dma_start(out=vall[:], in_=sb_v.ap().rearrange("(n p) c -> p n c", p=128)), writes=[vall.b])
        qz = [[T(p, "qz%d_%d" % (i, hh), [128, TT], BF16) for hh in range(2)] for i in range(2)]
        for i in range(2):
            for hh in range(2):
                o = (1 - hh) * 64
                p.op("pool", lambda e, i=i, hh=hh, o=o: e.memset(qz[i][hh][o:o + 64, :], 0.0), writes=[qz[i][hh].b])
        kp = [T(p, "kp%d" % i, [128, TT], BF16) for i in range(2)]
        pZ = [T(p, "pZ%d" % i, [128, 2, 512], F32, psum=True) for i in range(2)]
        pB = T(p, "pB", [128, 2, 512], F32, psum=True)
        pOo = T(p, "pOo", [128, 2, 512], F32, psum=True)
        NE, NS, NX, NA = 6, 4, 3, 3
        Et = [T(p, "Et%d" % i, [128, 2, 512], F32) for i in range(NE)]
        SPt = [T(p, "SPt%d" % i, [128, 2, 512], BF16) for i in range(NS)]
        Xt = [T(p, "Xt%d" % i, [128, 2, 512], F32) for i in range(NX)]
        At = [T(p, "At%d" % i, [128, 2, 512], BF16) for i in range(NA)]
        ost = [T(p, "ost%d" % i, [128, 512], BF16) for i in range(2)]
        tiles = []
        g = 0
        for pair in range(4):
            for Q in range(8):
                kmax = 4 * Q + 3
                kmin = max(0, 4 * Q - SB_WB)
                for kb in range(kmax, kmin - 1, -1):
                    tiles.append(dict(pair=pair, Q=Q, kb=kb, first=(kb == kmax), last=(kb == kmin), r=kb - 4 * Q, g=g, newpair=(Q == 0 and kb == kmax)))
                g += 1

        def S0(i, t):
            pair, kb, Q = t["pair"], t["kb"], t["Q"]
            kt = kp[pair % 2]
            w0 = max(t["r"], 0) * 128
            if t["newpair"]:
                for hh in range(2):
                    qh = qz[pair % 2][hh]
                    p.dma("sp", lambda e, hh=hh, qh=qh: e.dma_start(out=qh[hh * 64:(hh + 1) * 64, :], in_=sb_qT.ap()[pair * 128 + hh * 64:pair * 128 + (hh + 1) * 64, :]), writes=[qh.b])
                p.dma("sp", lambda e: e.dma_start(out=kt[:], in_=sb_kT.ap()[pair * 128:(pair + 1) * 128, :]), writes=[kt.b])
            z, e_ = pZ[i % 2], Et[i % NE]
            for hh in range(2):
                qt = qz[pair % 2][hh]
                p.op("pe", lambda e, hh=hh, qt=qt: e.matmul(z[:, hh, w0:512], lhsT=kt[:, kb * 128:(kb + 1) * 128], rhs=qt[:, Q * 512 + w0:(Q + 1) * 512], start=True, stop=True),
                     reads=[kt.b, qt.b], writes=[z.b])
            p.op("act", lambda e: e.activation(out=e_[:, :, w0:512], in_=z[:, :, w0:512], func=AF.Exp, scale=0.125), reads=[z.b], writes=[e_.b])

        def S1(i, t):
            e_, sp_ = Et[i % NE], SPt[i % NS]
            r = t["r"]
            w0 = max(r, 0) * 128
            p.op("act", lambda e: e.activation(out=sp_[:, :, w0:512], in_=e_[:, :, w0:512], func=AF.Ln, bias=1.0), reads=[e_.b], writes=[sp_.b])
            if r >= 0:
                for hh in range(2):
                    p.op("pool", lambda e, hh=hh: e.tensor_tensor(out=sp_[:, hh, w0:w0 + 128], in0=sp_[:, hh, w0:w0 + 128], in1=sbmask[:, r, w0:w0 + 128], op=ALU.mult), reads=[sp_.b, sbmask.b], writes=[sp_.b])

        def S2a(i, t):
            sp_ = SPt[i % NS]
            w0 = max(t["r"], 0) * 128
            for hh in range(2):
                p.op("pe", lambda e, hh=hh: e.matmul(pB[:, hh, w0:512], lhsT=negtri[:], rhs=sp_[:, hh, w0:512], start=t["first"], stop=True, skip_group_check=True), reads=[negtri.b, sp_.b], writes=[pB.b])

        def S2b(i, t):
            x = Xt[i % NX]
            w0 = max(t["r"], 0) * 128
            p.op("act", lambda e: e.activation(out=x[:, :, w0:512], in_=pB[:, :, w0:512], func=AF.Exp), reads=[pB.b], writes=[x.b])

        def S2c(i, t):
            sp_ = SPt[i % NS]
            w0 = max(t["r"], 0) * 128
            if not t["last"]:
                for hh in range(2):
                    p.op("pe", lambda e, hh=hh: e.matmul(pB[:, hh, w0:512], lhsT=negcomp[:], rhs=sp_[:, hh, w0:512], start=False, stop=True, skip_group_check=True), reads=[negcomp.b, sp_.b], writes=[pB.b])

        def S3(i, t):
            e_, x, a = Et[i % NE], Xt[i % NX], At[i % NA]
            r = t["r"]
            w0 = max(r, 0) * 128
            p.op("dve", lambda e: e.tensor_tensor(out=a[:, :, w0:512], in0=e_[:, :, w0:512], in1=x[:, :, w0:512], op=ALU.mult), reads=[e_.b, x.b], writes=[a.b])
            if r >= 0:
                for hh in range(2):
                    p.op("pool", lambda e, hh=hh: e.tensor_tensor(out=a[:, hh, w0:w0 + 128], in0=a[:, hh, w0:w0 + 128], in1=sbmask[:, r, w0:w0 + 128], op=ALU.mult), reads=[a.b, sbmask.b], writes=[a.b])

        def S4(i, t):
            a = At[i % NA]
            osb = ost[t["g"] % 2]
            kb, Q, pair = t["kb"], t["Q"], t["pair"]
            w0 = max(t["r"], 0) * 128
            for hh in range(2):
                p.op("pe", lambda e, hh=hh: e.matmul(pOo[:, hh, w0:512], lhsT=vall[:, kb, pair * 128:(pair + 1) * 128], rhs=a[:, hh, w0:512], start=t["first"], stop=t["last"], skip_group_check=True),
                     reads=[vall.b, a.b], writes=[pOo.b])
            if t["last"]:
                for hh in range(2):
                    p.op("dve", lambda e, hh=hh: e.tensor_copy(out=osb[hh * 64:(hh + 1) * 64, :], in_=pOo[hh * 64:(hh + 1) * 64, hh, :]), reads=[pOo.b], writes=[osb.b])
                p.dma("sp", lambda e: e.dma_start(out=ymixT.ap()[512 + pair * 128:512 + (pair + 1) * 128, Q * 512:(Q + 1) * 512], in_=osb[:]),
                      reads=[osb.b], writes=[ymixT.b(("s", pair, Q))])

        if xg is not None:
            zt = T(p, "zt", [128, CAP // 128, DM], BF16)
            p.op("pool", lambda e: e.memset(zt[:], 0.0), writes=[zt.b])
        NTL = len(tiles)
        order = ((3, S2a), (3, S2b), (0, S0), (5, S4), (3, S2c), (1, S1), (4, S3))
        for s_ in range(NTL + 5):
            if xg is not None and 250 <= s_ < 250 + NEXP * 10 and (s_ - 250) % 10 == 0:
                ex_ = (s_ - 250) // 10
                p.dma("sp", lambda e, ex_=ex_: e.dma_start(out=xg.ap()[ex_ * CAP:(ex_ + 1) * CAP, :].rearrange("(n p) d -> p n d", p=128), in_=zt[:]), reads=[zt.b], writes=[Buf()])
            if wcast is not None and s_ in (30, 100, 170):
                src_name, dst = wcast[(30, 100, 170).index(s_)]
                p.dma("pool", lambda e, src_name=src_name, dst=dst: e.dma_start(out=dst.ap(), in_=IN[src_name].ap()), writes=[Buf()])
            for d_, fn_ in order:
                if 0 <= s_ - d_ < NTL:
                    fn_(s_ - d_, tiles[s_ - d_])


def load_norm_T(p, W, src, r0, g, xs, hs, pT, ident, dstT, c0, dbuf, hdt_fp32=False, part=0):
    if part in (0, 1):
        p.dma("sp", lambda e: e.dma_start(out=xs[:], in_=src.ap()[r0:r0 + 128, :]), reads=[src.b(r0 // 128)], writes=[xs.b])
        rmsnorm_tile(p, W, xs, g, hs)
    if part == 1:
        return
    for k in range(8):
        p.op("pe", lambda e, k=k: e.transpose(out=pT[:, k, :], in_=hs[:, k * 128:(k + 1) * 128], identity=ident[:]), reads=[hs.b, ident.b], writes=[pT.b])
    p.op("act", lambda e: e.copy(out=dstT[:, :, c0:c0 + 128], in_=pT[:]), reads=[pT.b], writes=[dbuf])


def phase3(p, IN, env, ymixT, x_src, x_dst, wname, ykeys, xg=None):
    with p.phase():
        wo = T(p, "wo", [128, 8, DM], BF16)
        p.dma("pool", lambda e: e.dma_start(out=wo[:], in_=IN[wname].ap().rearrange("(k p) n -> p k n", p=128)), writes=[wo.b])
        ym = [T(p, "ym%d" % i, [128, 8, 512], BF16) for i in range(2)]
        xt = [T(p, "xt%d" % i, [128, DM], F32) for i in range(4)]
        xo = [T(p, "xo%d" % i, [128, DM], F32) for i in range(2)]
        pY = [T(p, "pY%d" % i, [128, 512], F32, psum=True) for i in range(4)]

        def ymload(it):
            t0 = it * 512
            ymt = ym[it % 2]
            p.dma("sp", lambda e: e.dma_start(out=ymt[:], in_=ymixT.ap()[:, t0:t0 + 512].rearrange("(k p) t -> p k t", p=128)),
                  reads=[ymixT.b(k) for k in ykeys(it)], writes=[ymt.b])

        def xload(n):
            xs = xt[n % 4]
            r0 = n * 128
            p.dma("sp", lambda e: e.dma_start(out=xs[:], in_=x_src.ap()[r0:r0 + 128, :]), reads=[x_src.b(n)], writes=[xs.b])

        ymload(0)
        for n in range(3):
            xload(n)

        def do_tile(it):
            t0 = it * 512
            ymt = ym[it % 2]
            if it + 1 < TT // 512:
                ymload(it + 1)
            for sub in range(4):
                n = it * 4 + sub
                xs, xos = xt[n % 4], xo[n % 2]
                r0 = t0 + sub * 128
                if n + 3 < NSUB:
                    xload(n + 3)
                for half in range(2):
                    py = pY[(n * 2 + half) % 4]
                    for k in range(8):
                        p.op("pe", lambda e, k=k, py=py, sub=sub, half=half: e.matmul(py[:], lhsT=ymt[:, k, sub * 128:(sub + 1) * 128], rhs=wo[:, k, half * 512:(half + 1) * 512], start=(k == 0), stop=(k == 7)),
                             reads=[ymt.b, wo.b], writes=[py.b])
                    p.op("dve", lambda e, py=py, xs=xs, xos=xos, half=half: e.tensor_tensor(out=xos[:, half * 512:(half + 1) * 512], in0=py[:], in1=xs[:, half * 512:(half + 1) * 512], op=ALU.add),
                         reads=[py.b, xs.b], writes=[xos.b])
                p.dma("sp", lambda e, xos=xos, r0=r0: e.dma_start(out=x_dst.ap()[r0:r0 + 128, :], in_=xos[:]), reads=[xos.b], writes=[x_dst.b(r0 // 128)])

        for it in range(TT // 512):
            do_tile(it)


def ffn_alloc(p, ntok_max):
    FB = {"n": 0, "ln": 0, "j": 0, "jj": 0}
    FB["wg"] = [T(p, "wg%d" % i, [128, 8, 512], BF16) for i in range(2)]
    FB["wu"] = [T(p, "wu%d" % i, [128, 8, 512], BF16) for i in range(2)]
    FB["wd"] = [T(p, "wd%d" % i, [128, 4, DM], BF16) for i in range(2)]
    FB["act"] = [T(p, "actT%d" % i, [128, 4, ntok_max], BF16) for i in range(2)]
    FB["sg"] = [T(p, "sg%d" % i, [128, 512], F32) for i in range(2)]
    FB["pG"] = [T(p, "pG%d" % i, [128, 512], F32, psum=True) for i in range(2)]
    FB["pU"] = [T(p, "pU%d" % i, [128, 512], F32, psum=True) for i in range(2)]
    FB["pY"] = [T(p, "pYf%d" % i, [128, 512], F32, psum=True) for i in range(2)]
    return FB


def ffn_load(p, FB, wg_ap, wu_ap, wd_ap, fs, fsz, wq="pool"):
    i = FB["ln"] % 2
    FB["ln"] += 1
    wg, wu, wd = FB["wg"][i], FB["wu"][i], FB["wd"][i]
    nfc = fsz // 128
    p.dma(wq, lambda e: e.dma_start(out=wg[:, :, 0:fsz], in_=wg_ap[:, fs:fs + fsz].rearrange("(k p) n -> p k n", p=128)), writes=[wg.b])
    p.dma(wq, lambda e: e.dma_start(out=wu[:, :, 0:fsz], in_=wu_ap[:, fs:fs + fsz].rearrange("(k p) n -> p k n", p=128)), writes=[wu.b])
    p.dma(wq, lambda e: e.dma_start(out=wd[:, 0:nfc, :], in_=wd_ap[fs:fs + fsz, :].rearrange("(c p) n -> p c n", p=128)), writes=[wd.b])


def ffn_block(p, FB, xT, xbufs, ntok, wg_ap, wu_ap, wd_ap, F, yacc, ybufs, hooks=(), hooks_early=(), wq="pool", preloaded=False):
    groups = [(fs, min(512, F - fs)) for fs in range(0, F, 512)]
    hooks = list(hooks)
    hooks_early = list(hooks_early)
    late_ready = []
    per = -(-len(hooks) // max(1, len(groups) - 1)) if hooks else 0

    def do_group(gi, fs, fsz):
        if gi == 0 and not preloaded:
            ffn_load(p, FB, wg_ap, wu_ap, wd_ap, fs, fsz, wq)
        if gi + 1 < len(groups):
            ffn_load(p, FB, wg_ap, wu_ap, wd_ap, groups[gi + 1][0], groups[gi + 1][1], wq)
        i = FB["n"] % 2
        FB["n"] += 1
        wg, wu, wd, act = FB["wg"][i], FB["wu"][i], FB["wd"][i], FB["act"][i]
        nfc = fsz // 128
        for tt0 in range(0, ntok, 512):
            tn = min(512, ntok - tt0)
            for fc in range(nfc):
                j = FB["j"] % 2
                FB["j"] += 1
                pg, pu, sg = FB["pG"][j], FB["pU"][j], FB["sg"][j]
                for k in range(8):
                    p.op("pe", lambda e, k=k, pg=pg, fc=fc, tt0=tt0, tn=tn: e.matmul(pg[:, 0:tn], lhsT=wg[:, k, fc * 128:(fc + 1) * 128], rhs=xT[:, k, tt0:tt0 + tn], start=(k == 0), stop=(k == 7)),
                         reads=[wg.b] + xbufs, writes=[pg.b])
                for k in range(8):
                    p.op("pe", lambda e, k=k, pu=pu, fc=fc, tt0=tt0, tn=tn: e.matmul(pu[:, 0:tn], lhsT=wu[:, k, fc * 128:(fc + 1) * 128], rhs=xT[:, k, tt0:tt0 + tn], start=(k == 0), stop=(k == 7)),
                         reads=[wu.b] + xbufs, writes=[pu.b])
                p.op("act", lambda e, pg=pg, sg=sg, tn=tn: e.activation(out=sg[:, 0:tn], in_=pg[:, 0:tn], func=AF.Silu), reads=[pg.b], writes=[sg.b])
                p.op("dve", lambda e, pu=pu, sg=sg, fc=fc, tt0=tt0, tn=tn: e.tensor_tensor(out=act[:, fc, tt0:tt0 + tn], in0=pu[:, 0:tn], in1=sg[:, 0:tn], op=ALU.mult),
                     reads=[pu.b, sg.b], writes=[act.b])
        if hooks_early or late_ready:
            for _ in range(len(late_ready)):
                late_ready.pop(0)()
            for _ in range(per):
                if hooks_early:
                    hooks_early.pop(0)()
                    late_ready.append(hooks.pop(0))
        for ts in range(ntok // 128):
            for half in range(2):
                jj = FB["jj"] % 2
                FB["jj"] += 1
                py = FB["pY"][jj]
                for fc in range(nfc):
                    p.op("pe", lambda e, fc=fc, py=py, ts=ts, half=half: e.matmul(py[:], lhsT=act[:, fc, ts * 128:(ts + 1) * 128], rhs=wd[:, fc, half * 512:(half + 1) * 512], start=(fc == 0), stop=(fc == nfc - 1)),
                         reads=[act.b, wd.b], writes=[py.b])
                if gi == 0:
                    p.op("act", lambda e, py=py, ts=ts, half=half: e.copy(out=yacc[:, ts, half * 512:(half + 1) * 512], in_=py[:]), reads=[py.b], writes=[ybufs[ts]])
                else:
                    p.op("dve", lambda e, py=py, ts=ts, half=half: e.tensor_tensor(out=yacc[:, ts, half * 512:(half + 1) * 512], in0=py[:], in1=yacc[:, ts, half * 512:(half + 1) * 512], op=ALU.add),
                         reads=[py.b, ybufs[ts]], writes=[ybufs[ts]])

    for gi, (fs, fsz) in enumerate(groups):
        do_group(gi, fs, fsz)
    for h_ in late_ready + hooks:
        h_()


def phase4(p, IN, env, x_src, x_dst, pre_hook=None, wbf=None):
    NTK = 1024
    with p.phase():
        C, G, W = env["CL"](), env["GL"](), env["newW"]()
        ident = C["ident"]
        FB = ffn_alloc(p, NTK)
        pT = T(p, "pT", [128, 8, 128], BF16, psum=True)
        xTs = [T(p, "xT%d" % i, [128, 8, NTK], BF16) for i in range(2)]
        xbs = [[Buf() for _ in range(NTK // 128)] for _ in range(2)]
        yacc = T(p, "yacc", [128, NTK // 128, DM], F32)
        yb = [Buf() for _ in range(NTK // 128)]
        xt = [T(p, "xt%d" % i, [128, DM], F32) for i in range(2)]
        xt2 = [T(p, "xtb%d" % i, [128, DM], F32) for i in range(2)]
        ht = [T(p, "ht%d" % i, [128, DM], BF16) for i in range(2)]

        def prep(s, sub, part=0):
            load_norm_T(p, W, x_src, s * NTK + sub * 128, G[1], xt2[sub % 2], ht[sub % 2], pT, ident, xTs[s % 2], sub * 128, xbs[s % 2][sub], part=part)

        for sub in range(NTK // 128):
            prep(0, sub)
        if pre_hook is not None:
            pre_hook()

        def do_super(s):
            nxt = s + 1 < TT // NTK
            hooks_e = [(lambda sub=sub: prep(s + 1, sub, 1)) for sub in range(NTK // 128)] if nxt else []
            hooks_l = [(lambda sub=sub: prep(s + 1, sub, 2)) for sub in range(NTK // 128)] if nxt else []
            waps = (IN["ev_gate"].ap(), IN["ev_up"].ap(), IN["ev_down"].ap()) if wbf is None else (wbf[0].ap(), wbf[1].ap(), wbf[2].ap())
            ffn_block(p, FB, xTs[s % 2], xbs[s % 2], NTK, waps[0], waps[1], waps[2], DFF, yacc, yb, hooks=hooks_l, hooks_early=hooks_e, preloaded=(s > 0))
            if nxt:
                ffn_load(p, FB, waps[0], waps[1], waps[2], 0, 512)
            xq = xt + xt2
            nsub = NTK // 128

            def rload(sub):
                r0 = s * NTK + sub * 128
                xs = xq[sub % 4]
                p.dma("sp", lambda e: e.dma_start(out=xs[:], in_=x_src.ap()[r0:r0 + 128, :]), reads=[x_src.b(r0 // 128)], writes=[xs.b])

            for sub in range(3):
                rload(sub)
            for sub in range(nsub):
                r0 = s * NTK + sub * 128
                xs = xq[sub % 4]
                p.op("pool", lambda e, xs=xs, sub=sub: e.tensor_tensor(out=xs[:], in0=xs[:], in1=yacc[:, sub, :], op=ALU.add), reads=[xs.b, yb[sub]], writes=[xs.b])
                if sub + 3 < nsub:
                    rload(sub + 3)
                p.dma("sp", lambda e, xs=xs, r0=r0: e.dma_start(out=x_dst.ap()[r0:r0 + 128, :], in_=xs[:]), reads=[xs.b], writes=[x_dst.b(r0 // 128)])

        for s in range(TT // NTK):
            do_super(s)


def p5_weights_alloc(p):
    PW = {"win": T(p, "win1", [128, 8, 2560], BF16), "wbs": [Buf() for _ in range(5)],
          "wa": T(p, "wa", [128, 4, 128], BF16), "wx": T(p, "wx", [128, 4, 128], BF16)}
    return PW


def p5_weights_load(p, IN, PW):
    win, wbs, wa, wx = PW["win"], PW["wbs"], PW["wa"], PW["wx"]
    for cg in (1, 2, 0, 3, 4):
        p.dma("pool", lambda e, cg=cg: e.dma_start(out=win[:, :, cg * 512:(cg + 1) * 512],
              in_=IN["od_w_in"].ap()[:, cg * 512:(cg + 1) * 512].rearrange("(k p) n -> p k n", p=128)), writes=[wbs[cg]])
    p.dma("pool", lambda e: e.dma_start(out=wa[:], in_=IN["od_wa"].ap()), writes=[wa.b])
    p.dma("pool", lambda e: e.dma_start(out=wx[:], in_=IN["od_wx"].ap()), writes=[wx.b])


def phase5(p, IN, env, x_src, x_dst, PW):
    with p.phase():
        C, G, W = env["CL"](), env["GL"](), env["newW"]()
        ident = C["ident"]
        win, wbs, wa, wx = PW["win"], PW["wbs"], PW["wa"], PW["wx"]
        wo = T(p, "wo1", [128, 8, DM], BF16)
        p.dma("pool", lambda e: e.dma_start(out=wo[:], in_=IN["od_w_out"].ap().rearrange("(k p) n -> p k n", p=128)), writes=[wo.b])
        sm = T(p, "sm", [128, 4, 12], F32)
        p.dma("sp", lambda e: e.dma_start(out=sm[:], in_=IN["od_small"].ap()), writes=[sm.b])
        asc = T(p, "asc", [128, 4], F32)
        p.op("act", lambda e: e.activation(out=asc[:], in_=sm[:, :, 10], func=AF.Exp, scale=-1.0), reads=[sm.b], writes=[asc.b])
        p.op("act", lambda e: e.activation(out=asc[:], in_=asc[:], func=AF.Ln, bias=1.0), reads=[asc.b], writes=[asc.b])
        p.op("dve", lambda e: e.tensor_scalar(out=asc[:], in0=asc[:], scalar1=-8.0, scalar2=None, op0=ALU.mult), reads=[asc.b], writes=[asc.b])
        xt = [T(p, "xt%d" % i, [128, DM], F32) for i in range(2)]
        xo = [T(p, "xo%d" % i, [128, DM], F32) for i in range(2)]
        xt2 = [T(p, "xtr%d" % i, [128, DM], F32) for i in range(2)]
        ht = [T(p, "ht%d" % i, [128, DM], BF16) for i in range(2)]
        hT = [T(p, "hT%d" % i, [128, 8, 512], BF16) for i in range(2)]
        hTb = [[Buf() for _ in range(4)] for _ in range(2)]
        ymT = [T(p, "ymT%d" % i, [128, 8, 512], BF16) for i in range(2)]
        ymb = [[Buf() for _ in range(8)] for _ in range(2)]
        pT = T(p, "pT", [128, 8, 128], BF16, psum=True)
        pF = [T(p, "pF%d" % i, [128, 512], F32, psum=True) for i in range(5)]
        pY = [T(p, "pY%d" % i, [128, 512], F32, psum=True) for i in range(2)]
        vbuf = [T(p, "vbuf%d" % c, [128, 514], F32) for c in range(4)]
        xbuf = [T(p, "xbuf%d" % c, [128, 515], F32) for c in range(4)]
        hprev = T(p, "hprev", [128, 4], F32)
        ctmp = [{k: T(p, "ct%d_%s" % (i, k), [128, 512], F32) for k in ("gc", "o")} for i in range(4)]
        ltmp = [{k: T(p, "lt%d_%s" % (i, k), [128, 512], F32) for k in ("xc", "r", "ig", "a", "a2", "b", "h", "xg", "x2", "th", "gl")} for i in range(2)]
        xcbs = [T(p, "xcb%d" % i, [128, 512], BF16) for i in range(2)]
        for c in range(4):
            p.op("pool", lambda e, c=c: e.memset(vbuf[c][:], 0.0), writes=[vbuf[c].b])
            p.op("pool", lambda e, c=c: e.memset(xbuf[c][:], 0.0), writes=[xbuf[c].b])
        p.op("pool", lambda e: e.memset(hprev[:], 0.0), writes=[hprev.b])
        cnt = [0]
        NT5 = TT // 512

        def run_rr(gens):
            gens = list(gens)
            while gens:
                for g_ in list(gens):
                    try:
                        next(g_)
                    except StopIteration:
                        gens.remove(g_)

        def A_gen(it):
            par = it % 2
            for part, sub in ((1, 0), (1, 1), (2, 0), (1, 2), (2, 1), (1, 3), (2, 2), (2, 3)):
                load_norm_T(p, W, x_src, it * 512 + sub * 128, G[2], xt[sub % 2], ht[sub % 2], pT, ident, hT[par], sub * 128, hTb[par][sub], part=part)
                yield

        def proj(it, c0):
            hTt, hbs = hT[it % 2], hTb[it % 2]
            ps = pF[cnt[0] % 5]
            cnt[0] += 1
            for k in range(8):
                p.op("pe", lambda e, k=k: e.matmul(ps[:], lhsT=win[:, k, c0:c0 + 128], rhs=hTt[:, k, :], start=(k == 0), stop=(k == 7)),
                     reads=[wbs[c0 // 512]] + hbs, writes=[ps.b])
            return ps

        def conv_gen(it, c):
            yt, ybs = ymT[it % 2], ymb[it % 2]
            vb = vbuf[c]
            gc, o = ctmp[c]["gc"], ctmp[c]["o"]
            pgc = proj(it, 512 + c * 128)
            p.op("act", lambda e: e.copy(out=gc[:], in_=pgc[:]), reads=[pgc.b], writes=[gc.b])
            yield
            pu = proj(it, 1024 + c * 128)
            p.op("dve", lambda e: e.tensor_tensor(out=vb[:, 2:514], in0=pu[:], in1=gc[:], op=ALU.mult), reads=[pu.b, gc.b], writes=[vb.b])
            yield
            p.op("dve", lambda e: e.tensor_scalar(out=o[:], in0=vb[:, 2:514], scalar1=sm[:, c, 2:3], scalar2=None, op0=ALU.mult), reads=[vb.b, sm.b], writes=[o.b])
            yield
            p.op("dve", lambda e: e.scalar_tensor_tensor(out=o[:], in0=vb[:, 1:513], scalar=sm[:, c, 1:2], in1=o[:], op0=ALU.mult, op1=ALU.add), reads=[vb.b, sm.b, o.b], writes=[o.b])
            yield
            p.op("dve", lambda e: e.scalar_tensor_tensor(out=o[:], in0=vb[:, 0:512], scalar=sm[:, c, 0:1], in1=o[:], op0=ALU.mult, op1=ALU.add), reads=[vb.b, sm.b, o.b], writes=[o.b])
            p.op("pool", lambda e: e.tensor_copy(out=vb[:, 0:2], in_=vb[:, 512:514]), reads=[vb.b], writes=[vb.b])
            yield
            pgb = proj(it, c * 128)
            p.op("dve", lambda e: e.tensor_tensor(out=yt[:, c, :], in0=pgb[:], in1=o[:], op=ALU.mult), reads=[pgb.b, o.b], writes=[ybs[c]])
            yield

        def lru_gen(it, c):
            yt, ybs = ymT[it % 2], ymb[it % 2]
            xb = xbuf[c]
            tm = ltmp[c % 2]
            xcb = xcbs[c % 2]
            xc, r, ig, a, a2, b, h, xg, x2, th, gl = (tm[k] for k in ("xc", "r", "ig", "a", "a2", "b", "h", "xg", "x2", "th", "gl"))
            pxr = proj(it, 1536 + c * 128)
            p.op("act", lambda e: e.copy(out=xb[:, 3:515], in_=pxr[:]), reads=[pxr.b], writes=[xb.b])
            yield
            p.op("dve", lambda e: e.tensor_scalar(out=xc[:], in0=xb[:, 3:515], scalar1=sm[:, c, 6:7], scalar2=sm[:, c, 7:8], op0=ALU.mult, op1=ALU.add), reads=[xb.b, sm.b], writes=[xc.b])
            yield
            for j, off in ((5, 2), (4, 1), (3, 0)):
                p.op("dve", lambda e, j=j, off=off: e.scalar_tensor_tensor(out=xc[:], in0=xb[:, off:off + 512], scalar=sm[:, c, j:j + 1], in1=xc[:], op0=ALU.mult, op1=ALU.add),
                     reads=[xb.b, sm.b, xc.b], writes=[xc.b])
                yield
            p.op("pool", lambda e: e.tensor_copy(out=xb[:, 0:3], in_=xb[:, 512:515]), reads=[xb.b], writes=[xb.b])
            p.op("act", lambda e: e.copy(out=xcb[:], in_=xc[:]), reads=[xc.b], writes=[xcb.b])
            yield
            pr = pF[cnt[0] % 5]
            cnt[0] += 1
            p.op("pe", lambda e: e.matmul(pr[:], lhsT=wa[:, c, :], rhs=xcb[:], start=True, stop=True), reads=[wa.b, xcb.b], writes=[pr.b])
            pi = pF[cnt[0] % 5]
            cnt[0] += 1
            p.op("pe", lambda e: e.matmul(pi[:], lhsT=wx[:, c, :], rhs=xcb[:], start=True, stop=True), reads=[wx.b, xcb.b], writes=[pi.b])
            yield
            p.op("act", lambda e: e.activation(out=r[:], in_=pr[:], func=AF.Sigmoid, bias=sm[:, c, 8:9]), reads=[pr.b, sm.b], writes=[r.b])
            p.op("act", lambda e: e.activation(out=ig[:], in_=pi[:], func=AF.Sigmoid, bias=sm[:, c, 9:10]), reads=[pi.b, sm.b], writes=[ig.b])
            yield
            p.op("act", lambda e: e.activation(out=a[:], in_=r[:], func=AF.Exp, scale=asc[:, c:c + 1]), reads=[r.b, asc.b], writes=[a.b])
            yield
            p.op("pool", lambda e: e.tensor_tensor(out=a2[:], in0=a[:], in1=a[:], op=ALU.mult), reads=[a.b], writes=[a2.b])
            yield
            p.op("act", lambda e: e.activation(out=a2[:], in_=a2[:], func=AF.Sqrt, scale=-1.0, bias=1.0), reads=[a2.b], writes=[a2.b])
            yield
            p.op("pool", lambda e: e.tensor_tensor(out=b[:], in0=a2[:], in1=ig[:], op=ALU.mult), reads=[a2.b, ig.b], writes=[b.b])
            yield
            p.op("pool", lambda e: e.tensor_tensor(out=b[:], in0=b[:], in1=xc[:], op=ALU.mult), reads=[b.b, xc.b], writes=[b.b])
            yield
            p.op("dve", lambda e: e.tensor_tensor_scan(out=h[:], data0=a[:], data1=b[:], initial=hprev[:, c:c + 1], op0=ALU.mult, op1=ALU.add),
                 reads=[a.b, b.b, hprev.b], writes=[h.b])
            p.op("pool", lambda e: e.tensor_copy(out=hprev[:, c:c + 1], in_=h[:, 511:512]), reads=[h.b], writes=[hprev.b])
            yield
            pxg = proj(it, 2048 + c * 128)
            p.op("act", lambda e: e.copy(out=xg[:], in_=pxg[:]), reads=[pxg.b], writes=[xg.b])
            yield
            p.op("pool", lambda e: e.tensor_tensor(out=x2[:], in0=xg[:], in1=xg[:], op=ALU.mult), reads=[xg.b], writes=[x2.b])
            yield
            p.op("dve", lambda e: e.tensor_scalar(out=x2[:], in0=x2[:], scalar1=0.044715, scalar2=1.0, op0=ALU.mult, op1=ALU.add), reads=[x2.b], writes=[x2.b])
            yield
            p.op("pool", lambda e: e.tensor_tensor(out=x2[:], in0=x2[:], in1=xg[:], op=ALU.mult), reads=[x2.b, xg.b], writes=[x2.b])
            yield
            p.op("act", lambda e: e.activation(out=th[:], in_=x2[:], func=AF.Tanh, scale=0.7978845608028654), reads=[x2.b], writes=[th.b])
            yield
            p.op("dve", lambda e: e.scalar_tensor_tensor(out=gl[:], in0=th[:], scalar=1.0, in1=xg[:], op0=ALU.add, op1=ALU.mult), reads=[th.b, xg.b], writes=[gl.b])
            yield
            p.op("dve", lambda e: e.scalar_tensor_tensor(out=yt[:, 4 + c, :], in0=h[:], scalar=0.5, in1=gl[:], op0=ALU.mult, op1=ALU.mult), reads=[h.b, gl.b], writes=[ybs[4 + c]])
            yield

        def out_gen(it):
            yt, ybs = ymT[it % 2], ymb[it % 2]
            t0 = it * 512
            for sub in range(4):
                n = it * 4 + sub
                xs, xos = xt2[n % 2], xo[n % 2]
                r0 = t0 + sub * 128
                p.dma("sp", lambda e, xs=xs, r0=r0: e.dma_start(out=xs[:], in_=x_src.ap()[r0:r0 + 128, :]), reads=[x_src.b(r0 // 128)], writes=[xs.b])
                for half in range(2):
                    py = pY[(n * 2 + half) % 2]
                    for k in range(8):
                        p.op("pe", lambda e, k=k, py=py, sub=sub, half=half: e.matmul(py[:], lhsT=yt[:, k, sub * 128:(sub + 1) * 128], rhs=wo[:, k, half * 512:(half + 1) * 512], start=(k == 0), stop=(k == 7)),
                             reads=[ybs[k], wo.b], writes=[py.b])
                    p.op("dve", lambda e, py=py, xs=xs, xos=xos, half=half: e.tensor_tensor(out=xos[:, half * 512:(half + 1) * 512], in0=py[:], in1=xs[:, half * 512:(half + 1) * 512], op=ALU.add),
                         reads=[py.b, xs.b], writes=[xos.b])
                    yield
                p.dma("sp", lambda e, xos=xos, r0=r0: e.dma_start(out=x_dst.ap()[r0:r0 + 128, :], in_=xos[:]), reads=[xos.b], writes=[x_dst.b(r0 // 128)])

        run_rr([A_gen(0)])
        for it in range(NT5):
            extra = []
            if it + 1 < NT5:
                extra.append(A_gen(it + 1))
            if it >= 1:
                extra.append(out_gen(it - 1))
            run_rr([conv_gen(it, 0), conv_gen(it, 1), lru_gen(it, 0), lru_gen(it, 1)] + extra)
            run_rr([conv_gen(it, 2), conv_gen(it, 3), lru_gen(it, 2), lru_gen(it, 3)])
        run_rr([out_gen(NT5 - 1)])


BIGIDX = 1.0e6


def phase6(p, IN, env, x_src, xg, R):
    with p.phase():
        C, G, W = env["CL"](), env["GL"](), env["newW"]()
        identf, triex, ones, eoff = C["identf"], C["triex"], C["ones"], C["eoff"]
        rt, idx12 = R["rt"], R["idx12"]
        wr = T(p, "wr", [128, 8, 8], F32)
        p.dma("sp", lambda e: e.dma_start(out=wr[:], in_=IN["od_rw"].ap().rearrange("(k p) n -> p k n", p=128)), writes=[wr.b])
        rbt = T(p, "rbt", [128, 8], F32)
        p.dma("sp", lambda e: e.dma_start(out=rbt[:], in_=IN["od_rb"].ap().partition_broadcast(128)), writes=[rbt.b])
        zb = []
        xt = [T(p, "xt%d" % i, [128, DM], F32) for i in range(2)]
        hf = [T(p, "hf%d" % i, [128, DM], F32) for i in range(2)]
        hb = [T(p, "hb%d" % i, [128, DM], BF16) for i in range(4)]
        pTf = [T(p, "pTf%d" % i, [128, 4, 128], F32, psum=True) for i in range(2)]
        hTfs = [T(p, "hTf%d" % i, [128, 8, 128], F32) for i in range(2)]
        pLs = [T(p, "pL%d" % i, [128, 8], F32, psum=True) for i in range(2)]
        pP = T(p, "pP", [128, 8], F32, psum=True)
        pC = T(p, "pC", [128, 8], F32, psum=True)
        cntb = T(p, "cntb", [128, 8], F32)
        p.op("pool", lambda e: e.memset(cntb[:], 0.0), writes=[cntb.b])
        s8 = {k: T(p, "s8_" + k, [128, 8], F32) for k in ("lg", "mx", "sel2", "pos", "v", "dst", "junk")}
        selb = T(p, "selb", [128, 8], BF16)
        s1 = {k: T(p, "s1_" + k, [128, 1], F32) for k in ("d", "ex", "i1", "i2")}
        dsti = [T(p, "dsti%d" % i, [128, 8], I32) for i in range(2)]

        regbox = {}

        def bcreg(e):
            if "r" not in regbox:
                regbox["r"] = e.to_reg(NEXP * CAP - 1)
            return regbox["r"]

        def front(n):
            xs, hfs, hbs = xt[n % 2], hf[n % 2], hb[n % 4]
            hTf, pL = hTfs[n % 2], pLs[n % 2]
            r0 = n * 128
            p.dma("sp", lambda e: e.dma_start(out=xs[:], in_=x_src.ap()[r0:r0 + 128, :]), reads=[x_src.b(n)], writes=[xs.b])
            rmsnorm_tile(p, W, xs, G[3], hfs)
            yield
            p.op("act", lambda e: e.copy(out=hbs[:], in_=hfs[:]), reads=[hfs.b], writes=[hbs.b])
            for k in range(8):
                pt = pTf[k // 4]
                p.op("pe", lambda e, k=k, pt=pt: e.transpose(out=pt[:, k % 4, :], in_=hfs[:, k * 128:(k + 1) * 128], identity=identf[:]), reads=[hfs.b, identf.b], writes=[pt.b])
            yield
            for j in range(2):
                p.op("act", lambda e, j=j: e.copy(out=hTf[:, j * 4:(j + 1) * 4, :], in_=pTf[j][:]), reads=[pTf[j].b], writes=[hTf.b])
            yield
            for k in range(8):
                p.op("pe", lambda e, k=k: e.matmul(pL[:], lhsT=hTf[:, k, :], rhs=wr[:, k, :], start=(k == 0), stop=(k == 7)), reads=[hTf.b, wr.b], writes=[pL.b])
            yield

        def back(n):
            hbs = hb[n % 4]
            pL = pLs[n % 2]
            lg, mx, sel2, pos, v, dst, junk = (s8[k] for k in ("lg", "mx", "sel2", "pos", "v", "dst", "junk"))
            d, ex, i1, i2 = (s1[k] for k in ("d", "ex", "i1", "i2"))
            ops = [
                lambda: p.op("dve", lambda e: e.tensor_tensor(out=lg[:], in0=pL[:], in1=rbt[:], op=ALU.add), reads=[pL.b, rbt.b], writes=[lg.b]),
                lambda: p.op("dve", lambda e: e.max(out=mx[:], in_=lg[:]), reads=[lg.b], writes=[mx.b]),
                lambda: p.op("dve", lambda e: e.tensor_scalar(out=rt[:, n, 0:8], in0=lg[:], scalar1=mx[:, 1:2], scalar2=None, op0=ALU.is_ge), reads=[lg.b, mx.b], writes=[rt.b]),
                lambda: p.op("dve", lambda e: e.tensor_scalar(out=rt[:, n, 8:16], in0=lg[:], scalar1=mx[:, 0:1], scalar2=None, op0=ALU.is_ge), reads=[lg.b, mx.b], writes=[rt.b]),
                lambda: p.op("dve", lambda e: e.tensor_tensor(out=sel2[:], in0=rt[:, n, 0:8], in1=rt[:, n, 8:16], op=ALU.subtract), reads=[rt.b], writes=[sel2.b]),
                lambda: p.op("dve", lambda e: e.tensor_tensor(out=rt[:, n, 16:17], in0=mx[:, 1:2], in1=mx[:, 0:1], op=ALU.subtract), reads=[mx.b], writes=[rt.b]),
                lambda: p.op("dve", lambda e: e.tensor_copy(out=selb[:], in_=rt[:, n, 0:8]), reads=[rt.b], writes=[selb.b]),
                lambda: (p.op("pe", lambda e: e.matmul(pP[:], lhsT=triex[:], rhs=selb[:], start=True, stop=True), reads=[triex.b, selb.b], writes=[pP.b]),
                         p.op("pe", lambda e: e.matmul(pC[:], lhsT=ones[:], rhs=selb[:], start=True, stop=True), reads=[ones.b, selb.b], writes=[pC.b])),
                lambda: (p.op("dve", lambda e: e.tensor_tensor(out=pos[:], in0=pP[:], in1=cntb[:], op=ALU.add), reads=[pP.b, cntb.b], writes=[pos.b]),
                         p.op("dve", lambda e: e.tensor_tensor(out=cntb[:], in0=pC[:], in1=cntb[:], op=ALU.add), reads=[pC.b, cntb.b], writes=[cntb.b])),
                lambda: p.op("dve", lambda e: e.tensor_scalar(out=v[:], in0=pos[:], scalar1=CAP - 0.5, scalar2=None, op0=ALU.is_lt), reads=[pos.b], writes=[v.b]),
                lambda: p.op("dve", lambda e: e.tensor_tensor(out=v[:], in0=v[:], in1=rt[:, n, 0:8], op=ALU.mult), reads=[v.b, rt.b], writes=[v.b]),
                lambda: p.op("dve", lambda e: e.tensor_tensor(out=dst[:], in0=pos[:], in1=eoff[:], op=ALU.add), reads=[pos.b, eoff.b], writes=[dst.b]),
                lambda: p.op("dve", lambda e: e.scalar_tensor_tensor(out=dst[:], in0=dst[:], scalar=-BIGIDX, in1=v[:], op0=ALU.add, op1=ALU.mult), reads=[dst.b, v.b], writes=[dst.b]),
                lambda: p.op("dve", lambda e: e.tensor_scalar(out=dst[:], in0=dst[:], scalar1=BIGIDX, scalar2=None, op0=ALU.add), reads=[dst.b], writes=[dst.b]),
                lambda: p.op("dve", lambda e: e.tensor_tensor(out=junk[:], in0=rt[:, n, 8:16], in1=dst[:], op=ALU.mult), reads=[rt.b, dst.b], writes=[junk.b]),
                lambda: p.op("dve", lambda e: e.tensor_reduce(out=i1[:], in_=junk[:], axis=AX.X, op=ALU.add), reads=[junk.b], writes=[i1.b]),
                lambda: p.op("dve", lambda e: e.tensor_copy(out=idx12[:, n, 0:1], in_=i1[:]), reads=[i1.b], writes=[idx12.b]),
                lambda: p.op("dve", lambda e: e.tensor_tensor(out=junk[:], in0=sel2[:], in1=dst[:], op=ALU.mult), reads=[sel2.b, dst.b], writes=[junk.b]),
                lambda: p.op("dve", lambda e: e.tensor_reduce(out=i2[:], in_=junk[:], axis=AX.X, op=ALU.add), reads=[junk.b], writes=[i2.b]),
                lambda: p.op("dve", lambda e: e.tensor_copy(out=idx12[:, n, 1:2], in_=i2[:]), reads=[i2.b], writes=[idx12.b]),
            ]
            for k, f_ in enumerate(ops):
                f_()
                if k % 4 == 3:
                    yield
            for j in range(2):
                p.dma("pool", lambda e, j=j: e.indirect_dma_start(out=xg.ap(), out_offset=bass.IndirectOffsetOnAxis(ap=idx12[:, n, j:j + 1], axis=0), in_=hbs[:], in_offset=None,
                                                                   bounds_check=bcreg(e), oob_is_err=False),
                      reads=[hbs.b, idx12.b] + zb, writes=[Buf()])
            yield

        def run_rr(gens):
            gens = list(gens)
            while gens:
                for g_ in list(gens):
                    try:
                        next(g_)
                    except StopIteration:
                        gens.remove(g_)

        run_rr([front(0)])
        for n in range(NSUB):
            run_rr([back(n)] + ([front(n + 1)] if n + 1 < NSUB else []))
        exa = T(p, "exa", [128, NSUB], F32)
        dna = T(p, "dna", [128, NSUB], F32)
        p.op("act", lambda e: e.activation(out=exa[:], in_=rt[:, :, 16], func=AF.Exp), reads=[rt.b], writes=[exa.b])
        p.op("dve", lambda e: e.tensor_scalar(out=dna[:], in0=exa[:], scalar1=1.0, scalar2=None, op0=ALU.add), reads=[exa.b], writes=[dna.b])
        p.op("dve", lambda e: e.reciprocal(out=rt[:, :, 16], in_=dna[:]), reads=[dna.b], writes=[rt.b])
        p.op("dve", lambda e: e.tensor_tensor(out=rt[:, :, 17], in0=exa[:], in1=rt[:, :, 16], op=ALU.mult), reads=[exa.b, rt.b], writes=[rt.b])

def phase7(p, IN, env, xg, yg):
    with p.phase():
        C = env["CL"]()
        ident = C["ident"]
        FB = ffn_alloc(p, CAP)
        NT_ = CAP // 128
        pT = T(p, "pT", [128, 8, 128], BF16, psum=True)
        xTs = [T(p, "xT%d" % i, [128, 8, CAP], BF16) for i in range(2)]
        xbs = [[Buf() for _ in range(NT_)] for _ in range(2)]
        yacc = T(p, "yacc", [128, NT_, DM], F32)
        yb = [Buf() for _ in range(NT_)]
        xr = [T(p, "xr%d" % i, [128, DM], BF16) for i in range(2)]

        def prep(ex_, n, part=0):
            r0 = ex_ * CAP + n * 128
            xs = xr[n % 2]
            xT = xTs[ex_ % 2]
            if part in (0, 1):
                p.dma("sp", lambda e: e.dma_start(out=xs[:], in_=xg.ap()[r0:r0 + 128, :]), writes=[xs.b])
            if part == 1:
                return
            for k in range(8):
                p.op("pe", lambda e, k=k: e.transpose(out=pT[:, k, :], in_=xs[:, k * 128:(k + 1) * 128], identity=ident[:]), reads=[xs.b, ident.b], writes=[pT.b])
            p.op("act", lambda e: e.copy(out=xT[:, :, n * 128:(n + 1) * 128], in_=pT[:]), reads=[pT.b], writes=[xbs[ex_ % 2][n]])

        for n in range(NT_):
            prep(0, n)

        def do_expert(ex_):
            nxt = ex_ + 1 < NEXP
            hooks_e = [(lambda n=n: prep(ex_ + 1, n, 1)) for n in range(NT_)] if nxt else []
            hooks_l = [(lambda n=n: prep(ex_ + 1, n, 2)) for n in range(NT_)] if nxt else []
            ffn_block(p, FB, xTs[ex_ % 2], xbs[ex_ % 2], CAP, IN["od_eg"].ap()[ex_], IN["od_eu"].ap()[ex_], IN["od_ed"].ap()[ex_], DEXP, yacc, yb, hooks=hooks_l, hooks_early=hooks_e,
                      preloaded=(ex_ > 0))
            if nxt:
                ffn_load(p, FB, IN["od_eg"].ap()[ex_ + 1], IN["od_eu"].ap()[ex_ + 1], IN["od_ed"].ap()[ex_ + 1], 0, 512)
            p.dma("sp", lambda e: e.dma_start(out=yg.ap()[ex_ * CAP:(ex_ + 1) * CAP, :].rearrange("(n p) d -> p n d", p=128), in_=yacc[:]), reads=yb, writes=[Buf()])

        for ex_ in range(NEXP):
            do_expert(ex_)


def phase8(p, IN, env, x_src, yg, out, R):
    with p.phase():
        G, W = env["GL"](), env["newW"]()
        rt, idx12 = R["rt"], R["idx12"]
        NB8 = 6
        xt = [T(p, "xt%d" % i, [128, DM], F32) for i in range(NB8)]
        g1 = [T(p, "g1_%d" % i, [128, DM], F32) for i in range(NB8)]
        g2 = [T(p, "g2_%d" % i, [128, DM], F32) for i in range(NB8)]
        ot = [T(p, "ot%d" % i, [128, DM], F32) for i in range(NB8)]

        regbox = {}

        def bcreg(e):
            if "r" not in regbox:
                regbox["r"] = e.to_reg(NEXP * CAP - 1)
            return regbox["r"]

        def do_tile(n):
            xs, a, b, o = xt[n % NB8], g1[n % NB8], g2[n % NB8], ot[n % NB8]
            r0 = n * 128
            Wn = Ws[n % 2]
            sq, ss, rs = Wn["sq"], Wn["ss"], Wn["rs"]
            gfin = G[4]
            for j, gt in enumerate((a, b)):
                p.op("dve", lambda e, gt=gt, j=j: e.scalar_tensor_tensor(out=xs[:], in0=gt[:], scalar=rt[:, n, 16 + j:17 + j], in1=xs[:], op0=ALU.mult, op1=ALU.add), reads=[gt.b, rt.b, xs.b], writes=[xs.b])
                yield
            p.op("act", lambda e: e.activation(out=sq[:], in_=xs[:], func=AF.Square, accum_out=ss[:]), reads=[xs.b], writes=[sq.b, ss.b])
            yield
            p.op("dve", lambda e: e.tensor_scalar(out=rs[:], in0=ss[:], scalar1=1.0 / DM, scalar2=EPS, op0=ALU.mult, op1=ALU.add), reads=[ss.b], writes=[rs.b])
            yield
            p.op("act", lambda e: e.activation(out=rs[:], in_=rs[:], func=AF.Sqrt), reads=[rs.b], writes=[rs.b])
            yield
            p.op("dve", lambda e: e.reciprocal(out=rs[:], in_=rs[:]), reads=[rs.b], writes=[rs.b])
            yield
            p.op("dve", lambda e: e.scalar_tensor_tensor(out=o[:], in0=xs[:], scalar=rs[:], in1=gfin[:], op0=ALU.mult, op1=ALU.mult), reads=[xs.b, rs.b, gfin.b], writes=[o.b])
            p.dma("sp", lambda e: e.dma_start(out=out.ap()[r0:r0 + 128, :], in_=o[:]), reads=[o.b], writes=[Buf()])
            yield

        def fetch(n):
            xs, a, b = xt[n % NB8], g1[n % NB8], g2[n % NB8]
            r0 = n * 128
            p.dma("sp", lambda e: e.dma_start(out=xs[:], in_=x_src.ap()[r0:r0 + 128, :]), writes=[xs.b])
            for j, gt in enumerate((a, b)):
                p.op("act", lambda e, gt=gt: e.memzero(gt[:]), writes=[gt.b])
                p.dma("pool", lambda e, gt=gt, j=j: e.indirect_dma_start(out=gt[:], out_offset=None, in_=yg.ap(), in_offset=bass.IndirectOffsetOnAxis(ap=idx12[:, n, j:j + 1], axis=0),
                                                                        bounds_check=bcreg(e), oob_is_err=False), reads=[idx12.b], writes=[gt.b])

        Ws = [W, env["newW"]()]

        def run_rr(gens):
            gens = list(gens)
            while gens:
                for g_ in list(gens):
                    try:
                        next(g_)
                    except StopIteration:
                        gens.remove(g_)

        for n in range(NB8 - 2):
            fetch(n)
        for n in range(0, NSUB, 2):
            for m_ in (n, n + 1):
                if m_ + NB8 - 2 < NSUB:
                    fetch(m_ + NB8 - 2)
            run_rr([do_tile(n), do_tile(n + 1)])
```

```python
import contextlib
import numpy as np
import ml_dtypes
import concourse.bass as bass
import concourse.mybir as mybir
from concourse.bass_utils import run_bass_kernel_spmd
from concourse.alu_op_type import AluOpType as ALU

AF = mybir.ActivationFunctionType
F32 = mybir.dt.float32
BF16 = mybir.dt.bfloat16
I32 = mybir.dt.int32
U32 = mybir.dt.uint32
AX = mybir.AxisListType

SAME_ENGINE_SYNC = True
NDMA_SLOTS = {"sp": 12, "pool": 6, "act": 4}


class Buf:
    __slots__ = ("w", "r")

    def __init__(self):
        self.w = None
        self.r = {}


class Prog:
    def __init__(self, nc, es):
        self.nc = nc
        self.es = es
        self.eng = {"pe": nc.tensor, "act": nc.scalar, "dve": nc.vector, "pool": nc.gpsimd, "sp": nc.sync}
        self.q = {e: [] for e in self.eng}
        self.sems = []
        self.own = {}
        for e in ("pe", "act", "dve", "pool"):
            self.own[e] = self._newsem("c_" + e)
        self.cnt = {e: 0 for e in self.own}
        self.waited = {e: {} for e in self.eng}
        self.slots = {}
        self.dn = {}
        for qn, k in NDMA_SLOTS.items():
            self.slots[qn] = [[self._newsem("d_%s%d" % (qn, i)), 0] for i in range(k)]
            self.dn[qn] = 0
        self.n_inst = 0

    def _newsem(self, name):
        s = self.es.enter_context(self.nc.semaphore(name))
        self.sems.append(s)
        return len(self.sems) - 1

    def _deps(self, reads, writes):
        deps = {}
        for b in reads:
            if b.w is not None and deps.get(b.w[0], 0) < b.w[1]:
                deps[b.w[0]] = b.w[1]
        for b in writes:
            if b.w is not None and deps.get(b.w[0], 0) < b.w[1]:
                deps[b.w[0]] = b.w[1]
            for s, v in b.r.items():
                if deps.get(s, 0) < v:
                    deps[s] = v
        return deps

    def _waits(self, eng, deps, skip=None):
        wd = self.waited[eng]
        waits = []
        for s, v in deps.items():
            if s == skip:
                continue
            if wd.get(s, 0) >= v:
                continue
            wd[s] = v
            waits.append((s, v))
        return waits

    def _mark(self, ev, reads, writes):
        s, v = ev
        for b in reads:
            if b.r.get(s, 0) < v:
                b.r[s] = v
        for b in writes:
            b.w = ev
            b.r = {}

    def op(self, eng, fn, reads=(), writes=()):
        own = self.own[eng]
        deps = self._deps(reads, writes)
        skip = own if (eng == "pe" or not SAME_ENGINE_SYNC) else None
        waits = self._waits(eng, deps, skip)
        self.cnt[eng] += 1
        ev = (own, self.cnt[eng])
        self.q[eng].append((waits, fn, own, 1))
        self._mark(ev, reads, writes)
        self.n_inst += 1

    def dma(self, qn, fn, reads=(), writes=()):
        deps = self._deps(reads, writes)
        sl = self.slots[qn][self.dn[qn] % len(self.slots[qn])]
        self.dn[qn] += 1
        if sl[1] > 0 and deps.get(sl[0], 0) < sl[1]:
            deps[sl[0]] = sl[1]
        waits = self._waits(qn, deps)
        sl[1] += 16
        ev = (sl[0], sl[1])
        self.q[qn].append((waits, fn, sl[0], 16))
        self._mark(ev, reads, writes)
        self.n_inst += 1

    def finish(self):
        waits = []
        for qn in self.slots:
            for s, v in self.slots[qn]:
                if v > 0:
                    waits.append((s, v))
        for e in self.own:
            if self.cnt[e] > 0:
                waits.append((self.own[e], self.cnt[e]))
        self.q["sp"].append((waits, None, None, 0))

    def barrier(self):
        allw = {}
        for qn in self.slots:
            for s, v in self.slots[qn]:
                if v > 0:
                    allw[s] = v
        for e in self.own:
            if self.cnt[e] > 0:
                allw[self.own[e]] = self.cnt[e]
        for eng in self.eng:
            waits = self._waits(eng, allw)
            if waits:
                self.q[eng].append((waits, None, None, 0))

    @contextlib.contextmanager
    def phase(self):
        old = self.es
        with contextlib.ExitStack() as pes:
            self.es = pes
            yield
            self.barrier()
            self.emit()
        self.es = old

    def simulate(self, q):
        if not hasattr(self, "_simval"):
            self._simval = {}
        val = self._simval
        pos = {e: 0 for e in q}
        progress = True
        while progress:
            progress = False
            for e in q:
                while pos[e] < len(q[e]):
                    waits, fn, s_, inc = q[e][pos[e]]
                    if any(val.get(ws, 0) < wv for ws, wv in waits):
                        break
                    if fn is not None:
                        val[s_] = val.get(s_, 0) + inc
                    pos[e] += 1
                    progress = True
        stuck = {e: pos[e] for e in q if pos[e] < len(q[e])}
        if stuck:
            msg = []
            for e, i in stuck.items():
                waits = q[e][i][0]
                msg.append("%s@%d/%d waits %s have %s" % (e, i, len(q[e]), waits, [val.get(ws, 0) for ws, _ in waits]))
            raise RuntimeError("DEADLOCK in emitted program: " + "; ".join(msg))

    def emit(self):
        nc = self.nc
        sems = self.sems
        q = self.q
        self.q = {e: [] for e in self.eng}
        self.simulate(q)
        with nc.Block() as block:
            def mk(ename):
                def body(e):
                    for waits, fn, s, inc in q[ename]:
                        for ws, wv in waits:
                            e.wait_ge(sems[ws], wv)
                        if fn is not None:
                            fn(e).then_inc(sems[s], inc)
                return body
            block.tensor(mk("pe"))
            block.scalar(mk("act"))
            block.vector(mk("dve"))
            block.gpsimd(mk("pool"))
            block.sync(mk("sp"))


class T:
    _n = [0]

    def __init__(self, p, name, shape, dtype, psum=False):
        T._n[0] += 1
        name = "%s_%d" % (name, T._n[0])
        if psum:
            self.t = p.es.enter_context(p.nc.psum_tensor(name, shape, dtype))
        else:
            self.t = p.es.enter_context(p.nc.sbuf_tensor(name, shape, dtype))
        self.b = Buf()

    def __getitem__(self, k):
        return self.t[k]


class D:
    def __init__(self, nc, name, shape, dtype, kind="Internal"):
        self.t = nc.dram_tensor(name, list(shape), dtype, kind=kind)
        self.bufs = {}

    @classmethod
    def wrap(cls, handle):
        o = cls.__new__(cls)
        o.t = handle
        o.bufs = {}
        return o

    def b(self, key=0):
        if key not in self.bufs:
            self.bufs[key] = Buf()
        return self.bufs[key]

    def ap(self):
        return self.t.ap()


TT = 4096
DM = 1024
NSUB = TT // 128
EVW = 4608
DFF = 2816
DEXP = 3584
NEXP = 8
CAP = 1280
SB_FILL = 0
SB_WB = 32
EPS = 1e-6


def host_consts():
    c = {}
    c["ident"] = np.eye(128, dtype=ml_dtypes.bfloat16)
    c["identf"] = np.eye(128, dtype=np.float32)
    inv = (1.0 / (np.float32(10000.0) ** (np.arange(0, 128, 2, dtype=np.float32) / np.float32(128)))).astype(np.float32)
    ang = (np.arange(TT, dtype=np.float32)[None, :] * inv[:, None]).astype(np.float32)
    cos = np.cos(ang.astype(np.float64)).astype(np.float32)
    sin = np.sin(ang.astype(np.float64)).astype(np.float32)
    c["cosT"] = np.concatenate([cos, cos], 0)
    c["sinT"] = np.concatenate([-sin, sin], 0)
    g = 1.0 - 2.0 ** (-5.0 - np.arange(4, dtype=np.float64))
    i = np.arange(128)
    jj, ii = np.meshgrid(i, i, indexing="ij")
    same = (jj // 64) == (ii // 64)
    causal2 = (jj < 64) & (ii >= 64)
    m = np.zeros((128, 4, 128), np.float64)
    for h in range(4):
        m[:, h, :] = np.where(same | causal2, g[h] ** np.abs(ii - jj), 0.0) * 128 ** -0.5
    c["rmask"] = m.astype(np.float32)
    qd = np.zeros((128, 4, 512), np.float64)
    for h in range(4):
        qd[:, h, :] = (g[h] ** ((np.arange(512) % 128) + 1.0))[None, :]
    c["qdec"] = qd.astype(np.float32)
    kd = np.zeros((128, 4), np.float64)
    for h in range(4):
        kd[:, h] = g[h] ** (127.0 - i) * 128 ** -0.5
    c["kdec"] = kd.astype(np.float32)
    c["_cd"] = [float(g[h] ** 128) for h in range(4)]
    sm = np.zeros((128, 4, 512), np.float32)
    s = np.arange(128)[:, None]
    t = np.arange(512)[None, :]
    for r in range(4):
        sm[:, r, :] = ((r * 128 + s) < t)
    c["sbmask"] = sm.astype(ml_dtypes.bfloat16)
    c["negtri"] = (-(jj >= ii).astype(np.float32)).astype(ml_dtypes.bfloat16)
    c["negones"] = (-np.ones((128, 128), np.float32)).astype(ml_dtypes.bfloat16)
    c["negcomp"] = (-(jj < ii).astype(np.float32)).astype(ml_dtypes.bfloat16)
    c["triex"] = ((jj < ii).astype(np.float32)).astype(ml_dtypes.bfloat16)
    c["ones"] = np.ones((128, 128), ml_dtypes.bfloat16)
    c["eoff"] = np.tile((np.arange(8, dtype=np.float32) * CAP)[None, :], (128, 1))
    return c


CONST_DT = {"ident": BF16, "identf": F32, "cosT": F32, "sinT": F32, "rmask": F32, "qdec": F32, "kdec": F32,
            "sbmask": BF16, "negtri": BF16, "negones": BF16, "negcomp": BF16, "triex": BF16, "ones": BF16, "eoff": F32}

IN_SHAPES = {
    "x": ([TT, DM], F32), "norms": ([5, DM], F32),
    "ev_w_in": ([DM, EVW], F32), "ev_gn": ([1, 512], F32), "ev_w_out": ([DM, DM], F32),
    "ev_gate": ([DM, DFF], F32), "ev_up": ([DM, DFF], F32), "ev_down": ([DFF, DM], F32),
    "od_w_in": ([DM, 2560], F32), "od_small": ([128, 4, 12], F32), "od_wa": ([128, 4, 128], F32),
    "od_wx": ([128, 4, 128], F32), "od_w_out": ([DM, DM], F32), "od_rw": ([DM, 8], F32), "od_rb": ([1, 8], F32),
    "od_eg": ([NEXP, DM, DEXP], F32), "od_eu": ([NEXP, DM, DEXP], F32), "od_ed": ([NEXP, DEXP, DM], F32),
}


def rmsnorm_tile(p, W, xt, gt, ht, tag=""):
    sq, ss, rs = W["sq"], W["ss"], W["rs"]
    p.op("act", lambda e: e.activation(out=sq[:], in_=xt[:], func=AF.Square, accum_out=ss[:]), reads=[xt.b], writes=[sq.b, ss.b])
    p.op("dve", lambda e: e.tensor_scalar(out=rs[:], in0=ss[:], scalar1=1.0 / DM, scalar2=EPS, op0=ALU.mult, op1=ALU.add), reads=[ss.b], writes=[rs.b])
    p.op("act", lambda e: e.activation(out=rs[:], in_=rs[:], func=AF.Sqrt), reads=[rs.b], writes=[rs.b])
    p.op("dve", lambda e: e.reciprocal(out=rs[:], in_=rs[:]), reads=[rs.b], writes=[rs.b])
    p.op("dve", lambda e: e.scalar_tensor_tensor(out=ht[:], in0=xt[:], scalar=rs[:], in1=gt[:], op0=ALU.mult, op1=ALU.mult), reads=[xt.b, rs.b, gt.b], writes=[ht.b])


def build(upto=99, taps=()):
    nc = bass.Bass("TRN2", target_bir_lowering=False)
    cs = host_consts()
    cd = cs.pop("_cd")
    shp = dict(IN_SHAPES)
    if upto < 7:
        for k in ("od_eg", "od_eu", "od_ed"):
            shp[k] = ([1, 8, 8], F32)
    IN = {k: nc.dram_tensor(k, v[0], v[1], kind="ExternalInput") for k, v in shp.items()}
    CI = {k: nc.dram_tensor("c_" + k, list(cs[k].shape), CONST_DT[k], kind="ExternalInput") for k in cs}
    out = D(nc, "out", [TT, DM], F32, kind="ExternalOutput")

    def scratch(name, shape, dt):
        return D(nc, name, shape, dt, kind="ExternalOutput" if name in taps else "Internal")

    sb_qT = scratch("sb_qT", [512, TT], BF16)
    sb_kT = scratch("sb_kT", [512, TT], BF16)
    sb_v = scratch("sb_v", [TT, 512], BF16)
    ymixT = scratch("ymixT", [DM, TT], BF16)
    x1 = scratch("x1", [TT, DM], F32)
    x2 = scratch("x2", [TT, DM], F32)
    ymix1T = scratch("ymix1T", [DM, TT], BF16)
    x3 = scratch("x3", [TT, DM], F32)
    hn = scratch("hn", [TT, DM], BF16)
    rinfo = scratch("rinfo", [TT, 8], F32)
    ffn_bf = [scratch("ffn_gate_bf", [DM, DFF], BF16), scratch("ffn_up_bf", [DM, DFF], BF16), scratch("ffn_down_bf", [DFF, DM], BF16)]
    xg = scratch("xg", [NEXP * CAP, DM], BF16)
    yg = scratch("yg", [NEXP * CAP, DM], F32)

    with contextlib.ExitStack() as es:
        p = Prog(nc, es)
        class CL:
            def __init__(self):
                self.c = {}

            def __getitem__(self, k):
                if k not in self.c:
                    t = T(p, "k_" + k, list(cs[k].shape), CONST_DT[k])
                    p.dma("sp", lambda e: e.dma_start(out=t[:], in_=CI[k].ap()), writes=[t.b])
                    self.c[k] = t
                return self.c[k]

        class GL:
            def __init__(self):
                self.c = {}

            def __getitem__(self, i):
                if i not in self.c:
                    g = T(p, "g%d" % i, [128, DM], F32)
                    p.dma("sp", lambda e: e.dma_start(out=g[:], in_=IN["norms"].ap()[i:i + 1, :].partition_broadcast(128)), writes=[g.b])
                    self.c[i] = g
                return self.c[i]

        def newW():
            return {"sq": T(p, "n_sq", [128, DM], F32), "ss": T(p, "n_ss", [128, 1], F32), "rs": T(p, "n_rs", [128, 1], F32)}
        env = {"CL": CL, "GL": GL, "newW": newW, "CI": CI}
        xin = D.wrap(IN["x"])

        if upto >= 1:
            phase1(p, IN, env, cd, sb_qT, sb_kT, sb_v, ymixT, xin)
        if upto >= 2:
            phase2(p, IN, env, sb_qT, sb_kT, sb_v, ymixT, wcast=list(zip(("ev_gate", "ev_up", "ev_down"), ffn_bf)), xg=xg)
        if upto >= 3:
            phase3(p, IN, env, ymixT, xin, x1, "ev_w_out", lambda it: [("r", it)] + [("s", pr_, it) for pr_ in range(4)])
        with contextlib.ExitStack() as s45:
            old_es, p.es = p.es, s45
            PW = p5_weights_alloc(p)
            p.es = old_es
            if upto >= 4:
                phase4(p, IN, env, x1, x2, pre_hook=(lambda: p5_weights_load(p, IN, PW)) if upto >= 5 else None, wbf=ffn_bf)
            if upto >= 5:
                phase5(p, IN, env, x2, x3, PW)
        R = {"rt": T(p, "rt", [128, NSUB, 20], F32), "idx12": T(p, "idx12", [128, NSUB, 2], I32)}
        if upto >= 6:
            phase6(p, IN, env, x3, xg, R)
        if upto >= 7:
            phase7(p, IN, env, xg, yg)
        if upto >= 8:
            phase8(p, IN, env, x3, yg, out, R)
        p.finish()
    return nc, cs


def phase1(p, IN, env, cd, sb_qT, sb_kT, sb_v, ymixT, x_src):
    with p.phase():
        C, G, W = env["CL"](), env["GL"](), env["newW"]()
        win = T(p, "win", [128, 8, EVW], BF16)
        wb = [Buf() for _ in range(9)]
        for cg in (0, 7, 1, 8, 4, 5, 2, 3, 6):
            p.dma("pool", lambda e, cg=cg: e.dma_start(out=win[:, :, cg * 512:(cg + 1) * 512],
                  in_=IN["ev_w_in"].ap()[:, cg * 512:(cg + 1) * 512].rearrange("(k p) n -> p k n", p=128)), writes=[wb[cg]])
        gnb = T(p, "gnb", [128, 512], F32)
        p.dma("sp", lambda e: e.dma_start(out=gnb[:], in_=IN["ev_gn"].ap().partition_broadcast(128)), writes=[gnb.b])
        xt = [T(p, "xt%d" % i, [128, DM], F32) for i in range(2)]
        ht = [T(p, "ht%d" % i, [128, DM], BF16) for i in range(2)]
        hT = [T(p, "hT%d" % i, [128, 8, 512], BF16) for i in range(2)]
        hTb = [[Buf() for _ in range(4)] for _ in range(2)]
        pT = T(p, "pT", [128, 8, 128], BF16, psum=True)
        pF = [T(p, "pF%d" % i, [128, 512], F32, psum=True) for i in range(2)]
        pM = T(p, "pM", [128, 512], F32, psum=True)
        pS = T(p, "pS", [128, 4, 128], F32, psum=True)
        pK = T(p, "pK", [128, 4, 128], BF16, psum=True)
        pOs = [T(p, "pO%d" % i, [128, 4, 128], F32, psum=True) for i in range(2)]
        pKV = pS
        t1 = [T(p, "t1_%d" % i, [128, 512], F32) for i in range(2)]
        t2 = [T(p, "t2_%d" % i, [128, 512], F32) for i in range(2)]
        qT = [T(p, "qT%d" % h, [128, 512], BF16) for h in range(4)]
        qdT = [T(p, "qdT%d" % h, [128, 512], BF16) for h in range(4)]
        kT = [T(p, "kT%d" % h, [128, 512], BF16) for h in range(4)]
        sbq = [T(p, "sbq0", [128, 4, 512], BF16)] * 2
        sbk = [T(p, "sbk0", [128, 4, 512], BF16)] * 2
        svt = [T(p, "svt0", [128, 4, 512], BF16)] * 2
        cstt = [T(p, "cst%d" % i, [128, 512], F32) for i in range(2)]
        sntt = [T(p, "snt%d" % i, [128, 512], F32) for i in range(2)]
        vt = [T(p, "vt%d" % i, [128, 512], BF16) for i in range(4)]
        Gt = [T(p, "Gt%d" % i, [128, 512], F32) for i in range(4)]
        Pt = T(p, "Pt", [128, 4, 128], BF16)
        kd = T(p, "kd", [128, 4, 128], BF16)
        st = T(p, "st", [128, 4, 128], F32)
        stbf = T(p, "stbf", [128, 4, 128], BF16)
        bst = T(p, "bst", [128, 4, 6], F32)
        mv = T(p, "mv", [128, 4, 2], F32)
        rstd = T(p, "rstd", [128, 4], F32)
        nb = T(p, "nb", [128, 4], F32)
        on = T(p, "on", [128, 512], F32)
        yr = T(p, "yr", [128, 512], BF16)
        yT = [T(p, "yT%d" % i, [128, 4, 512], BF16) for i in range(2)]
        ident = C["ident"]

        p.op("pool", lambda e: e.memset(st[:], 0.0), writes=[st.b])
        p.op("pool", lambda e: e.memset(stbf[:], 0.0), writes=[stbf.b])

        def wcols(c0, n):
            return [wb[i] for i in range(c0 // 512, (c0 + n - 1) // 512 + 1)]

        def fm_proj(ps, c0, hTt, hbs):
            for k in range(8):
                p.op("pe", lambda e, k=k: e.matmul(ps[:], lhsT=win[:, k, c0:c0 + 128], rhs=hTt[:, k, :], start=(k == 0), stop=(k == 7)),
                     reads=wcols(c0, 128) + hbs, writes=[ps.b])

        def run_rr(gens):
            gens = list(gens)
            while gens:
                for g_ in list(gens):
                    try:
                        next(g_)
                    except StopIteration:
                        gens.remove(g_)

        def A_gen(it):
            par = it % 2
            t0 = it * 512

            def nrm(sub):
                xs = xt[sub % 2]
                hs = ht[sub % 2]
                r0 = t0 + sub * 128
                p.dma("sp", lambda e, xs=xs, r0=r0: e.dma_start(out=xs[:], in_=x_src.ap()[r0:r0 + 128, :]), reads=[x_src.b(r0 // 128)], writes=[xs.b])
                rmsnorm_tile(p, W, xs, G[0], hs)

            def trs(sub):
                hs = ht[sub % 2]
                for k in range(8):
                    p.op("pe", lambda e, k=k, hs=hs: e.transpose(out=pT[:, k, :], in_=hs[:, k * 128:(k + 1) * 128], identity=ident[:]),
                         reads=[hs.b, ident.b], writes=[pT.b])
                p.op("act", lambda e, sub=sub, par=par: e.copy(out=hT[par][:, :, sub * 128:(sub + 1) * 128], in_=pT[:]), reads=[pT.b], writes=[hTb[par][sub]])

            for fn_, sub in ((nrm, 0), (nrm, 1), (trs, 0), (nrm, 2), (trs, 1), (nrm, 3), (trs, 2), (trs, 3)):
                fn_(sub)
                yield

        def do_tile(it):
            par = it % 2
            t0 = it * 512
            hTt = hT[par]
            hbs = hTb[par]
            cst, snt = cstt[par], sntt[par]
            p.dma("sp", lambda e, cst=cst: e.dma_start(out=cst[:], in_=env["CI"]["cosT"].ap()[:, t0:t0 + 512]), writes=[cst.b])
            p.dma("sp", lambda e, snt=snt: e.dma_start(out=snt[:], in_=env["CI"]["sinT"].ap()[:, t0:t0 + 512]), writes=[snt.b])
            cosv, sinv = cst[:], snt[:]
            for h in range(4):
                for which in range(2):
                    cbase = which * 512 + h * 128
                    sbase = 3584 + which * 512 + h * 128
                    a, b = t1[which], t2[which]
                    fm_proj(pF[0], cbase, hTt, hbs)
                    p.op("dve", lambda e, a=a: e.tensor_tensor(out=a[:], in0=pF[0][:], in1=cosv, op=ALU.mult), reads=[pF[0].b, cst.b], writes=[a.b])
                    fm_proj(pF[1], sbase, hTt, hbs)
                    p.op("dve", lambda e, b=b: e.tensor_tensor(out=b[:], in0=pF[1][:], in1=sinv, op=ALU.mult), reads=[pF[1].b, snt.b], writes=[b.b])
                    if which == 0:
                        p.op("pool", lambda e, a=a, b=b: e.tensor_tensor(out=a[:], in0=a[:], in1=b[:], op=ALU.add), reads=[a.b, b.b], writes=[a.b])
                        p.op("dve", lambda e, a=a, h=h: e.tensor_copy(out=qT[h][:], in_=a[:]), reads=[a.b], writes=[qT[h].b])
                        p.op("pool", lambda e, a=a, h=h: e.tensor_tensor(out=qdT[h][:], in0=a[:], in1=C["qdec"][:, h, :], op=ALU.mult), reads=[a.b, C["qdec"].b], writes=[qdT[h].b])
                    else:
                        p.op("dve", lambda e, a=a, b=b, h=h: e.tensor_tensor(out=kT[h][:], in0=a[:], in1=b[:], op=ALU.add), reads=[a.b, b.b], writes=[kT[h].b])
            def C_gen():
                for which, (stg, dst) in enumerate(((sbq[par], sb_qT), (sbk[par], sb_kT))):
                    for g in range(4):
                        ps = pF[g % 2]
                        fm_proj(ps, 2048 + which * 512 + g * 128, hTt, hbs)
                        p.op("act", lambda e, ps=ps, stg=stg, g=g: e.copy(out=stg[:, g, :], in_=ps[:]), reads=[ps.b], writes=[stg.b])
                        yield
                    p.dma("sp", lambda e, stg=stg, dst=dst: e.dma_start(out=dst.ap()[:, t0:t0 + 512].rearrange("(g p) t -> p g t", p=128), in_=stg[:]),
                          reads=[stg.b], writes=[dst.b(it)])
            def D_gen(sub):
                for (c0, kind) in ((1024, "v"), (1536, "g"), (3072, "sv")):
                    for k in range(8):
                        p.op("pe", lambda e, k=k, sub=sub, c0=c0: e.matmul(pM[:], lhsT=hTt[:, k, sub * 128:(sub + 1) * 128], rhs=win[:, k, c0:c0 + 512], start=(k == 0), stop=(k == 7)),
                             reads=wcols(c0, 512) + [hbs[sub]], writes=[pM.b])
                    if kind == "v":
                        p.op("act", lambda e, sub=sub: e.copy(out=vt[sub][:], in_=pM[:]), reads=[pM.b], writes=[vt[sub].b])
                    elif kind == "g":
                        p.op("act", lambda e, sub=sub: e.activation(out=Gt[sub][:], in_=pM[:], func=AF.Silu), reads=[pM.b], writes=[Gt[sub].b])
                        p.op("pool", lambda e, sub=sub: e.tensor_tensor(out=Gt[sub][:], in0=Gt[sub][:], in1=gnb[:], op=ALU.mult), reads=[Gt[sub].b, gnb.b], writes=[Gt[sub].b])
                    else:
                        p.op("act", lambda e, sub=sub, par=par: e.copy(out=svt[par][:, sub, :], in_=pM[:]), reads=[pM.b], writes=[svt[par].b])
                    yield
                if sub == 3:
                    p.dma("sp", lambda e, par=par: e.dma_start(out=sb_v.ap()[t0:t0 + 512, :].rearrange("(s p) c -> p s c", p=128), in_=svt[par][:]),
                          reads=[svt[par].b], writes=[sb_v.b(it)])
            def E1_gen(sub):
                sl = slice(sub * 128, (sub + 1) * 128)
                pO = pOs[sub % 2]
                for h in range(4):
                    p.op("pe", lambda e, h=h, sl=sl: e.matmul(pS[:, h, :], lhsT=kT[h][:, sl], rhs=qT[h][:, sl], start=True, stop=True, skip_group_check=True),
                         reads=[kT[h].b, qT[h].b], writes=[pS.b])
                yield
                p.op("dve", lambda e: e.tensor_tensor(out=Pt[:], in0=pS[:], in1=C["rmask"][:], op=ALU.mult), reads=[pS.b, C["rmask"].b], writes=[Pt.b])
                yield
                for h in range(4):
                    p.op("pe", lambda e, h=h, sl=sl: e.transpose(out=pK[:, h, :], in_=kT[h][:, sl], identity=ident[:]), reads=[kT[h].b, ident.b], writes=[pK.b])
                yield
                for h in range(4):
                    p.op("dve", lambda e, h=h: e.tensor_scalar(out=kd[:, h, :], in0=pK[:, h, :], scalar1=C["kdec"][:, h:h + 1], scalar2=None, op0=ALU.mult),
                         reads=[pK.b, C["kdec"].b], writes=[kd.b])
                yield
                for h in range(4):
                    hs_ = slice(h * 128, (h + 1) * 128)
                    p.op("pe", lambda e, h=h, hs_=hs_, sub=sub: e.matmul(pO[:, h, :], lhsT=Pt[:, h, :], rhs=vt[sub][:, hs_], start=True, stop=False, skip_group_check=True),
                         reads=[Pt.b, vt[sub].b], writes=[pO.b])
                    p.op("pe", lambda e, h=h, sl=sl: e.matmul(pO[:, h, :], lhsT=qdT[h][:, sl], rhs=stbf[:, h, :], start=False, stop=True, skip_group_check=True),
                         reads=[qdT[h].b, stbf.b], writes=[pO.b])
                yield
                for h in range(4):
                    hs_ = slice(h * 128, (h + 1) * 128)
                    p.op("pe", lambda e, h=h, hs_=hs_, sub=sub: e.matmul(pKV[:, h, :], lhsT=kd[:, h, :], rhs=vt[sub][:, hs_], start=True, stop=True, skip_group_check=True),
                         reads=[kd.b, vt[sub].b], writes=[pKV.b])
                yield
                for h in range(4):
                    p.op("dve", lambda e, h=h: e.scalar_tensor_tensor(out=st[:, h, :], in0=st[:, h, :], scalar=cd[h], in1=pKV[:, h, :], op0=ALU.mult, op1=ALU.add),
                         reads=[st.b, pKV.b], writes=[st.b])
                yield
                p.op("act", lambda e: e.copy(out=stbf[:], in_=st[:]), reads=[st.b], writes=[stbf.b])
                yield

            def E2_gen(sub):
                sl = slice(sub * 128, (sub + 1) * 128)
                pO = pOs[sub % 2]
                for h in range(4):
                    p.op("dve", lambda e, h=h: e.bn_stats(out=bst[:, h, :], in_=pO[:, h, :]), reads=[pO.b], writes=[bst.b])
                yield
                for h in range(4):
                    p.op("dve", lambda e, h=h: e.bn_aggr(out=mv[:, h, :], in_=bst[:, h, :]), reads=[bst.b], writes=[mv.b])
                yield
                p.op("dve", lambda e: e.tensor_scalar(out=rstd[:], in0=mv[:, :, 1], scalar1=EPS, scalar2=None, op0=ALU.add), reads=[mv.b], writes=[rstd.b])
                yield
                p.op("act", lambda e: e.activation(out=rstd[:], in_=rstd[:], func=AF.Sqrt), reads=[rstd.b], writes=[rstd.b])
                yield
                p.op("dve", lambda e: e.reciprocal(out=rstd[:], in_=rstd[:]), reads=[rstd.b], writes=[rstd.b])
                yield
                p.op("dve", lambda e: e.scalar_tensor_tensor(out=nb[:], in0=mv[:, :, 0], scalar=-1.0, in1=rstd[:], op0=ALU.mult, op1=ALU.mult), reads=[mv.b, rstd.b], writes=[nb.b])
                yield
                for h in range(4):
                    p.op("act", lambda e, h=h: e.activation(out=on[:, h * 128:(h + 1) * 128], in_=pO[:, h, :], func=AF.Identity, scale=rstd[:, h:h + 1], bias=nb[:, h:h + 1]),
                         reads=[pO.b, rstd.b, nb.b], writes=[on.b])
                yield
                p.op("dve", lambda e, sub=sub: e.tensor_tensor(out=yr[:], in0=on[:], in1=Gt[sub][:], op=ALU.mult), reads=[on.b, Gt[sub].b], writes=[yr.b])
                yield
                for c in range(4):
                    p.op("pe", lambda e, c=c: e.transpose(out=pT[:, c, :], in_=yr[:, c * 128:(c + 1) * 128], identity=ident[:]), reads=[yr.b, ident.b], writes=[pT.b])
                p.op("act", lambda e, sl=sl, par=par: e.copy(out=yT[par][:, :, sl], in_=pT[:, 0:4, :]), reads=[pT.b], writes=[yT[par].b])
                yield
                if sub == 3:
                    p.dma("sp", lambda e, par=par: e.dma_start(out=ymixT.ap()[0:512, t0:t0 + 512].rearrange("(c p) t -> p c t", p=128), in_=yT[par][:]),
                          reads=[yT[par].b], writes=[ymixT.b(("r", it))])

            run_rr([D_gen(0)])
            run_rr([E1_gen(0), D_gen(1), C_gen()])
            run_rr([E2_gen(0), E1_gen(1), D_gen(2)] + ([A_gen(it + 1)] if it + 1 < TT // 512 else []))
            run_rr([E2_gen(1), E1_gen(2), D_gen(3)])
            run_rr([E2_gen(2), E1_gen(3)])
            run_rr([E2_gen(3)])

        run_rr([A_gen(0)])
        for it in range(TT // 512):
            do_tile(it)


def prep_shared(inp):
    f = lambda a: np.ascontiguousarray(np.asarray(a, dtype=np.float32))
    d = {}
    d["norms"] = f(np.concatenate([inp["norm_mix"][0:1], inp["norm_ffn"][0:1], inp["norm_mix"][1:2], inp["norm_ffn"][1:2], inp["norm_final"][None, :]], 0))
    w = np.asarray(inp["ev_w_in"][0])
    swap = np.concatenate([np.arange(h * 128 + 64, h * 128 + 128).tolist() + np.arange(h * 128, h * 128 + 64).tolist() for h in range(4)]).astype(np.int64)
    d["ev_w_in"] = f(np.concatenate([w, w[:, 0:512][:, swap], w[:, 512:1024][:, swap]], 1))
    d["ev_gn"] = f(inp["ev_ret_gn"][0:1])
    d["ev_w_out"] = f(inp["ev_w_out"][0])
    d["ev_gate"] = f(inp["ev_ffn_gate"][0])
    d["ev_up"] = f(inp["ev_ffn_up"][0])
    d["ev_down"] = f(inp["ev_ffn_down"][0])
    d["od_w_in"] = f(inp["od_w_in"][0])
    sm = np.zeros((512, 12), np.float32)
    sm[:, 0:3] = np.asarray(inp["od_conv_w"][0]).T
    sm[:, 3:7] = np.asarray(inp["od_lru_conv_w"][0]).T
    sm[:, 7] = np.asarray(inp["od_lru_conv_b"][0])
    sm[:, 8] = np.asarray(inp["od_lru_ba"][0])
    sm[:, 9] = np.asarray(inp["od_lru_bx"][0])
    sm[:, 10] = np.asarray(inp["od_lru_lambda"][0])
    d["od_small"] = f(sm.reshape(4, 128, 12).transpose(1, 0, 2))
    for nm, key in (("od_wa", "od_lru_wa"), ("od_wx", "od_lru_wx")):
        wsrc = np.asarray(inp[key][0])
        bd = np.zeros((128, 4, 128), np.float32)
        for hh in range(8):
            c, o = hh // 2, (hh % 2) * 64
            bd[o:o + 64, c, o:o + 64] = wsrc[hh]
        d[nm] = bd
    d["od_w_out"] = f(inp["od_w_out"][0])
    d["od_rw"] = f(inp["od_router_w"][0])
    d["od_rb"] = f(inp["od_router_b"][0:1])
    d["od_eg"] = f(inp["od_exp_gate"][0])
    d["od_eu"] = f(inp["od_exp_up"][0])
    d["od_ed"] = f(inp["od_exp_down"][0])
    return d


_CACHE = {}


def kernel(**inputs):
    if "nc" not in _CACHE:
        _CACHE["nc"] = build()
    nc, cs = _CACHE["nc"]
    shared = prep_shared(inputs)
    for k, v in cs.items():
        shared["c_" + k] = v
    x = np.asarray(inputs["x"], dtype=np.float32)
    in_maps = []
    for c in range(8):
        m = dict(shared)
        m["x"] = np.ascontiguousarray(x[c])
        in_maps.append(m)
    res = run_bass_kernel_spmd(nc, in_maps, core_ids=list(range(8)))
    return np.stack([np.asarray(r["out"]) for r in res.results], 0).astype(np.float32)


def phase2(p, IN, env, sb_qT, sb_kT, sb_v, ymixT, wcast=None, xg=None):
    with p.phase():
        C = env["CL"]()
        sbmask, negtri, negcomp = C["sbmask"], C["negtri"], C["negcomp"]
        vall = T(p, "vall", [128, NSUB, 512], BF16)
        p.dma("sp", lambda e: e.dma_start(out=vall[:], in_=sb_v.ap().rearrange("(n p) c -> p n c", p=128)), writes=[vall.b])
        qz = [[T(p, "qz%d_%d" % (i, hh), [128, TT], BF16) for hh in range(2)] for i in range(2)]
        for i in range(2):
            for hh in range(2):
                o = (1 - hh) * 64
                p.op("pool", lambda e, i=i, hh=hh, o=o: e.memset(qz[i][hh][o:o + 64, :], 0.0), writes=[qz[i][hh].b])
        kp = [T(p, "kp%d" % i, [128, TT], BF16) for i in range(2)]
        pZ = [T(p, "pZ%d" % i, [128, 2, 512], F32, psum=True) for i in range(2)]
        pB = T(p, "pB", [128, 2, 512], F32, psum=True)
        pOo = T(p, "pOo", [128, 2, 512], F32, psum=True)
        NE, NS, NX, NA = 6, 4, 3, 3
        Et = [T(p, "Et%d" % i, [128, 2, 512], F32) for i in range(NE)]
        SPt = [T(p, "SPt%d" % i, [128, 2, 512], BF16) for i in range(NS)]
        Xt = [T(p, "Xt%d" % i, [128, 2, 512], F32) for i in range(NX)]
        At = [T(p, "At%d" % i, [128, 2, 512], BF16) for i in range(NA)]
        ost = [T(p, "ost%d" % i, [128, 512], BF16) for i in range(2)]
        tiles = []
        g = 0
        for pair in range(4):
            for Q in range(8):
                kmax = 4 * Q + 3
                kmin = max(0, 4 * Q - SB_WB)
                for kb in range(kmax, kmin - 1, -1):
                    tiles.append(dict(pair=pair, Q=Q, kb=kb, first=(kb == kmax), last=(kb == kmin), r=kb - 4 * Q, g=g, newpair=(Q == 0 and kb == kmax)))
                g += 1

        def S0(i, t):
            pair, kb, Q = t["pair"], t["kb"], t["Q"]
            kt = kp[pair % 2]
            w0 = max(t["r"], 0) * 128
            if t["newpair"]:
                for hh in range(2):
                    qh = qz[pair % 2][hh]
                    p.dma("sp", lambda e, hh=hh, qh=qh: e.dma_start(out=qh[hh * 64:(hh + 1) * 64, :], in_=sb_qT.ap()[pair * 128 + hh * 64:pair * 128 + (hh + 1) * 64, :]), writes=[qh.b])
                p.dma("sp", lambda e: e.dma_start(out=kt[:], in_=sb_kT.ap()[pair * 128:(pair + 1) * 128, :]), writes=[kt.b])
            z, e_ = pZ[i % 2], Et[i % NE]
            for hh in range(2):
                qt = qz[pair % 2][hh]
                p.op("pe", lambda e, hh=hh, qt=qt: e.matmul(z[:, hh, w0:512], lhsT=kt[:, kb * 128:(kb + 1) * 128], rhs=qt[:, Q * 512 + w0:(Q + 1) * 512], start=True, stop=True),
                     reads=[kt.b, qt.b], writes=[z.b])
            p.op("act", lambda e: e.activation(out=e_[:, :, w0:512], in_=z[:, :, w0:512], func=AF.Exp, scale=0.125), reads=[z.b], writes=[e_.b])

        def S1(i, t):
            e_, sp_ = Et[i % NE], SPt[i % NS]
            r = t["r"]
            w0 = max(r, 0) * 128
            p.op("act", lambda e: e.activation(out=sp_[:, :, w0:512], in_=e_[:, :, w0:512], func=AF.Ln, bias=1.0), reads=[e_.b], writes=[sp_.b])
            if r >= 0:
                for hh in range(2):
                    p.op("pool", lambda e, hh=hh: e.tensor_tensor(out=sp_[:, hh, w0:w0 + 128], in0=sp_[:, hh, w0:w0 + 128], in1=sbmask[:, r, w0:w0 + 128], op=ALU.mult), reads=[sp_.b, sbmask.b], writes=[sp_.b])

        def S2a(i, t):
            sp_ = SPt[i % NS]
            w0 = max(t["r"], 0) * 128
            for hh in range(2):
                p.op("pe", lambda e, hh=hh: e.matmul(pB[:, hh, w0:512], lhsT=negtri[:], rhs=sp_[:, hh, w0:512], start=t["first"], stop=True, skip_group_check=True), reads=[negtri.b, sp_.b], writes=[pB.b])

        def S2b(i, t):
            x = Xt[i % NX]
            w0 = max(t["r"], 0) * 128
            p.op("act", lambda e: e.activation(out=x[:, :, w0:512], in_=pB[:, :, w0:512], func=AF.Exp), reads=[pB.b], writes=[x.b])

        def S2c(i, t):
            sp_ = SPt[i % NS]
            w0 = max(t["r"], 0) * 128
            if not t["last"]:
                for hh in range(2):
                    p.op("pe", lambda e, hh=hh: e.matmul(pB[:, hh, w0:512], lhsT=negcomp[:], rhs=sp_[:, hh, w0:512], start=False, stop=True, skip_group_check=True), reads=[negcomp.b, sp_.b], writes=[pB.b])

        def S3(i, t):
            e_, x, a = Et[i % NE], Xt[i % NX], At[i % NA]
            r = t["r"]
            w0 = max(r, 0) * 128
            p.op("dve", lambda e: e.tensor_tensor(out=a[:, :, w0:512], in0=e_[:, :, w0:512], in1=x[:, :, w0:512], op=ALU.mult), reads=[e_.b, x.b], writes=[a.b])
            if r >= 0:
                for hh in range(2):
                    p.op("pool", lambda e, hh=hh: e.tensor_tensor(out=a[:, hh, w0:w0 + 128], in0=a[:, hh, w0:w0 + 128], in1=sbmask[:, r, w0:w0 + 128], op=ALU.mult), reads=[a.b, sbmask.b], writes=[a.b])

        def S4(i, t):
            a = At[i % NA]
            osb = ost[t["g"] % 2]
            kb, Q, pair = t["kb"], t["Q"], t["pair"]
            w0 = max(t["r"], 0) * 128
            for hh in range(2):
                p.op("pe", lambda e, hh=hh: e.matmul(pOo[:, hh, w0:512], lhsT=vall[:, kb, pair * 128:(pair + 1) * 128], rhs=a[:, hh, w0:512], start=t["first"], stop=t["last"], skip_group_check=True),
                     reads=[vall.b, a.b], writes=[pOo.b])
            if t["last"]:
                for hh in range(2):
                    p.op("dve", lambda e, hh=hh: e.tensor_copy(out=osb[hh * 64:(hh + 1) * 64, :], in_=pOo[hh * 64:(hh + 1) * 64, hh, :]), reads=[pOo.b], writes=[osb.b])
                p.dma("sp", lambda e: e.dma_start(out=ymixT.ap()[512 + pair * 128:512 + (pair + 1) * 128, Q * 512:(Q + 1) * 512], in_=osb[:]),
                      reads=[osb.b], writes=[ymixT.b(("s", pair, Q))])

        if xg is not None:
            zt = T(p, "zt", [128, CAP // 128, DM], BF16)
            p.op("pool", lambda e: e.memset(zt[:], 0.0), writes=[zt.b])
        NTL = len(tiles)
        order = ((3, S2a), (3, S2b), (0, S0), (5, S4), (3, S2c), (1, S1), (4, S3))
        for s_ in range(NTL + 5):
            if xg is not None and 250 <= s_ < 250 + NEXP * 10 and (s_ - 250) % 10 == 0:
                ex_ = (s_ - 250) // 10
                p.dma("sp", lambda e, ex_=ex_: e.dma_start(out=xg.ap()[ex_ * CAP:(ex_ + 1) * CAP, :].rearrange("(n p) d -> p n d", p=128), in_=zt[:]), reads=[zt.b], writes=[Buf()])
            if wcast is not None and s_ in (30, 100, 170):
                src_name, dst = wcast[(30, 100, 170).index(s_)]
                p.dma("pool", lambda e, src_name=src_name, dst=dst: e.dma_start(out=dst.ap(), in_=IN[src_name].ap()), writes=[Buf()])
            for d_, fn_ in order:
                if 0 <= s_ - d_ < NTL:
                    fn_(s_ - d_, tiles[s_ - d_])


def load_norm_T(p, W, src, r0, g, xs, hs, pT, ident, dstT, c0, dbuf, hdt_fp32=False, part=0):
    if part in (0, 1):
        p.dma("sp", lambda e: e.dma_start(out=xs[:], in_=src.ap()[r0:r0 + 128, :]), reads=[src.b(r0 // 128)], writes=[xs.b])
        rmsnorm_tile(p, W, xs, g, hs)
    if part == 1:
        return
    for k in range(8):
        p.op("pe", lambda e, k=k: e.transpose(out=pT[:, k, :], in_=hs[:, k * 128:(k + 1) * 128], identity=ident[:]), reads=[hs.b, ident.b], writes=[pT.b])
    p.op("act", lambda e: e.copy(out=dstT[:, :, c0:c0 + 128], in_=pT[:]), reads=[pT.b], writes=[dbuf])


def phase3(p, IN, env, ymixT, x_src, x_dst, wname, ykeys, xg=None):
    with p.phase():
        wo = T(p, "wo", [128, 8, DM], BF16)
        p.dma("pool", lambda e: e.dma_start(out=wo[:], in_=IN[wname].ap().rearrange("(k p) n -> p k n", p=128)), writes=[wo.b])
        ym = [T(p, "ym%d" % i, [128, 8, 512], BF16) for i in range(2)]
        xt = [T(p, "xt%d" % i, [128, DM], F32) for i in range(4)]
        xo = [T(p, "xo%d" % i, [128, DM], F32) for i in range(2)]
        pY = [T(p, "pY%d" % i, [128, 512], F32, psum=True) for i in range(4)]

        def ymload(it):
            t0 = it * 512
            ymt = ym[it % 2]
            p.dma("sp", lambda e: e.dma_start(out=ymt[:], in_=ymixT.ap()[:, t0:t0 + 512].rearrange("(k p) t -> p k t", p=128)),
                  reads=[ymixT.b(k) for k in ykeys(it)], writes=[ymt.b])

        def xload(n):
            xs = xt[n % 4]
            r0 = n * 128
            p.dma("sp", lambda e: e.dma_start(out=xs[:], in_=x_src.ap()[r0:r0 + 128, :]), reads=[x_src.b(n)], writes=[xs.b])

        ymload(0)
        for n in range(3):
            xload(n)

        def do_tile(it):
            t0 = it * 512
            ymt = ym[it % 2]
            if it + 1 < TT // 512:
                ymload(it + 1)
            for sub in range(4):
                n = it * 4 + sub
                xs, xos = xt[n % 4], xo[n % 2]
                r0 = t0 + sub * 128
                if n + 3 < NSUB:
                    xload(n + 3)
                for half in range(2):
                    py = pY[(n * 2 + half) % 4]
                    for k in range(8):
                        p.op("pe", lambda e, k=k, py=py, sub=sub, half=half: e.matmul(py[:], lhsT=ymt[:, k, sub * 128:(sub + 1) * 128], rhs=wo[:, k, half * 512:(half + 1) * 512], start=(k == 0), stop=(k == 7)),
                             reads=[ymt.b, wo.b], writes=[py.b])
                    p.op("dve", lambda e, py=py, xs=xs, xos=xos, half=half: e.tensor_tensor(out=xos[:, half * 512:(half + 1) * 512], in0=py[:], in1=xs[:, half * 512:(half + 1) * 512], op=ALU.add),
                         reads=[py.b, xs.b], writes=[xos.b])
                p.dma("sp", lambda e, xos=xos, r0=r0: e.dma_start(out=x_dst.ap()[r0:r0 + 128, :], in_=xos[:]), reads=[xos.b], writes=[x_dst.b(r0 // 128)])

        for it in range(TT // 512):
            do_tile(it)


def ffn_alloc(p, ntok_max):
    FB = {"n": 0, "ln": 0, "j": 0, "jj": 0}
    FB["wg"] = [T(p, "wg%d" % i, [128, 8, 512], BF16) for i in range(2)]
    FB["wu"] = [T(p, "wu%d" % i, [128, 8, 512], BF16) for i in range(2)]
    FB["wd"] = [T(p, "wd%d" % i, [128, 4, DM], BF16) for i in range(2)]
    FB["act"] = [T(p, "actT%d" % i, [128, 4, ntok_max], BF16) for i in range(2)]
    FB["sg"] = [T(p, "sg%d" % i, [128, 512], F32) for i in range(2)]
    FB["pG"] = [T(p, "pG%d" % i, [128, 512], F32, psum=True) for i in range(2)]
    FB["pU"] = [T(p, "pU%d" % i, [128, 512], F32, psum=True) for i in range(2)]
    FB["pY"] = [T(p, "pYf%d" % i, [128, 512], F32, psum=True) for i in range(2)]
    return FB


def ffn_load(p, FB, wg_ap, wu_ap, wd_ap, fs, fsz, wq="pool"):
    i = FB["ln"] % 2
    FB["ln"] += 1
    wg, wu, wd = FB["wg"][i], FB["wu"][i], FB["wd"][i]
    nfc = fsz // 128
    p.dma(wq, lambda e: e.dma_start(out=wg[:, :, 0:fsz], in_=wg_ap[:, fs:fs + fsz].rearrange("(k p) n -> p k n", p=128)), writes=[wg.b])
    p.dma(wq, lambda e: e.dma_start(out=wu[:, :, 0:fsz], in_=wu_ap[:, fs:fs + fsz].rearrange("(k p) n -> p k n", p=128)), writes=[wu.b])
    p.dma(wq, lambda e: e.dma_start(out=wd[:, 0:nfc, :], in_=wd_ap[fs:fs + fsz, :].rearrange("(c p) n -> p c n", p=128)), writes=[wd.b])


def ffn_block(p, FB, xT, xbufs, ntok, wg_ap, wu_ap, wd_ap, F, yacc, ybufs, hooks=(), hooks_early=(), wq="pool", preloaded=False):
    groups = [(fs, min(512, F - fs)) for fs in range(0, F, 512)]
    hooks = list(hooks)
    hooks_early = list(hooks_early)
    late_ready = []
    per = -(-len(hooks) // max(1, len(groups) - 1)) if hooks else 0

    def do_group(gi, fs, fsz):
        if gi == 0 and not preloaded:
            ffn_load(p, FB, wg_ap, wu_ap, wd_ap, fs, fsz, wq)
        if gi + 1 < len(groups):
            ffn_load(p, FB, wg_ap, wu_ap, wd_ap, groups[gi + 1][0], groups[gi + 1][1], wq)
        i = FB["n"] % 2
        FB["n"] += 1
        wg, wu, wd, act = FB["wg"][i], FB["wu"][i], FB["wd"][i], FB["act"][i]
        nfc = fsz // 128
        for tt0 in range(0, ntok, 512):
            tn = min(512, ntok - tt0)
            for fc in range(nfc):
                j = FB["j"] % 2
                FB["j"] += 1
                pg, pu, sg = FB["pG"][j], FB["pU"][j], FB["sg"][j]
                for k in range(8):
                    p.op("pe", lambda e, k=k, pg=pg, fc=fc, tt0=tt0, tn=tn: e.matmul(pg[:, 0:tn], lhsT=wg[:, k, fc * 128:(fc + 1) * 128], rhs=xT[:, k, tt0:tt0 + tn], start=(k == 0), stop=(k == 7)),
                         reads=[wg.b] + xbufs, writes=[pg.b])
                for k in range(8):
                    p.op("pe", lambda e, k=k, pu=pu, fc=fc, tt0=tt0, tn=tn: e.matmul(pu[:, 0:tn], lhsT=wu[:, k, fc * 128:(fc + 1) * 128], rhs=xT[:, k, tt0:tt0 + tn], start=(k == 0), stop=(k == 7)),
                         reads=[wu.b] + xbufs, writes=[pu.b])
                p.op("act", lambda e, pg=pg, sg=sg, tn=tn: e.activation(out=sg[:, 0:tn], in_=pg[:, 0:tn], func=AF.Silu), reads=[pg.b], writes=[sg.b])
                p.op("dve", lambda e, pu=pu, sg=sg, fc=fc, tt0=tt0, tn=tn: e.tensor_tensor(out=act[:, fc, tt0:tt0 + tn], in0=pu[:, 0:tn], in1=sg[:, 0:tn], op=ALU.mult),
                     reads=[pu.b, sg.b], writes=[act.b])
        if hooks_early or late_ready:
            for _ in range(len(late_ready)):
                late_ready.pop(0)()
            for _ in range(per):
                if hooks_early:
                    hooks_early.pop(0)()
                    late_ready.append(hooks.pop(0))
        for ts in range(ntok // 128):
            for half in range(2):
                jj = FB["jj"] % 2
                FB["jj"] += 1
                py = FB["pY"][jj]
                for fc in range(nfc):
                    p.op("pe", lambda e, fc=fc, py=py, ts=ts, half=half: e.matmul(py[:], lhsT=act[:, fc, ts * 128:(ts + 1) * 128], rhs=wd[:, fc, half * 512:(half + 1) * 512], start=(fc == 0), stop=(fc == nfc - 1)),
                         reads=[act.b, wd.b], writes=[py.b])
                if gi == 0:
                    p.op("act", lambda e, py=py, ts=ts, half=half: e.copy(out=yacc[:, ts, half * 512:(half + 1) * 512], in_=py[:]), reads=[py.b], writes=[ybufs[ts]])
                else:
                    p.op("dve", lambda e, py=py, ts=ts, half=half: e.tensor_tensor(out=yacc[:, ts, half * 512:(half + 1) * 512], in0=py[:], in1=yacc[:, ts, half * 512:(half + 1) * 512], op=ALU.add),
                         reads=[py.b, ybufs[ts]], writes=[ybufs[ts]])

    for gi, (fs, fsz) in enumerate(groups):
        do_group(gi, fs, fsz)
    for h_ in late_ready + hooks:
        h_()


def phase4(p, IN, env, x_src, x_dst, pre_hook=None, wbf=None):
    NTK = 1024
    with p.phase():
        C, G, W = env["CL"](), env["GL"](), env["newW"]()
        ident = C["ident"]
        FB = ffn_alloc(p, NTK)
        pT = T(p, "pT", [128, 8, 128], BF16, psum=True)
        xTs = [T(p, "xT%d" % i, [128, 8, NTK], BF16) for i in range(2)]
        xbs = [[Buf() for _ in range(NTK // 128)] for _ in range(2)]
        yacc = T(p, "yacc", [128, NTK // 128, DM], F32)
        yb = [Buf() for _ in range(NTK // 128)]
        xt = [T(p, "xt%d" % i, [128, DM], F32) for i in range(2)]
        xt2 = [T(p, "xtb%d" % i, [128, DM], F32) for i in range(2)]
        ht = [T(p, "ht%d" % i, [128, DM], BF16) for i in range(2)]

        def prep(s, sub, part=0):
            load_norm_T(p, W, x_src, s * NTK + sub * 128, G[1], xt2[sub % 2], ht[sub % 2], pT, ident, xTs[s % 2], sub * 128, xbs[s % 2][sub], part=part)

        for sub in range(NTK // 128):
            prep(0, sub)
        if pre_hook is not None:
            pre_hook()

        def do_super(s):
            nxt = s + 1 < TT // NTK
            hooks_e = [(lambda sub=sub: prep(s + 1, sub, 1)) for sub in range(NTK // 128)] if nxt else []
            hooks_l = [(lambda sub=sub: prep(s + 1, sub, 2)) for sub in range(NTK // 128)] if nxt else []
            waps = (IN["ev_gate"].ap(), IN["ev_up"].ap(), IN["ev_down"].ap()) if wbf is None else (wbf[0].ap(), wbf[1].ap(), wbf[2].ap())
            ffn_block(p, FB, xTs[s % 2], xbs[s % 2], NTK, waps[0], waps[1], waps[2], DFF, yacc, yb, hooks=hooks_l, hooks_early=hooks_e, preloaded=(s > 0))
            if nxt:
                ffn_load(p, FB, waps[0], waps[1], waps[2], 0, 512)
            xq = xt + xt2
            nsub = NTK // 128

            def rload(sub):
                r0 = s * NTK + sub * 128
                xs = xq[sub % 4]
                p.dma("sp", lambda e: e.dma_start(out=xs[:], in_=x_src.ap()[r0:r0 + 128, :]), reads=[x_src.b(r0 // 128)], writes=[xs.b])

            for sub in range(3):
                rload(sub)
            for sub in range(nsub):
                r0 = s * NTK + sub * 128
                xs = xq[sub % 4]
                p.op("pool", lambda e, xs=xs, sub=sub: e.tensor_tensor(out=xs[:], in0=xs[:], in1=yacc[:, sub, :], op=ALU.add), reads=[xs.b, yb[sub]], writes=[xs.b])
                if sub + 3 < nsub:
                    rload(sub + 3)
                p.dma("sp", lambda e, xs=xs, r0=r0: e.dma_start(out=x_dst.ap()[r0:r0 + 128, :], in_=xs[:]), reads=[xs.b], writes=[x_dst.b(r0 // 128)])

        for s in range(TT // NTK):
            do_super(s)


def p5_weights_alloc(p):
    PW = {"win": T(p, "win1", [128, 8, 2560], BF16), "wbs": [Buf() for _ in range(5)],
          "wa": T(p, "wa", [128, 4, 128], BF16), "wx": T(p, "wx", [128, 4, 128], BF16)}
    return PW


def p5_weights_load(p, IN, PW):
    win, wbs, wa, wx = PW["win"], PW["wbs"], PW["wa"], PW["wx"]
    for cg in (1, 2, 0, 3, 4):
        p.dma("pool", lambda e, cg=cg: e.dma_start(out=win[:, :, cg * 512:(cg + 1) * 512],
              in_=IN["od_w_in"].ap()[:, cg * 512:(cg + 1) * 512].rearrange("(k p) n -> p k n", p=128)), writes=[wbs[cg]])
    p.dma("pool", lambda e: e.dma_start(out=wa[:], in_=IN["od_wa"].ap()), writes=[wa.b])
    p.dma("pool", lambda e: e.dma_start(out=wx[:], in_=IN["od_wx"].ap()), writes=[wx.b])


def phase5(p, IN, env, x_src, x_dst, PW):
    with p.phase():
        C, G, W = env["CL"](), env["GL"](), env["newW"]()
        ident = C["ident"]
        win, wbs, wa, wx = PW["win"], PW["wbs"], PW["wa"], PW["wx"]
        wo = T(p, "wo1", [128, 8, DM], BF16)
        p.dma("pool", lambda e: e.dma_start(out=wo[:], in_=IN["od_w_out"].ap().rearrange("(k p) n -> p k n", p=128)), writes=[wo.b])
        sm = T(p, "sm", [128, 4, 12], F32)
        p.dma("sp", lambda e: e.dma_start(out=sm[:], in_=IN["od_small"].ap()), writes=[sm.b])
        asc = T(p, "asc", [128, 4], F32)
        p.op("act", lambda e: e.activation(out=asc[:], in_=sm[:, :, 10], func=AF.Exp, scale=-1.0), reads=[sm.b], writes=[asc.b])
        p.op("act", lambda e: e.activation(out=asc[:], in_=asc[:], func=AF.Ln, bias=1.0), reads=[asc.b], writes=[asc.b])
        p.op("dve", lambda e: e.tensor_scalar(out=asc[:], in0=asc[:], scalar1=-8.0, scalar2=None, op0=ALU.mult), reads=[asc.b], writes=[asc.b])
        xt = [T(p, "xt%d" % i, [128, DM], F32) for i in range(2)]
        xo = [T(p, "xo%d" % i, [128, DM], F32) for i in range(2)]
        xt2 = [T(p, "xtr%d" % i, [128, DM], F32) for i in range(2)]
        ht = [T(p, "ht%d" % i, [128, DM], BF16) for i in range(2)]
        hT = [T(p, "hT%d" % i, [128, 8, 512], BF16) for i in range(2)]
        hTb = [[Buf() for _ in range(4)] for _ in range(2)]
        ymT = [T(p, "ymT%d" % i, [128, 8, 512], BF16) for i in range(2)]
        ymb = [[Buf() for _ in range(8)] for _ in range(2)]
        pT = T(p, "pT", [128, 8, 128], BF16, psum=True)
        pF = [T(p, "pF%d" % i, [128, 512], F32, psum=True) for i in range(5)]
        pY = [T(p, "pY%d" % i, [128, 512], F32, psum=True) for i in range(2)]
        vbuf = [T(p, "vbuf%d" % c, [128, 514], F32) for c in range(4)]
        xbuf = [T(p, "xbuf%d" % c, [128, 515], F32) for c in range(4)]
        hprev = T(p, "hprev", [128, 4], F32)
        ctmp = [{k: T(p, "ct%d_%s" % (i, k), [128, 512], F32) for k in ("gc", "o")} for i in range(4)]
        ltmp = [{k: T(p, "lt%d_%s" % (i, k), [128, 512], F32) for k in ("xc", "r", "ig", "a", "a2", "b", "h", "xg", "x2", "th", "gl")} for i in range(2)]
        xcbs = [T(p, "xcb%d" % i, [128, 512], BF16) for i in range(2)]
        for c in range(4):
            p.op("pool", lambda e, c=c: e.memset(vbuf[c][:], 0.0), writes=[vbuf[c].b])
            p.op("pool", lambda e, c=c: e.memset(xbuf[c][:], 0.0), writes=[xbuf[c].b])
        p.op("pool", lambda e: e.memset(hprev[:], 0.0), writes=[hprev.b])
        cnt = [0]
        NT5 = TT // 512

        def run_rr(gens):
            gens = list(gens)
            while gens:
                for g_ in list(gens):
                    try:
                        next(g_)
                    except StopIteration:
                        gens.remove(g_)

        def A_gen(it):
            par = it % 2
            for part, sub in ((1, 0), (1, 1), (2, 0), (1, 2), (2, 1), (1, 3), (2, 2), (2, 3)):
                load_norm_T(p, W, x_src, it * 512 + sub * 128, G[2], xt[sub % 2], ht[sub % 2], pT, ident, hT[par], sub * 128, hTb[par][sub], part=part)
                yield

        def proj(it, c0):
            hTt, hbs = hT[it % 2], hTb[it % 2]
            ps = pF[cnt[0] % 5]
            cnt[0] += 1
            for k in range(8):
                p.op("pe", lambda e, k=k: e.matmul(ps[:], lhsT=win[:, k, c0:c0 + 128], rhs=hTt[:, k, :], start=(k == 0), stop=(k == 7)),
                     reads=[wbs[c0 // 512]] + hbs, writes=[ps.b])
            return ps

        def conv_gen(it, c):
            yt, ybs = ymT[it % 2], ymb[it % 2]
            vb = vbuf[c]
            gc, o = ctmp[c]["gc"], ctmp[c]["o"]
            pgc = proj(it, 512 + c * 128)
            p.op("act", lambda e: e.copy(out=gc[:], in_=pgc[:]), reads=[pgc.b], writes=[gc.b])
            yield
            pu = proj(it, 1024 + c * 128)
            p.op("dve", lambda e: e.tensor_tensor(out=vb[:, 2:514], in0=pu[:], in1=gc[:], op=ALU.mult), reads=[pu.b, gc.b], writes=[vb.b])
            yield
            p.op("dve", lambda e: e.tensor_scalar(out=o[:], in0=vb[:, 2:514], scalar1=sm[:, c, 2:3], scalar2=None, op0=ALU.mult), reads=[vb.b, sm.b], writes=[o.b])
            yield
            p.op("dve", lambda e: e.scalar_tensor_tensor(out=o[:], in0=vb[:, 1:513], scalar=sm[:, c, 1:2], in1=o[:], op0=ALU.mult, op1=ALU.add), reads=[vb.b, sm.b, o.b], writes=[o.b])
            yield
            p.op("dve", lambda e: e.scalar_tensor_tensor(out=o[:], in0=vb[:, 0:512], scalar=sm[:, c, 0:1], in1=o[:], op0=ALU.mult, op1=ALU.add), reads=[vb.b, sm.b, o.b], writes=[o.b])
            p.op("pool", lambda e: e.tensor_copy(out=vb[:, 0:2], in_=vb[:, 512:514]), reads=[vb.b], writes=[vb.b])
            yield
            pgb = proj(it, c * 128)
            p.op("dve", lambda e: e.tensor_tensor(out=yt[:, c, :], in0=pgb[:], in1=o[:], op=ALU.mult), reads=[pgb.b, o.b], writes=[ybs[c]])
            yield

        def lru_gen(it, c):
            yt, ybs = ymT[it % 2], ymb[it % 2]
            xb = xbuf[c]
            tm = ltmp[c % 2]
            xcb = xcbs[c % 2]
            xc, r, ig, a, a2, b, h, xg, x2, th, gl = (tm[k] for k in ("xc", "r", "ig", "a", "a2", "b", "h", "xg", "x2", "th", "gl"))
            pxr = proj(it, 1536 + c * 128)
            p.op("act", lambda e: e.copy(out=xb[:, 3:515], in_=pxr[:]), reads=[pxr.b], writes=[xb.b])
            yield
            p.op("dve", lambda e: e.tensor_scalar(out=xc[:], in0=xb[:, 3:515], scalar1=sm[:, c, 6:7], scalar2=sm[:, c, 7:8], op0=ALU.mult, op1=ALU.add), reads=[xb.b, sm.b], writes=[xc.b])
            yield
            for j, off in ((5, 2), (4, 1), (3, 0)):
                p.op("dve", lambda e, j=j, off=off: e.scalar_tensor_tensor(out=xc[:], in0=xb[:, off:off + 512], scalar=sm[:, c, j:j + 1], in1=xc[:], op0=ALU.mult, op1=ALU.add),
                     reads=[xb.b, sm.b, xc.b], writes=[xc.b])
                yield
            p.op("pool", lambda e: e.tensor_copy(out=xb[:, 0:3], in_=xb[:, 512:515]), reads=[xb.b], writes=[xb.b])
            p.op("act", lambda e: e.copy(out=xcb[:], in_=xc[:]), reads=[xc.b], writes=[xcb.b])
            yield
            pr = pF[cnt[0] % 5]
            cnt[0] += 1
            p.op("pe", lambda e: e.matmul(pr[:], lhsT=wa[:, c, :], rhs=xcb[:], start=True, stop=True), reads=[wa.b, xcb.b], writes=[pr.b])
            pi = pF[cnt[0] % 5]
            cnt[0] += 1
            p.op("pe", lambda e: e.matmul(pi[:], lhsT=wx[:, c, :], rhs=xcb[:], start=True, stop=True), reads=[wx.b, xcb.b], writes=[pi.b])
            yield
            p.op("act", lambda e: e.activation(out=r[:], in_=pr[:], func=AF.Sigmoid, bias=sm[:, c, 8:9]), reads=[pr.b, sm.b], writes=[r.b])
            p.op("act", lambda e: e.activation(out=ig[:], in_=pi[:], func=AF.Sigmoid, bias=sm[:, c, 9:10]), reads=[pi.b, sm.b], writes=[ig.b])
            yield
            p.op("act", lambda e: e.activation(out=a[:], in_=r[:], func=AF.Exp, scale=asc[:, c:c + 1]), reads=[r.b, asc.b], writes=[a.b])
            yield
            p.op("pool", lambda e: e.tensor_tensor(out=a2[:], in0=a[:], in1=a[:], op=ALU.mult), reads=[a.b], writes=[a2.b])
            yield
            p.op("act", lambda e: e.activation(out=a2[:], in_=a2[:], func=AF.Sqrt, scale=-1.0, bias=1.0), reads=[a2.b], writes=[a2.b])
            yield
            p.op("pool", lambda e: e.tensor_tensor(out=b[:], in0=a2[:], in1=ig[:], op=ALU.mult), reads=[a2.b, ig.b], writes=[b.b])
            yield
            p.op("pool", lambda e: e.tensor_tensor(out=b[:], in0=b[:], in1=xc[:], op=ALU.mult), reads=[b.b, xc.b], writes=[b.b])
            yield
            p.op("dve", lambda e: e.tensor_tensor_scan(out=h[:], data0=a[:], data1=b[:], initial=hprev[:, c:c + 1], op0=ALU.mult, op1=ALU.add),
                 reads=[a.b, b.b, hprev.b], writes=[h.b])
            p.op("pool", lambda e: e.tensor_copy(out=hprev[:, c:c + 1], in_=h[:, 511:512]), reads=[h.b], writes=[hprev.b])
            yield
            pxg = proj(it, 2048 + c * 128)
            p.op("act", lambda e: e.copy(out=xg[:], in_=pxg[:]), reads=[pxg.b], writes=[xg.b])
            yield
            p.op("pool", lambda e: e.tensor_tensor(out=x2[:], in0=xg[:], in1=xg[:], op=ALU.mult), reads=[xg.b], writes=[x2.b])
            yield
            p.op("dve", lambda e: e.tensor_scalar(out=x2[:], in0=x2[:], scalar1=0.044715, scalar2=1.0, op0=ALU.mult, op1=ALU.add), reads=[x2.b], writes=[x2.b])
            yield
            p.op("pool", lambda e: e.tensor_tensor(out=x2[:], in0=x2[:], in1=xg[:], op=ALU.mult), reads=[x2.b, xg.b], writes=[x2.b])
            yield
            p.op("act", lambda e: e.activation(out=th[:], in_=x2[:], func=AF.Tanh, scale=0.7978845608028654), reads=[x2.b], writes=[th.b])
            yield
            p.op("dve", lambda e: e.scalar_tensor_tensor(out=gl[:], in0=th[:], scalar=1.0, in1=xg[:], op0=ALU.add, op1=ALU.mult), reads=[th.b, xg.b], writes=[gl.b])
            yield
            p.op("dve", lambda e: e.scalar_tensor_tensor(out=yt[:, 4 + c, :], in0=h[:], scalar=0.5, in1=gl[:], op0=ALU.mult, op1=ALU.mult), reads=[h.b, gl.b], writes=[ybs[4 + c]])
            yield

        def out_gen(it):
            yt, ybs = ymT[it % 2], ymb[it % 2]
            t0 = it * 512
            for sub in range(4):
                n = it * 4 + sub
                xs, xos = xt2[n % 2], xo[n % 2]
                r0 = t0 + sub * 128
                p.dma("sp", lambda e, xs=xs, r0=r0: e.dma_start(out=xs[:], in_=x_src.ap()[r0:r0 + 128, :]), reads=[x_src.b(r0 // 128)], writes=[xs.b])
                for half in range(2):
                    py = pY[(n * 2 + half) % 2]
                    for k in range(8):
                        p.op("pe", lambda e, k=k, py=py, sub=sub, half=half: e.matmul(py[:], lhsT=yt[:, k, sub * 128:(sub + 1) * 128], rhs=wo[:, k, half * 512:(half + 1) * 512], start=(k == 0), stop=(k == 7)),
                             reads=[ybs[k], wo.b], writes=[py.b])
                    p.op("dve", lambda e, py=py, xs=xs, xos=xos, half=half: e.tensor_tensor(out=xos[:, half * 512:(half + 1) * 512], in0=py[:], in1=xs[:, half * 512:(half + 1) * 512], op=ALU.add),
                         reads=[py.b, xs.b], writes=[xos.b])
                    yield
                p.dma("sp", lambda e, xos=xos, r0=r0: e.dma_start(out=x_dst.ap()[r0:r0 + 128, :], in_=xos[:]), reads=[xos.b], writes=[x_dst.b(r0 // 128)])

        run_rr([A_gen(0)])
        for it in range(NT5):
            extra = []
            if it + 1 < NT5:
                extra.append(A_gen(it + 1))
            if it >= 1:
                extra.append(out_gen(it - 1))
            run_rr([conv_gen(it, 0), conv_gen(it, 1), lru_gen(it, 0), lru_gen(it, 1)] + extra)
            run_rr([conv_gen(it, 2), conv_gen(it, 3), lru_gen(it, 2), lru_gen(it, 3)])
        run_rr([out_gen(NT5 - 1)])


BIGIDX = 1.0e6


def phase6(p, IN, env, x_src, xg, R):
    with p.phase():
        C, G, W = env["CL"](), env["GL"](), env["newW"]()
        identf, triex, ones, eoff = C["identf"], C["triex"], C["ones"], C["eoff"]
        rt, idx12 = R["rt"], R["idx12"]
        wr = T(p, "wr", [128, 8, 8], F32)
        p.dma("sp", lambda e: e.dma_start(out=wr[:], in_=IN["od_rw"].ap().rearrange("(k p) n -> p k n", p=128)), writes=[wr.b])
        rbt = T(p, "rbt", [128, 8], F32)
        p.dma("sp", lambda e: e.dma_start(out=rbt[:], in_=IN["od_rb"].ap().partition_broadcast(128)), writes=[rbt.b])
        zb = []
        xt = [T(p, "xt%d" % i, [128, DM], F32) for i in range(2)]
        hf = [T(p, "hf%d" % i, [128, DM], F32) for i in range(2)]
        hb = [T(p, "hb%d" % i, [128, DM], BF16) for i in range(4)]
        pTf = [T(p, "pTf%d" % i, [128, 4, 128], F32, psum=True) for i in range(2)]
        hTfs = [T(p, "hTf%d" % i, [128, 8, 128], F32) for i in range(2)]
        pLs = [T(p, "pL%d" % i, [128, 8], F32, psum=True) for i in range(2)]
        pP = T(p, "pP", [128, 8], F32, psum=True)
        pC = T(p, "pC", [128, 8], F32, psum=True)
        cntb = T(p, "cntb", [128, 8], F32)
        p.op("pool", lambda e: e.memset(cntb[:], 0.0), writes=[cntb.b])
        s8 = {k: T(p, "s8_" + k, [128, 8], F32) for k in ("lg", "mx", "sel2", "pos", "v", "dst", "junk")}
        selb = T(p, "selb", [128, 8], BF16)
        s1 = {k: T(p, "s1_" + k, [128, 1], F32) for k in ("d", "ex", "i1", "i2")}
        dsti = [T(p, "dsti%d" % i, [128, 8], I32) for i in range(2)]

        regbox = {}

        def bcreg(e):
            if "r" not in regbox:
                regbox["r"] = e.to_reg(NEXP * CAP - 1)
            return regbox["r"]

        def front(n):
            xs, hfs, hbs = xt[n % 2], hf[n % 2], hb[n % 4]
            hTf, pL = hTfs[n % 2], pLs[n % 2]
            r0 = n * 128
            p.dma("sp", lambda e: e.dma_start(out=xs[:], in_=x_src.ap()[r0:r0 + 128, :]), reads=[x_src.b(n)], writes=[xs.b])
            rmsnorm_tile(p, W, xs, G[3], hfs)
            yield
            p.op("act", lambda e: e.copy(out=hbs[:], in_=hfs[:]), reads=[hfs.b], writes=[hbs.b])
            for k in range(8):
                pt = pTf[k // 4]
                p.op("pe", lambda e, k=k, pt=pt: e.transpose(out=pt[:, k % 4, :], in_=hfs[:, k * 128:(k + 1) * 128], identity=identf[:]), reads=[hfs.b, identf.b], writes=[pt.b])
            yield
            for j in range(2):
                p.op("act", lambda e, j=j: e.copy(out=hTf[:, j * 4:(j + 1) * 4, :], in_=pTf[j][:]), reads=[pTf[j].b], writes=[hTf.b])
            yield
            for k in range(8):
                p.op("pe", lambda e, k=k: e.matmul(pL[:], lhsT=hTf[:, k, :], rhs=wr[:, k, :], start=(k == 0), stop=(k == 7)), reads=[hTf.b, wr.b], writes=[pL.b])
            yield

        def back(n):
            hbs = hb[n % 4]
            pL = pLs[n % 2]
            lg, mx, sel2, pos, v, dst, junk = (s8[k] for k in ("lg", "mx", "sel2", "pos", "v", "dst", "junk"))
            d, ex, i1, i2 = (s1[k] for k in ("d", "ex", "i1", "i2"))
            ops = [
                lambda: p.op("dve", lambda e: e.tensor_tensor(out=lg[:], in0=pL[:], in1=rbt[:], op=ALU.add), reads=[pL.b, rbt.b], writes=[lg.b]),
                lambda: p.op("dve", lambda e: e.max(out=mx[:], in_=lg[:]), reads=[lg.b], writes=[mx.b]),
                lambda: p.op("dve", lambda e: e.tensor_scalar(out=rt[:, n, 0:8], in0=lg[:], scalar1=mx[:, 1:2], scalar2=None, op0=ALU.is_ge), reads=[lg.b, mx.b], writes=[rt.b]),
                lambda: p.op("dve", lambda e: e.tensor_scalar(out=rt[:, n, 8:16], in0=lg[:], scalar1=mx[:, 0:1], scalar2=None, op0=ALU.is_ge), reads=[lg.b, mx.b], writes=[rt.b]),
                lambda: p.op("dve", lambda e: e.tensor_tensor(out=sel2[:], in0=rt[:, n, 0:8], in1=rt[:, n, 8:16], op=ALU.subtract), reads=[rt.b], writes=[sel2.b]),
                lambda: p.op("dve", lambda e: e.tensor_tensor(out=rt[:, n, 16:17], in0=mx[:, 1:2], in1=mx[:, 0:1], op=ALU.subtract), reads=[mx.b], writes=[rt.b]),
                lambda: p.op("dve", lambda e: e.tensor_copy(out=selb[:], in_=rt[:, n, 0:8]), reads=[rt.b], writes=[selb.b]),
                lambda: (p.op("pe", lambda e: e.matmul(pP[:], lhsT=triex[:], rhs=selb[:], start=True, stop=True), reads=[triex.b, selb.b], writes=[pP.b]),
                         p.op("pe", lambda e: e.matmul(pC[:], lhsT=ones[:], rhs=selb[:], start=True, stop=True), reads=[ones.b, selb.b], writes=[pC.b])),
                lambda: (p.op("dve", lambda e: e.tensor_tensor(out=pos[:], in0=pP[:], in1=cntb[:], op=ALU.add), reads=[pP.b, cntb.b], writes=[pos.b]),
                         p.op("dve", lambda e: e.tensor_tensor(out=cntb[:], in0=pC[:], in1=cntb[:], op=ALU.add), reads=[pC.b, cntb.b], writes=[cntb.b])),
                lambda: p.op("dve", lambda e: e.tensor_scalar(out=v[:], in0=pos[:], scalar1=CAP - 0.5, scalar2=None, op0=ALU.is_lt), reads=[pos.b], writes=[v.b]),
                lambda: p.op("dve", lambda e: e.tensor_tensor(out=v[:], in0=v[:], in1=rt[:, n, 0:8], op=ALU.mult), reads=[v.b, rt.b], writes=[v.b]),
                lambda: p.op("dve", lambda e: e.tensor_tensor(out=dst[:], in0=pos[:], in1=eoff[:], op=ALU.add), reads=[pos.b, eoff.b], writes=[dst.b]),
                lambda: p.op("dve", lambda e: e.scalar_tensor_tensor(out=dst[:], in0=dst[:], scalar=-BIGIDX, in1=v[:], op0=ALU.add, op1=ALU.mult), reads=[dst.b, v.b], writes=[dst.b]),
                lambda: p.op("dve", lambda e: e.tensor_scalar(out=dst[:], in0=dst[:], scalar1=BIGIDX, scalar2=None, op0=ALU.add), reads=[dst.b], writes=[dst.b]),
                lambda: p.op("dve", lambda e: e.tensor_tensor(out=junk[:], in0=rt[:, n, 8:16], in1=dst[:], op=ALU.mult), reads=[rt.b, dst.b], writes=[junk.b]),
                lambda: p.op("dve", lambda e: e.tensor_reduce(out=i1[:], in_=junk[:], axis=AX.X, op=ALU.add), reads=[junk.b], writes=[i1.b]),
                lambda: p.op("dve", lambda e: e.tensor_copy(out=idx12[:, n, 0:1], in_=i1[:]), reads=[i1.b], writes=[idx12.b]),
                lambda: p.op("dve", lambda e: e.tensor_tensor(out=junk[:], in0=sel2[:], in1=dst[:], op=ALU.mult), reads=[sel2.b, dst.b], writes=[junk.b]),
                lambda: p.op("dve", lambda e: e.tensor_reduce(out=i2[:], in_=junk[:], axis=AX.X, op=ALU.add), reads=[junk.b], writes=[i2.b]),
                lambda: p.op("dve", lambda e: e.tensor_copy(out=idx12[:, n, 1:2], in_=i2[:]), reads=[i2.b], writes=[idx12.b]),
            ]
            for k, f_ in enumerate(ops):
                f_()
                if k % 4 == 3:
                    yield
            for j in range(2):
                p.dma("pool", lambda e, j=j: e.indirect_dma_start(out=xg.ap(), out_offset=bass.IndirectOffsetOnAxis(ap=idx12[:, n, j:j + 1], axis=0), in_=hbs[:], in_offset=None,
                                                                   bounds_check=bcreg(e), oob_is_err=False),
                      reads=[hbs.b, idx12.b] + zb, writes=[Buf()])
            yield

        def run_rr(gens):
            gens = list(gens)
            while gens:
                for g_ in list(gens):
                    try:
                        next(g_)
                    except StopIteration:
                        gens.remove(g_)

        run_rr([front(0)])
        for n in range(NSUB):
            run_rr([back(n)] + ([front(n + 1)] if n + 1 < NSUB else []))
        exa = T(p, "exa", [128, NSUB], F32)
        dna = T(p, "dna", [128, NSUB], F32)
        p.op("act", lambda e: e.activation(out=exa[:], in_=rt[:, :, 16], func=AF.Exp), reads=[rt.b], writes=[exa.b])
        p.op("dve", lambda e: e.tensor_scalar(out=dna[:], in0=exa[:], scalar1=1.0, scalar2=None, op0=ALU.add), reads=[exa.b], writes=[dna.b])
        p.op("dve", lambda e: e.reciprocal(out=rt[:, :, 16], in_=dna[:]), reads=[dna.b], writes=[rt.b])
        p.op("dve", lambda e: e.tensor_tensor(out=rt[:, :, 17], in0=exa[:], in1=rt[:, :, 16], op=ALU.mult), reads=[exa.b, rt.b], writes=[rt.b])

def phase7(p, IN, env, xg, yg):
    with p.phase():
        C = env["CL"]()
        ident = C["ident"]
        FB = ffn_alloc(p, CAP)
        NT_ = CAP // 128
        pT = T(p, "pT", [128, 8, 128], BF16, psum=True)
        xTs = [T(p, "xT%d" % i, [128, 8, CAP], BF16) for i in range(2)]
        xbs = [[Buf() for _ in range(NT_)] for _ in range(2)]
        yacc = T(p, "yacc", [128, NT_, DM], F32)
        yb = [Buf() for _ in range(NT_)]
        xr = [T(p, "xr%d" % i, [128, DM], BF16) for i in range(2)]

        def prep(ex_, n, part=0):
            r0 = ex_ * CAP + n * 128
            xs = xr[n % 2]
            xT = xTs[ex_ % 2]
            if part in (0, 1):
                p.dma("sp", lambda e: e.dma_start(out=xs[:], in_=xg.ap()[r0:r0 + 128, :]), writes=[xs.b])
            if part == 1:
                return
            for k in range(8):
                p.op("pe", lambda e, k=k: e.transpose(out=pT[:, k, :], in_=xs[:, k * 128:(k + 1) * 128], identity=ident[:]), reads=[xs.b, ident.b], writes=[pT.b])
            p.op("act", lambda e: e.copy(out=xT[:, :, n * 128:(n + 1) * 128], in_=pT[:]), reads=[pT.b], writes=[xbs[ex_ % 2][n]])

        for n in range(NT_):
            prep(0, n)

        def do_expert(ex_):
            nxt = ex_ + 1 < NEXP
            hooks_e = [(lambda n=n: prep(ex_ + 1, n, 1)) for n in range(NT_)] if nxt else []
            hooks_l = [(lambda n=n: prep(ex_ + 1, n, 2)) for n in range(NT_)] if nxt else []
            ffn_block(p, FB, xTs[ex_ % 2], xbs[ex_ % 2], CAP, IN["od_eg"].ap()[ex_], IN["od_eu"].ap()[ex_], IN["od_ed"].ap()[ex_], DEXP, yacc, yb, hooks=hooks_l, hooks_early=hooks_e,
                      preloaded=(ex_ > 0))
            if nxt:
                ffn_load(p, FB, IN["od_eg"].ap()[ex_ + 1], IN["od_eu"].ap()[ex_ + 1], IN["od_ed"].ap()[ex_ + 1], 0, 512)
            p.dma("sp", lambda e: e.dma_start(out=yg.ap()[ex_ * CAP:(ex_ + 1) * CAP, :].rearrange("(n p) d -> p n d", p=128), in_=yacc[:]), reads=yb, writes=[Buf()])

        for ex_ in range(NEXP):
            do_expert(ex_)


def phase8(p, IN, env, x_src, yg, out, R):
    with p.phase():
        G, W = env["GL"](), env["newW"]()
        rt, idx12 = R["rt"], R["idx12"]
        NB8 = 6
        xt = [T(p, "xt%d" % i, [128, DM], F32) for i in range(NB8)]
        g1 = [T(p, "g1_%d" % i, [128, DM], F32) for i in range(NB8)]
        g2 = [T(p, "g2_%d" % i, [128, DM], F32) for i in range(NB8)]
        ot = [T(p, "ot%d" % i, [128, DM], F32) for i in range(NB8)]

        regbox = {}

        def bcreg(e):
            if "r" not in regbox:
                regbox["r"] = e.to_reg(NEXP * CAP - 1)
            return regbox["r"]

        def do_tile(n):
            xs, a, b, o = xt[n % NB8], g1[n % NB8], g2[n % NB8], ot[n % NB8]
            r0 = n * 128
            Wn = Ws[n % 2]
            sq, ss, rs = Wn["sq"], Wn["ss"], Wn["rs"]
            gfin = G[4]
            for j, gt in enumerate((a, b)):
                p.op("dve", lambda e, gt=gt, j=j: e.scalar_tensor_tensor(out=xs[:], in0=gt[:], scalar=rt[:, n, 16 + j:17 + j], in1=xs[:], op0=ALU.mult, op1=ALU.add), reads=[gt.b, rt.b, xs.b], writes=[xs.b])
                yield
            p.op("act", lambda e: e.activation(out=sq[:], in_=xs[:], func=AF.Square, accum_out=ss[:]), reads=[xs.b], writes=[sq.b, ss.b])
            yield
            p.op("dve", lambda e: e.tensor_scalar(out=rs[:], in0=ss[:], scalar1=1.0 / DM, scalar2=EPS, op0=ALU.mult, op1=ALU.add), reads=[ss.b], writes=[rs.b])
            yield
            p.op("act", lambda e: e.activation(out=rs[:], in_=rs[:], func=AF.Sqrt), reads=[rs.b], writes=[rs.b])
            yield
            p.op("dve", lambda e: e.reciprocal(out=rs[:], in_=rs[:]), reads=[rs.b], writes=[rs.b])
            yield
            p.op("dve", lambda e: e.scalar_tensor_tensor(out=o[:], in0=xs[:], scalar=rs[:], in1=gfin[:], op0=ALU.mult, op1=ALU.mult), reads=[xs.b, rs.b, gfin.b], writes=[o.b])
            p.dma("sp", lambda e: e.dma_start(out=out.ap()[r0:r0 + 128, :], in_=o[:]), reads=[o.b], writes=[Buf()])
            yield

        def fetch(n):
            xs, a, b = xt[n % NB8], g1[n % NB8], g2[n % NB8]
            r0 = n * 128
            p.dma("sp", lambda e: e.dma_start(out=xs[:], in_=x_src.ap()[r0:r0 + 128, :]), writes=[xs.b])
            for j, gt in enumerate((a, b)):
                p.op("act", lambda e, gt=gt: e.memzero(gt[:]), writes=[gt.b])
                p.dma("pool", lambda e, gt=gt, j=j: e.indirect_dma_start(out=gt[:], out_offset=None, in_=yg.ap(), in_offset=bass.IndirectOffsetOnAxis(ap=idx12[:, n, j:j + 1], axis=0),
                                                                        bounds_check=bcreg(e), oob_is_err=False), reads=[idx12.b], writes=[gt.b])

        Ws = [W, env["newW"]()]

        def run_rr(gens):
            gens = list(gens)
            while gens:
                for g_ in list(gens):
                    try:
                        next(g_)
                    except StopIteration:
                        gens.remove(g_)

        for n in range(NB8 - 2):
            fetch(n)
        for n in range(0, NSUB, 2):
            for m_ in (n, n + 1):
                if m_ + NB8 - 2 < NSUB:
                    fetch(m_ + NB8 - 2)
            run_rr([do_tile(n), do_tile(n + 1)])
```
